# Optimizing a Trainium2 kernel written in Bass

```python
import jax
import jax.numpy as jnp
from jax import lax
import numpy as np

D_MODEL = 4096
BATCH = 2
SEQ = 4096
DEPTH = 2

HEAD_DIM = 128
A_HEADS = (D_MODEL // 2) // HEAD_DIM
A_WIDTH = A_HEADS * HEAD_DIM
A_PATTERNS = ((128, 1), (512, 4), (2048, 16))
B_WIDTH = D_MODEL - A_WIDTH
B_HEADS = 4
B_V_DIM = B_WIDTH // B_HEADS
B_QK_DIM = B_V_DIM // 2
B_QK_WIDTH = B_HEADS * B_QK_DIM
B_CONV_WIDTH = 4
B_CHUNK = 64
C_HEADS = (D_MODEL // 2) // HEAD_DIM
C_WIDTH = C_HEADS * HEAD_DIM
MOBA_BLOCK = 256
MOBA_TOPK = 3
MOBA_QUERY_CHUNK = 16
D_CHANNELS = D_MODEL - C_WIDTH
D_CONV_WIDTH = 31
D_FF = 2 * D_MODEL
N_SUBLAYERS = 3
ADA_WIDTH = 3 * N_SUBLAYERS * D_MODEL
AB_IN_WIDTH = 3 * A_WIDTH + 2 * B_QK_WIDTH + 2 * B_WIDTH + 2 * B_HEADS
CD_IN_WIDTH = 3 * C_WIDTH + 2 * D_CHANNELS
DEEPNORM_ALPHA = (2 * DEPTH) ** 0.25
DEEPNORM_BETA = (8 * DEPTH) ** -0.25
LN_EPS = 1e-5

kernel_name = 'hybrid_dilated_mlstm_moba_conv_block'


def layer_norm(x, g, b):
    xf = x.astype(jnp.float32)
    mu = xf.mean(-1, keepdims=True)
    var = jnp.square(xf - mu).mean(-1, keepdims=True)
    return ((xf - mu) * lax.rsqrt(var + LN_EPS)).astype(x.dtype) * g + b


def split_cols(t, sizes):
    out, start = [], 0
    for n in sizes:
        out.append(t[..., start:start + n])
        start += n
    return out


def split_heads(t, n_heads):
    b, s, _ = t.shape
    return t.reshape(b, s, n_heads, -1).transpose(0, 2, 1, 3)


def merge_heads(t):
    b, h, s, d = t.shape
    return t.transpose(0, 2, 1, 3).reshape(b, s, h * d)


def alibi_slopes(n_heads):
    return jnp.asarray(2.0 ** (-8.0 * np.arange(1, n_heads + 1) / n_heads), jnp.float32)


def causal_depthwise_conv(x, w):
    width, ch = w.shape
    xp = jnp.pad(x, ((0, 0), (width - 1, 0), (0, 0)))
    return lax.conv_general_dilated(xp, w[:, None, :], window_strides=(1,), padding='VALID',
                                    dimension_numbers=('NWC', 'WIO', 'NWC'), feature_group_count=ch)


def swiglu_ffn(u, w_up, w_down):
    g, v = jnp.split(u @ w_up, 2, axis=-1)
    return (jax.nn.silu(g) * v) @ w_down


def dilated_window_branch(q, k, v, slopes, window, dilation):
    b, h, s, dh = q.shape
    steps = window // dilation
    unit = dilation * steps
    p = -(-s // unit) * unit
    n_blk = p // unit

    def to_strided(t):
        t = jnp.pad(t, ((0, 0), (0, 0), (0, p - s), (0, 0)))
        t = t.reshape(b, h, p // dilation, dilation, dh).transpose(0, 1, 3, 2, 4)
        return t.reshape(b, h, dilation, n_blk, steps, dh)

    def with_prev(t):
        prev = jnp.pad(t, ((0, 0), (0, 0), (0, 0), (1, 0), (0, 0), (0, 0)))[:, :, :, :-1]
        return jnp.concatenate([prev, t], axis=4)

    qs = to_strided(q)
    ks = with_prev(to_strided(k))
    vs = with_prev(to_strided(v))
    scores = jnp.einsum('bhrnqd,bhrnkd->bhrnqk', qs, ks).astype(jnp.float32)
    step = steps + jnp.arange(steps)[:, None] - jnp.arange(2 * steps)[None, :]
    in_band = (step >= 0) & (step <= steps)
    after_start = (jnp.arange(n_blk)[:, None, None] > 0) | (jnp.arange(2 * steps)[None, None, :] >= steps)
    mask = in_band[None] & after_start
    dist = (step * dilation).astype(jnp.float32)
    scores = jnp.where(mask, scores - slopes[:, None, None, None, None] * dist, -jnp.inf)
    row_max = scores.max(-1, keepdims=True)
    probs = jnp.exp(scores - row_max)
    denom = probs.sum(-1)
    out = jnp.einsum('bhrnqk,bhrnkd->bhrnqd', probs, vs) / denom[..., None]

    def from_strided(t):
        t = t.reshape(b, h, dilation, p // dilation, *t.shape[5:])
        t = jnp.moveaxis(t, 2, 3)
        return t.reshape(b, h, p, *t.shape[4:])[:, :, :s]

    return from_strided(out), from_strided(row_max[..., 0]), from_strided(denom)


def dilated_attention(q, k, v, slopes):
    branches = [dilated_window_branch(q, k, v, slopes, w, d) for (w, d) in A_PATTERNS]
    outs = jnp.stack([o for o, _, _ in branches])
    maxes = jnp.stack([m for _, m, _ in branches])
    denoms = jnp.stack([z for _, _, z in branches])
    weight = denoms * jnp.exp(maxes - maxes.max(0, keepdims=True))
    return (weight[..., None] * outs).sum(0) / weight.sum(0)[..., None]


def mlstm_chunkwise(q, k, v, i_pre, f_pre):
    b, h, s, dk = q.shape
    dv = v.shape[-1]
    nc = s // B_CHUNK

    def chunks(t):
        return jnp.moveaxis(t.reshape(b, h, nc, B_CHUNK, *t.shape[3:]), 2, 0)

    causal = jnp.tril(jnp.ones((B_CHUNK, B_CHUNK), bool))

    def step(carry, xs):
        c_state, n_state, m_state = carry
        qc, kc, vc, ic, lfc = xs
        cum = jnp.cumsum(lfc, axis=-1)
        log_w = jnp.where(causal, cum[..., :, None] - cum[..., None, :] + ic[..., None, :], -jnp.inf)
        log_inter = cum + m_state[..., None]
        m_row = jnp.maximum(log_inter, log_w.max(-1))
        w_intra = jnp.exp(log_w - m_row[..., None])
        w_inter = jnp.exp(log_inter - m_row)
        attn = w_intra * jnp.einsum('bhtd,bhsd->bhts', qc, kc)
        num = w_inter[..., None] * jnp.einsum('bhtd,bhde->bhte', qc, c_state) + jnp.einsum('bhts,bhse->bhte', attn, vc)
        den = w_inter * jnp.einsum('bhtd,bhd->bht', qc, n_state) + attn.sum(-1)
        h_out = num / jnp.maximum(jnp.abs(den), jnp.exp(-m_row))[..., None]
        log_to_end = cum[..., -1:] - cum + ic
        m_new = jnp.maximum(cum[..., -1] + m_state, log_to_end.max(-1))
        w_end = jnp.exp(log_to_end - m_new[..., None])
        decay = jnp.exp(cum[..., -1] + m_state - m_new)
        c_new = decay[..., None, None] * c_state + jnp.einsum('bhs,bhsd,bhse->bhde', w_end, kc, vc)
        n_new = decay[..., None] * n_state + jnp.einsum('bhs,bhsd->bhd', w_end, kc)
        return (c_new, n_new, m_new), h_out

    f32 = jnp.float32
    init = (jnp.zeros((b, h, dk, dv), f32), jnp.zeros((b, h, dk), f32), jnp.zeros((b, h), f32))
    xs = (chunks(q), chunks(k), chunks(v), chunks(i_pre), chunks(jax.nn.log_sigmoid(f_pre)))
    _, hs = lax.scan(step, init, xs)
    return jnp.moveaxis(hs, 0, 2).reshape(b, h, s, dv)


def mixer_dilated_mlstm(u, w_in, conv_qk, b_igate, b_fgate, norm_g, w_out):
    a_q, a_k, a_v, b_qk, b_v, b_o, b_i, b_f = split_cols(
        u @ w_in, (A_WIDTH, A_WIDTH, A_WIDTH, 2 * B_QK_WIDTH, B_WIDTH, B_WIDTH, B_HEADS, B_HEADS))
    attn = dilated_attention(split_heads(a_q, A_HEADS) * HEAD_DIM ** -0.5, split_heads(a_k, A_HEADS),
                             split_heads(a_v, A_HEADS), alibi_slopes(A_HEADS))
    a_out = merge_heads(attn).astype(u.dtype)
    b_q, b_k = split_cols(jax.nn.silu(causal_depthwise_conv(b_qk, conv_qk)), (B_QK_WIDTH, B_QK_WIDTH))
    f32 = jnp.float32
    h_cell = mlstm_chunkwise(
        split_heads(b_q, B_HEADS).astype(f32),
        split_heads(b_k, B_HEADS).astype(f32) * B_QK_DIM ** -0.5,
        split_heads(b_v, B_HEADS).astype(f32),
        (b_i + b_igate).astype(f32).transpose(0, 2, 1),
        (b_f + b_fgate).astype(f32).transpose(0, 2, 1))
    mu = h_cell.mean(-1, keepdims=True)
    var = jnp.square(h_cell - mu).mean(-1, keepdims=True)
    h_norm = merge_heads((h_cell - mu) * lax.rsqrt(var + LN_EPS)).astype(u.dtype) * norm_g
    b_out = jax.nn.sigmoid(b_o) * h_norm
    return jnp.concatenate([a_out, b_out], axis=-1) @ w_out


def moba_attention(q, k, v, slopes):
    b, h, s, dh = q.shape
    p = -(-s // MOBA_BLOCK) * MOBA_BLOCK
    n_blk = p // MOBA_BLOCK
    top_k = min(MOBA_TOPK, n_blk)
    pad = ((0, 0), (0, 0), (0, p - s), (0, 0))
    q, k, v = (jnp.pad(t, pad) for t in (q, k, v))
    k_blocks = k.reshape(b, h, n_blk, MOBA_BLOCK, dh)
    v_blocks = v.reshape(b, h, n_blk, MOBA_BLOCK, dh)
    gate = jnp.einsum('bhsd,bhnd->bhsn', q, k_blocks.mean(3)).astype(jnp.float32)
    q_blk = jnp.arange(p) // MOBA_BLOCK
    gate = jnp.where(jnp.arange(n_blk)[None, :] < q_blk[:, None], gate, -jnp.inf)
    _, sel = lax.top_k(gate, top_k)
    sel_valid = jnp.arange(top_k)[None, :] < q_blk[:, None]
    n_chunks = p // MOBA_QUERY_CHUNK

    def chunk_t(t):
        return jnp.moveaxis(t.reshape(b, h, n_chunks, MOBA_QUERY_CHUNK, *t.shape[3:]), 2, 0)

    xs = (chunk_t(q), chunk_t(sel), sel_valid.reshape(n_chunks, MOBA_QUERY_CHUNK, top_k),
          jnp.arange(p).reshape(n_chunks, MOBA_QUERY_CHUNK))
    bi = jnp.arange(b)[:, None, None, None]
    hi = jnp.arange(h)[None, :, None, None]
    key_off = jnp.arange(MOBA_BLOCK)

    def attend(args):
        qc, selc, validc, pos = args
        k_sel = k_blocks[bi, hi, selc]
        v_sel = v_blocks[bi, hi, selc]
        own = pos[0] // MOBA_BLOCK
        k_own = lax.dynamic_index_in_dim(k_blocks, own, axis=2, keepdims=False)
        v_own = lax.dynamic_index_in_dim(v_blocks, own, axis=2, keepdims=False)
        s_sel = jnp.einsum('bhqd,bhqjkd->bhqjk', qc, k_sel).astype(jnp.float32)
        dist_sel = (pos[:, None, None] - (selc[..., None] * MOBA_BLOCK + key_off)).astype(jnp.float32)
        s_sel = jnp.where(validc[:, :, None], s_sel - slopes[:, None, None, None] * dist_sel, -jnp.inf)
        dist_own = (pos[:, None] - (own * MOBA_BLOCK + key_off)[None, :]).astype(jnp.float32)
        s_own = jnp.einsum('bhqd,bhkd->bhqk', qc, k_own).astype(jnp.float32)
        s_own = jnp.where(dist_own >= 0, s_own - slopes[:, None, None] * dist_own, -jnp.inf)
        n_sel = top_k * MOBA_BLOCK
        probs = jax.nn.softmax(jnp.concatenate([s_sel.reshape(b, h, MOBA_QUERY_CHUNK, n_sel), s_own], axis=-1), axis=-1)
        p_sel = probs[..., :n_sel].reshape(b, h, MOBA_QUERY_CHUNK, top_k, MOBA_BLOCK)
        return (jnp.einsum('bhqjk,bhqjkd->bhqd', p_sel, v_sel)
                + jnp.einsum('bhqk,bhkd->bhqd', probs[..., n_sel:], v_own))

    out = lax.map(attend, xs)
    return jnp.moveaxis(out, 0, 2).reshape(b, h, p, dh)[:, :, :s]


def mixer_moba_conv(u, w_in, conv_dw, conv_ln_g, conv_ln_b, w_out):
    c_q, c_k, c_v, d_a, d_b = split_cols(u @ w_in, (C_WIDTH, C_WIDTH, C_WIDTH, D_CHANNELS, D_CHANNELS))
    attn = moba_attention(split_heads(c_q, C_HEADS) * HEAD_DIM ** -0.5, split_heads(c_k, C_HEADS),
                          split_heads(c_v, C_HEADS), alibi_slopes(C_HEADS))
    c_out = merge_heads(attn).astype(u.dtype)
    d = causal_depthwise_conv(d_a * jax.nn.sigmoid(d_b), conv_dw)
    d_out = jax.nn.silu(layer_norm(d, conv_ln_g, conv_ln_b))
    return jnp.concatenate([c_out, d_out], axis=-1) @ w_out


def setup_inputs(seed: int = 0) -> dict:
    key = jax.random.key(seed)
    keys = iter(jax.random.split(key, 64))

    def normal(shape, scale):
        return jax.random.normal(next(keys), shape, jnp.float32) * scale

    def gain(shape):
        return 1.0 + normal(shape, 0.02)

    inputs = {'x': normal((BATCH, SEQ, D_MODEL), 1.0), 'c': normal((BATCH, D_MODEL), 1.0)}
    for layer in range(DEPTH):
        p = 'l%d_' % layer
        inputs[p + 'w_ada'] = normal((D_MODEL, ADA_WIDTH), 0.1 * D_MODEL ** -0.5)
        inputs[p + 'b_ada'] = normal((ADA_WIDTH,), 0.01)
        inputs[p + 'ln_g'] = gain((N_SUBLAYERS, D_MODEL))
        inputs[p + 'ln_b'] = normal((N_SUBLAYERS, D_MODEL), 0.02)
        for f in ('ffn1_', 'ffn2_'):
            inputs[p + f + 'w_up'] = normal((D_MODEL, 2 * D_FF), D_MODEL ** -0.5)
            inputs[p + f + 'w_down'] = normal((D_FF, D_MODEL), DEEPNORM_BETA * D_FF ** -0.5)
        if layer % 2 == 0:
            inputs[p + 'w_in'] = normal((D_MODEL, AB_IN_WIDTH), D_MODEL ** -0.5)
            inputs[p + 'conv_qk'] = normal((B_CONV_WIDTH, 2 * B_QK_WIDTH), B_CONV_WIDTH ** -0.5)
            inputs[p + 'b_igate'] = normal((B_HEADS,), 0.1)
            inputs[p + 'b_fgate'] = 3.0 + 3.0 * jax.random.uniform(next(keys), (B_HEADS,), jnp.float32)
            inputs[p + 'mlstm_norm_g'] = gain((B_WIDTH,))
            inputs[p + 'w_out'] = normal((A_WIDTH + B_WIDTH, D_MODEL), DEEPNORM_BETA * (A_WIDTH + B_WIDTH) ** -0.5)
        else:
            inputs[p + 'w_in'] = normal((D_MODEL, CD_IN_WIDTH), D_MODEL ** -0.5)
            inputs[p + 'conv_dw'] = normal((D_CONV_WIDTH, D_CHANNELS), D_CONV_WIDTH ** -0.5)
            inputs[p + 'conv_ln_g'] = gain((D_CHANNELS,))
            inputs[p + 'conv_ln_b'] = normal((D_CHANNELS,), 0.02)
            inputs[p + 'w_out'] = normal((C_WIDTH + D_CHANNELS, D_MODEL), DEEPNORM_BETA * (C_WIDTH + D_CHANNELS) ** -0.5)
    return inputs


def reference(x, c,
              l0_w_ada, l0_b_ada, l0_ln_g, l0_ln_b, l0_ffn1_w_up, l0_ffn1_w_down, l0_ffn2_w_up, l0_ffn2_w_down,
              l0_w_in, l0_conv_qk, l0_b_igate, l0_b_fgate, l0_mlstm_norm_g, l0_w_out,
              l1_w_ada, l1_b_ada, l1_ln_g, l1_ln_b, l1_ffn1_w_up, l1_ffn1_w_down, l1_ffn2_w_up, l1_ffn2_w_down,
              l1_w_in, l1_conv_dw, l1_conv_ln_g, l1_conv_ln_b, l1_w_out):
    common = (
        (l0_w_ada, l0_b_ada, l0_ln_g, l0_ln_b, l0_ffn1_w_up, l0_ffn1_w_down, l0_ffn2_w_up, l0_ffn2_w_down),
        (l1_w_ada, l1_b_ada, l1_ln_g, l1_ln_b, l1_ffn1_w_up, l1_ffn1_w_down, l1_ffn2_w_up, l1_ffn2_w_down),
    )
    mixers = (
        lambda u: mixer_dilated_mlstm(u, l0_w_in, l0_conv_qk, l0_b_igate, l0_b_fgate, l0_mlstm_norm_g, l0_w_out),
        lambda u: mixer_moba_conv(u, l1_w_in, l1_conv_dw, l1_conv_ln_g, l1_conv_ln_b, l1_w_out),
    )
    batch = c.shape[0]
    for layer in range(DEPTH):
        w_ada, b_ada, ln_g, ln_b, f1_up, f1_down, f2_up, f2_down = common[layer]
        mod = (jax.nn.silu(c) @ w_ada + b_ada).reshape(batch, 3 * N_SUBLAYERS, 1, D_MODEL)
        sublayers = (
            (0.5, lambda u: swiglu_ffn(u, f1_up, f1_down)),
            (1.0, mixers[layer]),
            (0.5, lambda u: swiglu_ffn(u, f2_up, f2_down)),
        )
        for j, (res_weight, fn) in enumerate(sublayers):
            shift, scale, gate = mod[:, 3 * j], mod[:, 3 * j + 1], mod[:, 3 * j + 2]
            branch = fn(x * (1.0 + scale) + shift)
            x = layer_norm(DEEPNORM_ALPHA * x + res_weight * (1.0 + gate) * branch, ln_g[j], ln_b[j])
    return x
```

```python
import contextlib
import numpy as np
import concourse.bass as bass
import concourse.mybir as mybir
from concourse.bass_utils import run_bass_kernel_spmd

F32 = mybir.dt.float32
F32R = mybir.dt.float32r
AF = mybir.ActivationFunctionType
ALU = mybir.AluOpType
AX = mybir.AxisListType
ENGS = ('pe', 'act', 'dve', 'pool', 'sp')

D_MODEL = 4096
SEQ = 4096
BATCH = 2
DEPTH = 2
D_FF = 2 * D_MODEL
ALPHA = (2 * DEPTH) ** 0.25
LN_EPS = 1e-5
NCORES = 8


class Buf:
    __slots__ = ('t', 'w', 'r', 'dsem', 'dcnt', 'name')

    def __init__(self, t, name):
        self.t = t
        self.name = name
        self.w = {}
        self.r = {}
        self.dsem = None
        self.dcnt = 0

    def __getitem__(self, k):
        return self.t[k]


class Prog:
    def __init__(self, nc, es):
        self.nc = nc
        self.top = es
        self.es = es
        self.q = {e: [] for e in ENGS}
        self.sem = {e: es.enter_context(nc.semaphore('c_' + e)) for e in ENGS}
        self.cnt = {e: 0 for e in ENGS}
        self.seen = {}
        self.ndsem = 0
        self.dpool = []
        self.dall = []
        self.pbufs = []
        self.prefix = ''

    def sb(self, name, shape, dt=F32):
        b = Buf(self.es.enter_context(self.nc.sbuf_tensor('s_' + self.prefix + name, list(shape), dt)), name)
        self.pbufs.append(b)
        return b

    def ps(self, name, shape, dt=F32):
        b = Buf(self.es.enter_context(self.nc.psum_tensor('p_' + self.prefix + name, list(shape), dt)), name)
        self.pbufs.append(b)
        return b

    def begin_phase(self, prefix):
        self.prefix = prefix
        self.es = contextlib.ExitStack()
        self.pbufs = []

    def end_phase(self):
        toks = [(self.sem[o], self.cnt[o], 'bar') for o in ENGS if self.cnt[o] > 0]
        toks += [(r[0], r[1], 'bar') for r in self.dall if r[1] > 0]
        for e in ENGS:
            self._waits(e, toks)
        self.emit()
        self.q = {e: [] for e in ENGS}
        for b in self.pbufs:
            if b.dsem is not None:
                self.dpool.append(b.dsem)
                b.dsem = None
        self.pbufs = []
        self.es.close()
        self.es = self.top
        self.prefix = ''

    def dram(self, name, shape, dt=F32, kind="Internal"):
        return Buf(self.nc.dram_tensor(name, list(shape), dt, kind=kind).ap(), name)

    def _waits(self, eng, toks):
        for sem, val, src in toks:
            if src == 'pe' and eng == 'pe':
                continue
            key = (eng, id(sem))
            if self.seen.get(key, 0) >= val:
                continue
            self.seen[key] = val
            self.q[eng].append(lambda e, sem=sem, val=val: e.wait_ge(sem, val))

    @staticmethod
    def _upd(d, tok):
        k = id(tok[0])
        if k not in d or d[k][1] < tok[1]:
            d[k] = tok

    @staticmethod
    def _deps(reads, writes):
        toks = []
        for b in reads:
            toks += list(b.w.values())
        for b in writes:
            toks += list(b.w.values()) + list(b.r.values())
        return toks

    def _record(self, tok, reads, writes, partial):
        for b in reads:
            self._upd(b.r, tok)
        for b in writes:
            if partial:
                self._upd(b.w, tok)
            else:
                b.w = {id(tok[0]): tok}
                b.r = {}

    def op(self, eng, fn, reads=(), writes=(), partial=False):
        self._waits(eng, self._deps(reads, writes))
        self.cnt[eng] += 1
        n = self.cnt[eng]
        sem = self.sem[eng]
        self.q[eng].append(lambda e: fn(e).then_inc(sem, 1))
        self._record((sem, n, eng), reads, writes, partial)

    def dma(self, eng, out, in_, reads=(), writes=(), partial=False, owner=None):
        self._waits(eng, self._deps(reads, writes))
        b = owner if owner is not None else (list(writes) + list(reads))[0]
        if b.dsem is None:
            if self.dpool:
                b.dsem = self.dpool.pop()
            else:
                b.dsem = [self.top.enter_context(self.nc.semaphore('d%d' % self.ndsem)), 0]
                self.ndsem += 1
                self.dall.append(b.dsem)
        b.dsem[1] += 16
        sem, val = b.dsem[0], b.dsem[1]
        self.q[eng].append(lambda e: e.dma_start(out=out, in_=in_).then_inc(sem, 16))
        self._record((sem, val, 'dma'), reads, writes, partial)

    def finish(self, eng, bufs):
        toks = []
        for b in bufs:
            toks += list(b.w.values()) + list(b.r.values())
        self._waits(eng, toks)

    def emit(self):
        q = self.q
        with self.nc.Block() as block:
            @block.tensor
            def _(e):
                for f in q['pe']:
                    f(e)

            @block.scalar
            def _(e):
                for f in q['act']:
                    f(e)

            @block.vector
            def _(e):
                for f in q['dve']:
                    f(e)

            @block.gpsimd
            def _(e):
                for f in q['pool']:
                    f(e)

            @block.sync
            def _(e):
                for f in q['sp']:
                    f(e)


def new_nc():
    nc = bass.Bass("TRN2", target_bir_lowering=False)
    nc.dge_precook = False
    return nc


class Ring:
    def __init__(self, p, name, n, width):
        self.p = p
        self.slots = [p.sb('%s%d' % (name, i), [128, width], F32R) for i in range(n)]
        self.i = 0

    def load(self, src_ap):
        b = self.slots[self.i % len(self.slots)]
        self.i += 1
        self.p.dma('sp', b[:], src_ap, writes=[b])
        return b


def tile_w(W, kcu):
    K, N = W.shape
    H = K // (128 * kcu)
    t = W.reshape(H, kcu, 128, N // 128, 128).transpose(3, 0, 2, 1, 4)
    return np.ascontiguousarray(t).reshape(N // 128, H, 128, kcu * 128)


def fm_vec(v):
    return np.ascontiguousarray(v.reshape(-1, 128).T)


def emit_ffn(p, D, DFF, NTOK, T, xT, vecs, w_up, w_dn, oT, res_w=0.5, tiles=None):
    KC = D // 128
    FC = DFF // 128
    KCU = min(32, KC)
    HU = KC // KCU
    HD = FC // KCU
    if tiles is None:
        tiles = [(t0, min(T, NTOK - t0)) for t0 in range(0, NTOK, T)]
    xv = xT.rearrange("(c p) t -> p c t", p=128)
    xvr = xT.bitcast(F32R).rearrange("(c p) t -> p c t", p=128)
    ov = oT.rearrange("(c p) t -> p c t", p=128)
    eps2 = LN_EPS / (ALPHA * ALPHA)
    u = p.sb("u", [128, KC, T], F32R)
    a = p.sb("a", [128, FC, T], F32R)
    ring = Ring(p, "wr", 3, KCU * 128)
    vs = p.sb("vs", [128, 5, KC])
    sc1 = p.sb("sc1", [128, KC])
    gwa = p.sb("gwa", [128, KC])
    ones = p.sb("ones", [128, 128])
    sil = [p.sb("sil%d" % i, [128, T]) for i in range(2)]
    xc = [p.sb("xc%d" % i, [128, T]) for i in range(2)]
    sq = [p.sb("sq%d" % i, [128, T]) for i in range(2)]
    oc = [p.sb("oc%d" % i, [128, T]) for i in range(2)]
    t1 = [p.sb("t1%d" % i, [128, T]) for i in range(2)]
    mean = p.sb("mean", [128, T])
    msq = p.sb("msq", [128, T])
    rstd = p.sb("rstd", [128, T])
    pg = [p.ps("pg%d" % i, [128, T]) for i in range(2)]
    pv = [p.ps("pv%d" % i, [128, T]) for i in range(2)]
    py = [p.ps("py%d" % i, [128, T]) for i in range(2)]
    s1 = p.ps("s1", [128, T])
    s2 = p.ps("s2", [128, T])

    p.dma('pool', vs[:], vecs, writes=[vs])
    p.op('dve', lambda e: e.memset(ones[:], 1.0), writes=[ones])
    p.op('dve', lambda e: e.tensor_scalar_add(sc1[:], vs[:, 1, :], 1.0), reads=[vs], writes=[sc1])
    p.op('dve', lambda e: e.tensor_scalar(gwa[:], vs[:, 2, :], 1.0, res_w / ALPHA, ALU.add, ALU.mult),
         reads=[vs], writes=[gwa])

    for (t0, tt) in tiles:
        p.dma('pool', u[:, :, 0:tt], xvr[:, :, t0:t0 + tt], writes=[u])
        for c in range(KC):
            p.op('dve', lambda e, c=c, tt=tt: e.tensor_scalar(u[:, c, 0:tt], u[:, c, 0:tt].bitcast(F32), sc1[:, c:c + 1],
                                                              vs[:, 0, c:c + 1], ALU.mult, ALU.add),
                 reads=[u, sc1, vs], writes=[u], partial=True)
        for f in range(FC):
            g_ps, v_ps = pg[f % 2], pv[f % 2]
            for (n, ps) in ((f, g_ps), (FC + f, v_ps)):
                for h in range(HU):
                    wb = ring.load(w_up[n, h])
                    for kc in range(KCU):
                        k = h * KCU + kc
                        p.op('pe', lambda e, ps=ps, wb=wb, kc=kc, k=k, tt=tt: e.matmul(
                            ps[:, 0:tt], wb[:, kc * 128:(kc + 1) * 128], u[:, k, 0:tt], start=(k == 0), stop=(k == KC - 1)),
                            reads=[wb, u], writes=[ps], partial=(k > 0))
            sb_ = sil[f % 2]
            p.op('act', lambda e, sb_=sb_, g_ps=g_ps, tt=tt: e.activation(sb_[:, 0:tt], g_ps[:, 0:tt], AF.Silu),
                 reads=[g_ps], writes=[sb_])
            p.op('dve', lambda e, sb_=sb_, v_ps=v_ps, f=f, tt=tt: e.tensor_tensor(a[:, f, 0:tt], sb_[:, 0:tt], v_ps[:, 0:tt], ALU.mult),
                 reads=[sb_, v_ps], writes=[a], partial=True)
        for n in range(KC):
            ps = py[n % 2]
            for h in range(HD):
                wb = ring.load(w_dn[n, h])
                for kc in range(KCU):
                    k = h * KCU + kc
                    p.op('pe', lambda e, ps=ps, wb=wb, kc=kc, k=k, tt=tt: e.matmul(
                        ps[:, 0:tt], wb[:, kc * 128:(kc + 1) * 128], a[:, k, 0:tt], start=(k == 0), stop=(k == FC - 1)),
                        reads=[wb, a], writes=[ps], partial=(k > 0))
            xb = xc[n % 2]
            p.dma('pool', xb[:, 0:tt], xv[:, n, t0:t0 + tt], writes=[xb])
            p.op('dve', lambda e, ps=ps, xb=xb, n=n, tt=tt: e.scalar_tensor_tensor(
                u[:, n, 0:tt], ps[:, 0:tt], gwa[:, n:n + 1], xb[:, 0:tt], ALU.mult, ALU.add),
                reads=[ps, xb, gwa], writes=[u], partial=True)
            qb = sq[n % 2]
            p.op('act', lambda e, qb=qb, n=n, tt=tt: e.activation(qb[:, 0:tt], u[:, n, 0:tt].bitcast(F32), AF.Square),
                 reads=[u], writes=[qb])
            p.op('pe', lambda e, n=n, tt=tt: e.matmul(s1[:, 0:tt], ones[:], u[:, n, 0:tt].bitcast(F32),
                                                      start=(n == 0), stop=(n == KC - 1)),
                 reads=[ones, u], writes=[s1], partial=(n > 0))
            p.op('pe', lambda e, n=n, qb=qb, tt=tt: e.matmul(s2[:, 0:tt], ones[:], qb[:, 0:tt], start=(n == 0), stop=(n == KC - 1)),
                 reads=[ones, qb], writes=[s2], partial=(n > 0))
        p.op('dve', lambda e, tt=tt: e.tensor_scalar_mul(mean[:, 0:tt], s1[:, 0:tt], 1.0 / D), reads=[s1], writes=[mean])
        p.op('dve', lambda e, tt=tt: e.tensor_tensor(msq[:, 0:tt], mean[:, 0:tt], mean[:, 0:tt], ALU.mult), reads=[mean], writes=[msq])
        p.op('dve', lambda e, tt=tt: e.scalar_tensor_tensor(rstd[:, 0:tt], s2[:, 0:tt], 1.0 / D, msq[:, 0:tt], ALU.mult, ALU.subtract),
             reads=[s2, msq], writes=[rstd])
        p.op('dve', lambda e, tt=tt: e.tensor_scalar_add(rstd[:, 0:tt], rstd[:, 0:tt], eps2), reads=[rstd], writes=[rstd])
        p.op('act', lambda e, tt=tt: e.activation(rstd[:, 0:tt], rstd[:, 0:tt], AF.Sqrt), reads=[rstd], writes=[rstd])
        p.op('dve', lambda e, tt=tt: e.reciprocal(rstd[:, 0:tt], rstd[:, 0:tt]), reads=[rstd], writes=[rstd])
        for n in range(KC):
            tb, ob = t1[n % 2], oc[n % 2]
            p.op('dve', lambda e, tb=tb, n=n, tt=tt: e.tensor_tensor(tb[:, 0:tt], u[:, n, 0:tt].bitcast(F32), mean[:, 0:tt], ALU.subtract),
                 reads=[u, mean], writes=[tb])
            p.op('dve', lambda e, tb=tb, tt=tt: e.tensor_tensor(tb[:, 0:tt], tb[:, 0:tt], rstd[:, 0:tt], ALU.mult),
                 reads=[tb, rstd], writes=[tb])
            p.op('act', lambda e, tb=tb, ob=ob, n=n, tt=tt: e.activation(ob[:, 0:tt], tb[:, 0:tt], AF.Identity,
                                                                       bias=vs[:, 4, n:n + 1], scale=vs[:, 3, n:n + 1]),
                 reads=[tb, vs], writes=[ob])
            p.dma('pool', ov[:, n, t0:t0 + tt], ob[:, 0:tt], reads=[ob])
    return oc


def build_ffn(D, DFF, NTOK, T, res_w=0.5, tiles=None):
    KC = D // 128
    FC = DFF // 128
    KCU = min(32, KC)
    nc = new_nc()
    xT = nc.dram_tensor("xT", [D, NTOK], F32, kind="ExternalInput").ap()
    vecs = nc.dram_tensor("vecs", [128, 5, KC], F32, kind="ExternalInput").ap()
    w_up = nc.dram_tensor("w_up", [2 * FC, KC // KCU, 128, KCU * 128], F32R, kind="ExternalInput").ap()
    w_dn = nc.dram_tensor("w_dn", [KC, FC // KCU, 128, KCU * 128], F32R, kind="ExternalInput").ap()
    oT = nc.dram_tensor("oT", [D, NTOK], F32, kind="ExternalOutput").ap()
    with contextlib.ExitStack() as es:
        p = Prog(nc, es)
        oc = emit_ffn(p, D, DFF, NTOK, T, xT, vecs, w_up, w_dn, oT, res_w, tiles)
        p.finish('pool', oc)
        p.emit()
    return nc


def ffn_inputs(xT_core, shift, scale, gate, ln_g, ln_b, w_up_t, w_dn_t):
    vecs = np.ascontiguousarray(np.stack([fm_vec(v) for v in (shift, scale, gate, ln_g, ln_b)], axis=1))
    return {"xT": np.ascontiguousarray(xT_core), "vecs": vecs, "w_up": w_up_t, "w_dn": w_dn_t}


def emit_gemm(p, ring, w_ap, n, HU, KCU, src, ps, KC):
    for h in range(HU):
        wb = ring.load(w_ap[n, h])
        for kc in range(KCU):
            k = h * KCU + kc
            p.op('pe', lambda e, ps=ps, wb=wb, kc=kc, k=k: e.matmul(
                ps[:], wb[:, kc * 128:(kc + 1) * 128], src[:, k, :], start=(k == 0), stop=(k == KC - 1)),
                reads=[wb, src], writes=[ps], partial=(k > 0))


def emit_ln_stats_finalize(p, s1, s2, mean, msq, rstd, nfeat, eps):
    p.op('dve', lambda e: e.tensor_scalar_mul(mean[:], s1[:], 1.0 / nfeat), reads=[s1], writes=[mean])
    p.op('dve', lambda e: e.tensor_tensor(msq[:], mean[:], mean[:], ALU.mult), reads=[mean], writes=[msq])
    p.op('dve', lambda e: e.scalar_tensor_tensor(rstd[:], s2[:], 1.0 / nfeat, msq[:], ALU.mult, ALU.subtract),
         reads=[s2, msq], writes=[rstd])
    p.op('dve', lambda e: e.tensor_scalar_add(rstd[:], rstd[:], eps), reads=[rstd], writes=[rstd])
    p.op('act', lambda e: e.activation(rstd[:], rstd[:], AF.Sqrt), reads=[rstd], writes=[rstd])
    p.op('dve', lambda e: e.reciprocal(rstd[:], rstd[:]), reads=[rstd], writes=[rstd])


def emit_proj(p, D, NCH, NTOK, T, xT, vecs, w_in, oT):
    KC = D // 128
    KCU = min(32, KC)
    HU = KC // KCU
    xvr = xT.bitcast(F32R).rearrange("(c p) t -> p c t", p=128)
    ov = oT.rearrange("(c p) t -> p c t", p=128)
    u = p.sb("u", [128, KC, T], F32R)
    ring = Ring(p, "wr", 4, KCU * 128)
    vs = p.sb("vs", [128, 5, KC])
    sc1 = p.sb("sc1", [128, KC])
    oc = [p.sb("oc%d" % i, [128, T]) for i in range(4)]
    pps = [p.ps("pp%d" % i, [128, T]) for i in range(4)]
    p.dma('pool', vs[:], vecs, writes=[vs])
    p.op('dve', lambda e: e.tensor_scalar_add(sc1[:], vs[:, 1, :], 1.0), reads=[vs], writes=[sc1])
    for t0 in range(0, NTOK, T):
        p.dma('pool', u[:], xvr[:, :, t0:t0 + T], writes=[u])
        for c in range(KC):
            p.op('dve', lambda e, c=c: e.tensor_scalar(u[:, c, :], u[:, c, :].bitcast(F32), sc1[:, c:c + 1],
                                                       vs[:, 0, c:c + 1], ALU.mult, ALU.add),
                 reads=[u, sc1, vs], writes=[u], partial=True)
        for n in range(NCH):
            ps = pps[n % 4]
            ob = oc[n % 4]
            emit_gemm(p, ring, w_in, n, HU, KCU, u, ps, KC)
            if n % 2 == 0:
                p.op('act', lambda e, ob=ob, ps=ps: e.copy(ob[:], ps[:]), reads=[ps], writes=[ob])
            else:
                p.op('dve', lambda e, ob=ob, ps=ps: e.tensor_copy(ob[:], ps[:]), reads=[ps], writes=[ob])
            p.dma('pool', ov[:, n, t0:t0 + T], ob[:], reads=[ob])
    return oc


def build_proj(D, NCH, NTOK, T):
    KC = D // 128
    KCU = min(32, KC)
    nc = new_nc()
    xT = nc.dram_tensor("xT", [D, NTOK], F32, kind="ExternalInput").ap()
    vecs = nc.dram_tensor("vecs", [128, 5, KC], F32, kind="ExternalInput").ap()
    w_in = nc.dram_tensor("w_in", [NCH, KC // KCU, 128, KCU * 128], F32R, kind="ExternalInput").ap()
    oT = nc.dram_tensor("oT", [NCH * 128, NTOK], F32, kind="ExternalOutput").ap()
    with contextlib.ExitStack() as es:
        p = Prog(nc, es)
        oc = emit_proj(p, D, NCH, NTOK, T, xT, vecs, w_in, oT)
        p.finish('pool', oc)
        p.emit()
    return nc


def emit_out(p, D, NTOK, T, xT, cT, vecs, w_o, oT, res_w=1.0):
    KC = D // 128
    KCU = min(32, KC)
    HD = KC // KCU
    xv = xT.rearrange("(c p) t -> p c t", p=128)
    cv = cT.rearrange("(c p) t -> p c t", p=128)
    ov = oT.rearrange("(c p) t -> p c t", p=128)
    eps2 = LN_EPS / (ALPHA * ALPHA)
    a = p.sb("a", [128, KC, T], F32R)
    z = p.sb("z", [128, KC, T], F32)
    ring = Ring(p, "wr", 3, KCU * 128)
    vs = p.sb("vs", [128, 5, KC])
    gwa = p.sb("gwa", [128, KC])
    ones = p.sb("ones", [128, 128])
    xc = [p.sb("xc%d" % i, [128, T]) for i in range(2)]
    sq = [p.sb("sq%d" % i, [128, T]) for i in range(2)]
    oc = [p.sb("oc%d" % i, [128, T]) for i in range(2)]
    t1 = [p.sb("t1%d" % i, [128, T]) for i in range(2)]
    mean = p.sb("mean", [128, T])
    msq = p.sb("msq", [128, T])
    rstd = p.sb("rstd", [128, T])
    py = [p.ps("py%d" % i, [128, T]) for i in range(2)]
    s1 = p.ps("s1", [128, T])
    s2 = p.ps("s2", [128, T])
    p.dma('pool', vs[:], vecs, writes=[vs])
    p.op('dve', lambda e: e.memset(ones[:], 1.0), writes=[ones])
    p.op('dve', lambda e: e.tensor_scalar(gwa[:], vs[:, 2, :], 1.0, res_w / ALPHA, ALU.add, ALU.mult),
         reads=[vs], writes=[gwa])
    for t0 in range(0, NTOK, T):
        p.dma('pool', a[:], cv[:, :, t0:t0 + T], writes=[a])
        emit_down_ln(p, ring, w_o, KC, HD, KCU, KC, a, z, py, s1, s2, xc, sq, t1, oc, mean, msq, rstd,
                     xv, ov, t0, T, gwa, vs, ones, D, eps2)
    return oc


def build_out(D, NTOK, T, res_w=1.0):
    KC = D // 128
    KCU = min(32, KC)
    nc = new_nc()
    xT = nc.dram_tensor("xT", [D, NTOK], F32, kind="ExternalInput").ap()
    cT = nc.dram_tensor("cT", [D, NTOK], F32R, kind="ExternalInput").ap()
    vecs = nc.dram_tensor("vecs", [128, 5, KC], F32, kind="ExternalInput").ap()
    w_o = nc.dram_tensor("w_o", [KC, KC // KCU, 128, KCU * 128], F32R, kind="ExternalInput").ap()
    oT = nc.dram_tensor("oT", [D, NTOK], F32, kind="ExternalOutput").ap()
    with contextlib.ExitStack() as es:
        p = Prog(nc, es)
        oc = emit_out(p, D, NTOK, T, xT, cT, vecs, w_o, oT, res_w)
        p.finish('pool', oc)
        p.emit()
    return nc


def build_chain(D, DFF, NTOK, stages, out_T, ffn_T, ffn_tiles, proj_T=512):
    KC = D // 128
    FC = DFF // 128
    KCU = min(32, KC)
    nc = new_nc()
    xT = nc.dram_tensor("xT", [D, NTOK], F32, kind="ExternalInput").ap()
    xprod = [i for i, (k, _) in enumerate(stages) if k != 'proj']
    with contextlib.ExitStack() as es:
        p = Prog(nc, es)
        cur = xT
        for i, (kind, arg) in enumerate(stages):
            vecs = nc.dram_tensor("vecs%d" % i, [128, 5, KC], F32, kind="ExternalInput").ap()
            p.begin_phase("ph%d_" % i)
            if kind == 'proj':
                w_in = nc.dram_tensor("w_in%d" % i, [arg, KC // KCU, 128, KCU * 128], F32R, kind="ExternalInput").ap()
                pT = nc.dram_tensor("pT%d" % i, [arg * 128, NTOK], F32, kind="ExternalOutput").ap()
                oc = emit_proj(p, D, arg, NTOK, proj_T, cur, vecs, w_in, pT)
            else:
                ext = (i == xprod[-1]) or (i + 1 < len(stages) and stages[i + 1][0] == 'proj')
                dst = nc.dram_tensor("oT%d" % i, [D, NTOK], F32, kind="ExternalOutput" if ext else "Internal").ap()
                if kind == 'out':
                    cT = nc.dram_tensor("cT%d" % i, [D, NTOK], F32R, kind="ExternalInput").ap()
                    w_o = nc.dram_tensor("w_o%d" % i, [KC, KC // KCU, 128, KCU * 128], F32R, kind="ExternalInput").ap()
                    oc = emit_out(p, D, NTOK, out_T, cur, cT, vecs, w_o, dst, 1.0)
                else:
                    w_up = nc.dram_tensor("w_up%d" % i, [2 * FC, KC // KCU, 128, KCU * 128], F32R, kind="ExternalInput").ap()
                    w_dn = nc.dram_tensor("w_dn%d" % i, [KC, FC // KCU, 128, KCU * 128], F32R, kind="ExternalInput").ap()
                    oc = emit_ffn(p, D, DFF, NTOK, ffn_T, cur, vecs, w_up, w_dn, dst, 0.5, ffn_tiles)
                cur = dst
            p.finish('pool', oc)
            p.end_phase()
    return nc


def emit_down_ln(p, ring, w_dn, KC, HD, KCU, FC, a, z, py, s1, s2, xc, sq, t1, oc, mean, msq, rstd,
                 xv, ov, t0, T, gwa, vs, ones, D, eps2):
    for n in range(KC):
        ps = py[n % 2]
        emit_gemm(p, ring, w_dn, n, HD, KCU, a, ps, FC)
        xb = xc[n % 2]
        p.dma('pool', xb[:], xv[:, n, t0:t0 + T], writes=[xb])
        p.op('dve', lambda e, ps=ps, xb=xb, n=n: e.scalar_tensor_tensor(
            z[:, n, :], ps[:], gwa[:, n:n + 1], xb[:], ALU.mult, ALU.add),
            reads=[ps, xb, gwa], writes=[z], partial=True)
        qb = sq[n % 2]
        p.op('act', lambda e, qb=qb, n=n: e.activation(qb[:], z[:, n, :], AF.Square), reads=[z], writes=[qb])
        p.op('pe', lambda e, n=n: e.matmul(s1[:], ones[:], z[:, n, :], start=(n == 0), stop=(n == KC - 1)),
             reads=[ones, z], writes=[s1], partial=(n > 0))
        p.op('pe', lambda e, n=n, qb=qb: e.matmul(s2[:], ones[:], qb[:], start=(n == 0), stop=(n == KC - 1)),
             reads=[ones, qb], writes=[s2], partial=(n > 0))
    emit_ln_stats_finalize(p, s1, s2, mean, msq, rstd, D, eps2)
    for n in range(KC):
        tb, ob = t1[n % 2], oc[n % 2]
        p.op('dve', lambda e, tb=tb, n=n: e.tensor_tensor(tb[:], z[:, n, :], mean[:], ALU.subtract),
             reads=[z, mean], writes=[tb])
        p.op('dve', lambda e, tb=tb: e.tensor_tensor(tb[:], tb[:], rstd[:], ALU.mult),
             reads=[tb, rstd], writes=[tb])
        p.op('act', lambda e, tb=tb, ob=ob, n=n: e.activation(ob[:], tb[:], AF.Identity,
                                                            bias=vs[:, 4, n:n + 1], scale=vs[:, 3, n:n + 1]),
             reads=[tb, vs], writes=[ob])
        p.dma('pool', ov[:, n, t0:t0 + T], ob[:], reads=[ob])


def build_ada(D, NCOL, NL, NB):
    KC = D // 128
    CB = NCOL // 512
    nc = new_nc()
    cT = nc.dram_tensor("cT", [128, KC, NB], F32, kind="ExternalInput").ap()
    ws = [nc.dram_tensor("w%d" % l, [D, NCOL], F32R, kind="ExternalInput").ap() for l in range(NL)]
    bs = [nc.dram_tensor("b%d" % l, [NB, NCOL], F32, kind="ExternalInput").ap() for l in range(NL)]
    outs = [nc.dram_tensor("m%d" % l, [NB, NCOL], F32, kind="ExternalOutput").ap() for l in range(NL)]
    with contextlib.ExitStack() as es:
        p = Prog(nc, es)
        cs = p.sb("cs", [128, KC, NB])
        sc = p.sb("sc", [128, KC, NB], F32R)
        ring = Ring(p, "wr", 6, 8 * 512)
        bsb = [p.sb("bsb%d" % l, [NB, NCOL]) for l in range(NL)]
        osb = [p.sb("osb%d" % l, [NB, NCOL]) for l in range(NL)]
        pps = [p.ps("pp%d" % i, [NB, 512]) for i in range(2)]
        p.dma('pool', cs[:], cT, writes=[cs])
        p.op('act', lambda e: e.activation(sc[:], cs[:], AF.Silu), reads=[cs], writes=[sc])
        for l in range(NL):
            p.dma('pool', bsb[l][:], bs[l], writes=[bsb[l]])
            wv = ws[l].rearrange("(k p) n -> p k n", p=128)
            for cb in range(CB):
                ps = pps[cb % 2]
                for h in range(KC // 8):
                    b = ring.slots[ring.i % len(ring.slots)]
                    ring.i += 1
                    p.dma('sp', b[:].rearrange("p (k n) -> p k n", k=8), wv[:, h * 8:(h + 1) * 8, cb * 512:(cb + 1) * 512],
                          writes=[b])
                    for kc in range(8):
                        k = h * 8 + kc
                        p.op('pe', lambda e, ps=ps, b=b, kc=kc, k=k: e.matmul(
                            ps[:], sc[:, k, :], b[:, kc * 512:(kc + 1) * 512], start=(k == 0), stop=(k == KC - 1)),
                            reads=[b, sc], writes=[ps], partial=(k > 0))
                p.op('dve', lambda e, ps=ps, l=l, cb=cb: e.tensor_tensor(
                    osb[l][:, cb * 512:(cb + 1) * 512], ps[:], bsb[l][:, cb * 512:(cb + 1) * 512], ALU.add),
                    reads=[ps, bsb[l]], writes=[osb[l]], partial=True)
            p.dma('pool', outs[l], osb[l][:], reads=[osb[l]])
        p.finish('pool', osb)
        p.emit()
    return nc


NEG = -30000.0


def emit_dilattn(p, NH, S, qT_d, kT_d, v_d, tbl_d, out_d, banks, ones_r, scale, qk, scr):
    NT = S // 128
    NQB = S // 512
    LA = 2
    q = qk.t[:, 0, :]
    k = qk.t[:, 1, :]
    v = qk.t[:, 2, :].rearrange("p (t d) -> p t d", d=128)
    tbl = p.sb('atbl', [128, 23 * 128])
    sring = [banks[0], banks[1], banks[6], banks[7]]
    sc = scr['f'][0:2] + scr['f'][5:7]
    pt = scr['r'][0:4]
    ob, rd = scr['f'][2:4], scr['f'][4]
    for h in range(NH):
        p.dma('sp', q, qT_d[h], writes=[qk])
        p.dma('sp', k, kT_d[h], writes=[qk], partial=True)
        p.dma('sp', v, v_d[h], writes=[qk], partial=True)
        p.dma('sp', tbl[:], tbl_d[h], writes=[tbl])
        pairs = []
        for qb in range(NQB):
            kts = list(range(max(0, 4 * qb - 16), 4 * qb + 4))
            for i, kt in enumerate(kts):
                pairs.append((qb, kt, i == 0, i == len(kts) - 1))
        n = len(pairs)
        for it in range(n + LA):
            if it < n:
                qb, kt, first, last = pairs[it]
                s_ps = sring[it % 4]
                p.op('pe', lambda e, s_ps=s_ps, kt=kt, qb=qb: e.matmul(
                    s_ps[:], k[:, kt * 128:(kt + 1) * 128], q[:, qb * 512:(qb + 1) * 512], start=True, stop=True),
                    reads=[qk], writes=[s_ps])
            j = it - LA
            if j < 0:
                continue
            qb, kt, first, last = pairs[j]
            s_ps = sring[j % 4]
            sci, pti = sc[j % 4], pt[j % 4]
            o_ps = banks[2 + (qb % 2) * 2]
            d_ps = banks[3 + (qb % 2) * 2]
            off = (4 * qb - kt + 3) * 128
            p.op('dve', lambda e, s_ps=s_ps, sci=sci, off=off: e.scalar_tensor_tensor(
                sci[:], s_ps[:], scale, tbl[:, off:off + 512], ALU.mult, ALU.add),
                reads=[s_ps, tbl], writes=[sci])
            p.op('act', lambda e, sci=sci, pti=pti: e.activation(pti[:], sci[:], AF.Exp), reads=[sci], writes=[pti])
            p.op('pe', lambda e, o_ps=o_ps, kt=kt, pti=pti, first=first, last=last: e.matmul(
                o_ps[:], v[:, kt, :], pti[:], start=first, stop=last), reads=[qk, pti], writes=[o_ps], partial=not first)
            p.op('pe', lambda e, d_ps=d_ps, pti=pti, first=first, last=last: e.matmul(
                d_ps[:], ones_r[:], pti[:], start=first, stop=last), reads=[ones_r, pti], writes=[d_ps], partial=not first)
            if last:
                obi = ob[qb % 2]
                p.op('dve', lambda e, d_ps=d_ps: e.reciprocal(rd[:], d_ps[:]), reads=[d_ps], writes=[rd])
                p.op('dve', lambda e, o_ps=o_ps, obi=obi: e.tensor_tensor(obi[:], o_ps[:], rd[:], ALU.mult),
                     reads=[o_ps, rd], writes=[obi])
                p.dma('pool', out_d[h * 128:(h + 1) * 128, qb * 512:(qb + 1) * 512], obi[:], reads=[obi])
    return ob


def dil_table(slope):
    c = np.arange(23 * 128)[None, :]
    jl = np.arange(128)[:, None]
    dist = c - 384 - jl
    cnt = ((dist <= 128).astype(np.float64) + ((dist % 4 == 0) & (dist <= 512)) + ((dist % 16 == 0) & (dist <= 2048)))
    ok = (dist >= 0) & (dist <= 2048) & (cnt > 0)
    val = np.where(ok, -slope * dist + np.log(np.maximum(cnt, 1.0)), NEG)
    return val.astype(np.float32)


def emit_mlstm(p, S, qkpre_d, cw_d, bv_d, bo_d, gi_d, gf_d, gb_d, ng_d, mask_d, cst_d, out_d, banks, ones_r, ones_f, qk, scr):
    NT = S // 128
    NQB = S // 512
    DK = 256
    PIECE = 512
    pre = [p.sb('bpre%d' % i, [128, PIECE + 3]) for i in range(2)]
    acc = [p.sb('bacc%d' % i, [128, PIECE]) for i in range(2)]
    cw = p.sb('bcw', [128, 4, 4])
    gb = p.sb('bgb', [32, 2])
    ngb = p.sb('bngb', [32, 1])
    ng = p.sb('bng', [128, 4])
    mask = p.sb('bmask', [128, 4, 512])
    bcol = p.sb('bbcol', [128, NT])
    fb = [p.sb('bfb%d' % i, [128, 512]) for i in range(2)]
    dt_ = scr['f'][0:2]
    at = scr['r'][0:2]
    bvt = [p.sb('bbvt%d' % i, [128, 512], F32R) for i in range(3)]
    dt_ = dt_ + [scr['f'][10]]
    dt_ = dt_[0:2]
    bo = [p.sb('bbo', [128, 4, 512])] * 2
    hT = p.sb('bhT', [128, 4, 512])
    sqh = scr['f'][5:7]
    rden = scr['f'][4]
    mean = p.sb('bmean', [128, 512])
    msq = p.sb('bmsq', [128, 512])
    rstd = p.sb('brstd', [128, 512])
    t1 = scr['f'][7:9]
    sg = scr['f'][9:11]
    ob = scr['f'][2:4]

    p.dma('pool', cw[:], cw_d, writes=[cw])
    p.dma('pool', gb[:], gb_d, writes=[gb])
    p.dma('pool', ng[:], ng_d, writes=[ng])
    p.dma('pool', mask[:], mask_d, writes=[mask])
    NTT = S // 128
    gi = p.sb('bgi', [NTT, 128])
    ga = p.sb('bga', [NTT, 128])
    gc = p.sb('bgc', [NTT, 128])
    F2d = p.sb('bF2d', [NTT, 128])
    b2d = p.sb('bb2d', [NTT, 128])
    offs = p.sb('boffs', [NTT, 1])
    cst = p.sb('bcst', [NTT, 2 * NTT + NTT * 128])
    p.dma('pool', cst[:], cst_d, writes=[cst])
    p.dma('pool', gi[:], gi_d, writes=[gi])
    p.dma('pool', ga[:], gf_d, writes=[ga])
    p.op('dve', lambda e: e.tensor_scalar_mul(ngb[:], gb[:, 1:2], -1.0), reads=[gb], writes=[ngb])
    p.op('act', lambda e: e.activation(gc[:], ga[:], AF.Exp, bias=ngb[:, 0:1], scale=-1.0), reads=[ga, ngb], writes=[gc])
    p.op('dve', lambda e: e.tensor_scalar_add(gc[:], gc[:], 1.0), reads=[gc], writes=[gc])
    p.op('act', lambda e: e.activation(ga[:], gc[:], AF.Ln), reads=[gc], writes=[ga])
    p.op('dve', lambda e: e.tensor_scalar_mul(ga[:], ga[:], -1.0), reads=[ga], writes=[ga])
    cur, oth = ga, gc
    sh = 1
    while sh < 128:
        p.op('dve', lambda e, cur=cur, oth=oth, sh=sh: e.tensor_copy(oth[:, 0:sh], cur[:, 0:sh]), reads=[cur], writes=[oth])
        p.op('dve', lambda e, cur=cur, oth=oth, sh=sh: e.tensor_tensor(oth[:, sh:128], cur[:, sh:128], cur[:, 0:128 - sh], ALU.add),
             reads=[cur], writes=[oth], partial=True)
        cur, oth = oth, cur
        sh *= 2
    misc = banks[7]
    p.op('pe', lambda e, cur=cur: e.matmul(misc[0:NTT, 0:1], cst[:, 0:NTT], cur[:, 127:128], start=True, stop=True),
         reads=[cst, cur], writes=[misc])
    p.op('dve', lambda e: e.tensor_copy(offs[:], misc[0:NTT, 0:1]), reads=[misc], writes=[offs])
    p.op('dve', lambda e, cur=cur: e.tensor_scalar(F2d[:], cur[:], offs[:, 0:1], None, ALU.add), reads=[cur, offs], writes=[F2d])
    p.op('dve', lambda e: e.scalar_tensor_tensor(b2d[:], gi[:], gb[:, 0:1], F2d[:], ALU.add, ALU.subtract),
         reads=[gi, gb, F2d], writes=[b2d])
    p.op('pe', lambda e: e.matmul(misc[:, 0:NTT], b2d[:], cst[:, NTT:2 * NTT], start=True, stop=True),
         reads=[b2d, cst], writes=[misc])
    p.op('dve', lambda e: e.tensor_copy(bcol[:], misc[:, 0:NT]), reads=[misc], writes=[bcol])
    for c in range(4):
        for pc in range(S // PIECE):
            pr, ac = pre[(c * (S // PIECE) + pc) % 2], acc[(c * (S // PIECE) + pc) % 2]
            p.dma('sp', pr[:], qkpre_d[c, :, pc * PIECE:pc * PIECE + PIECE + 3], writes=[pr])
            p.op('dve', lambda e, pr=pr, ac=ac, c=c: e.tensor_scalar(ac[:], pr[:, 0:PIECE], cw[:, c, 0:1], None, ALU.mult),
                 reads=[pr, cw], writes=[ac])
            for j in range(1, 4):
                p.op('dve', lambda e, pr=pr, ac=ac, c=c, j=j: e.scalar_tensor_tensor(
                    ac[:], pr[:, j:j + PIECE], cw[:, c, j:j + 1], ac[:], ALU.mult, ALU.add), reads=[pr, cw, ac], writes=[ac])
            p.op('act', lambda e, ac=ac, c=c, pc=pc: e.activation(qk[:, c, pc * PIECE:(pc + 1) * PIECE], ac[:], AF.Silu),
                 reads=[ac], writes=[qk], partial=True)
    nld = 0
    for qb in range(NQB):
        fbi = fb[qb % 2]
        for i4 in range(4):
            tl = 4 * qb + i4
            p.op('pe', lambda e, tl=tl, i4=i4: e.matmul(misc[:, i4 * 128:(i4 + 1) * 128],
                                                         cst[:, 2 * NTT + tl * 128:2 * NTT + (tl + 1) * 128], F2d[:],
                                                         start=True, stop=True),
                 reads=[cst, F2d], writes=[misc], partial=(i4 > 0))
        p.op('act', lambda e, fbi=fbi: e.copy(fbi[:], misc[:]), reads=[misc], writes=[fbi])
        p.dma('pool', bo[qb % 2][:], bo_d[:, :, qb * 512:(qb + 1) * 512].rearrange("c p t -> p c t"), writes=[bo[qb % 2]])
        nkt = 4 * qb + 4
        bvs = {}
        for it in range(nkt + 1):
            if it < nkt:
                kt = it
                s_ps = banks[kt % 2]
                dti = dt_[kt % 2]
                bvb = bvt[nld % 3]
                nld += 1
                bvs[kt] = bvb
                p.dma('sp', bvb[:], bv_d[:, kt, :], writes=[bvb])
                for c in range(2):
                    p.op('pe', lambda e, s_ps=s_ps, c=c, kt=kt, qb=qb: e.matmul(
                        s_ps[:], qk[:, 2 + c, kt * 128:(kt + 1) * 128], qk[:, c, qb * 512:(qb + 1) * 512],
                        start=(c == 0), stop=(c == 1)), reads=[qk], writes=[s_ps], partial=(c > 0))
                p.op('act', lambda e, dti=dti, fbi=fbi, kt=kt: e.activation(dti[:], fbi[:], AF.Exp, bias=bcol[:, kt:kt + 1]),
                     reads=[fbi, bcol], writes=[dti])
                if kt >= 4 * qb:
                    p.op('dve', lambda e, dti=dti, kt=kt, qb=qb: e.tensor_tensor(dti[:], dti[:], mask[:, kt - 4 * qb, :], ALU.mult),
                         reads=[dti, mask], writes=[dti])
            kt = it - 1
            if kt < 0:
                continue
            s_ps = banks[kt % 2]
            dti, ati = dt_[kt % 2], at[kt % 2]
            bvb = bvs[kt]
            p.op('dve', lambda e, s_ps=s_ps, dti=dti, ati=ati: e.scalar_tensor_tensor(
                ati[:], s_ps[:], DK ** -0.5, dti[:], ALU.mult, ALU.mult), reads=[s_ps, dti], writes=[ati])
            first, last = (kt == 0), (kt == nkt - 1)
            for ec in range(4):
                p.op('pe', lambda e, ec=ec, bvb=bvb, ati=ati, first=first, last=last: e.matmul(
                    banks[2 + ec][:], bvb[:, ec * 128:(ec + 1) * 128], ati[:], start=first, stop=last),
                    reads=[bvb, ati], writes=[banks[2 + ec]], partial=not first)
            p.op('pe', lambda e, ati=ati, first=first, last=last: e.matmul(
                banks[6][:], ones_r[:], ati[:], start=first, stop=last), reads=[ones_r, ati], writes=[banks[6]], partial=not first)
        p.op('act', lambda e: e.activation(rden[:], banks[6][:], AF.Abs), reads=[banks[6]], writes=[rden])
        p.op('dve', lambda e: e.tensor_scalar_max(rden[:], rden[:], 1.0), reads=[rden], writes=[rden])
        p.op('dve', lambda e: e.reciprocal(rden[:], rden[:]), reads=[rden], writes=[rden])
        for ec in range(4):
            p.op('dve', lambda e, ec=ec: e.tensor_tensor(hT[:, ec, :], banks[2 + ec][:], rden[:], ALU.mult),
                 reads=[banks[2 + ec], rden], writes=[hT], partial=True)
            sqi = sqh[ec % 2]
            p.op('act', lambda e, ec=ec, sqi=sqi: e.activation(sqi[:], hT[:, ec, :], AF.Square), reads=[hT], writes=[sqi])
            p.op('pe', lambda e, ec=ec: e.matmul(banks[0][:], ones_f[:], hT[:, ec, :], start=(ec == 0), stop=(ec == 3)),
                 reads=[ones_f, hT], writes=[banks[0]], partial=(ec > 0))
            p.op('pe', lambda e, ec=ec, sqi=sqi: e.matmul(banks[1][:], ones_f[:], sqi[:], start=(ec == 0), stop=(ec == 3)),
                 reads=[ones_f, sqi], writes=[banks[1]], partial=(ec > 0))
        emit_ln_stats_finalize(p, banks[0], banks[1], mean, msq, rstd, 512, LN_EPS)
        for ec in range(4):
            tb, sgi, obi = t1[ec % 2], sg[ec % 2], ob[ec % 2]
            p.op('dve', lambda e, tb=tb, ec=ec: e.tensor_tensor(tb[:], hT[:, ec, :], mean[:], ALU.subtract),
                 reads=[hT, mean], writes=[tb])
            p.op('dve', lambda e, tb=tb: e.tensor_tensor(tb[:], tb[:], rstd[:], ALU.mult), reads=[tb, rstd], writes=[tb])
            p.op('act', lambda e, sgi=sgi, ec=ec, qb=qb: e.activation(sgi[:], bo[qb % 2][:, ec, :], AF.Sigmoid),
                 reads=[bo[qb % 2]], writes=[sgi])
            p.op('dve', lambda e, tb=tb, sgi=sgi, obi=obi, ec=ec: e.scalar_tensor_tensor(
                obi[:], tb[:], ng[:, ec:ec + 1], sgi[:], ALU.mult, ALU.mult), reads=[tb, ng, sgi], writes=[obi])
            p.dma('pool', out_d[ec * 128:(ec + 1) * 128, qb * 512:(qb + 1) * 512], obi[:], reads=[obi])
    return ob


def build_mix0(S, NH, with_a=True, with_b=True):
    nc = new_nc()
    NT = S // 128
    qT_d = nc.dram_tensor("aq", [NH, 128, S], F32R, kind="ExternalInput").ap()
    kT_d = nc.dram_tensor("ak", [NH, 128, S], F32R, kind="ExternalInput").ap()
    v_d = nc.dram_tensor("av", [NH, 128, NT, 128], F32R, kind="ExternalInput").ap()
    tbl_d = nc.dram_tensor("atbl", [NH, 128, 23 * 128], F32, kind="ExternalInput").ap()
    ao_d = nc.dram_tensor("ao", [NH * 128, S], F32, kind="ExternalOutput").ap()
    qkpre_d = nc.dram_tensor("bqk", [4, 128, S + 3], F32, kind="ExternalInput").ap()
    cw_d = nc.dram_tensor("bcw", [128, 4, 4], F32, kind="ExternalInput").ap()
    bv_d = nc.dram_tensor("bv", [128, NT, 512], F32R, kind="ExternalInput").ap()
    bo_d = nc.dram_tensor("bo", [4, 128, S], F32, kind="ExternalInput").ap()
    gi_d = nc.dram_tensor("bgi", [NT, 128], F32, kind="ExternalInput").ap()
    gf_d = nc.dram_tensor("bgf", [NT, 128], F32, kind="ExternalInput").ap()
    gb_d = nc.dram_tensor("bgb", [NT, 2], F32, kind="ExternalInput").ap()
    cst_d = nc.dram_tensor("bcst", [NT, 2 * NT + NT * 128], F32, kind="ExternalInput").ap()
    ng_d = nc.dram_tensor("bng", [128, 4], F32, kind="ExternalInput").ap()
    mask_d = nc.dram_tensor("bmask", [128, 4, 512], F32, kind="ExternalInput").ap()
    bout_d = nc.dram_tensor("bout", [512, S], F32, kind="ExternalOutput").ap()
    with contextlib.ExitStack() as es:
        p = Prog(nc, es)
        banks = [p.ps("bank%d" % i, [128, 512]) for i in range(8)]
        ones_r = p.sb("ones_r", [128, 128], F32R)
        ones_f = p.sb("ones_f", [128, 128], F32)
        p.op('dve', lambda e: e.memset(ones_f[:], 1.0), writes=[ones_f])
        p.op('dve', lambda e: e.tensor_copy(ones_r[:], ones_f[:]), reads=[ones_f], writes=[ones_r])
        outs = []
        qk = p.sb('qk', [128, 4, S], F32R)
        scr = {'f': [p.sb('scrf%d' % i, [128, 512]) for i in range(11)],
               'r': [p.sb('scrr%d' % i, [128, 512], F32R) for i in range(4)]}
        if with_a:
            outs += emit_dilattn(p, NH, S, qT_d, kT_d, v_d, tbl_d, ao_d, banks, ones_r, 128 ** -0.5, qk, scr)
        if with_b:
            outs += emit_mlstm(p, S, qkpre_d, cw_d, bv_d, bo_d, gi_d, gf_d, gb_d, ng_d, mask_d, cst_d, bout_d, banks, ones_r, ones_f, qk, scr)
        p.finish('pool', outs)
        p.emit()
    return nc


def causal_mask_4x512():
    s = np.arange(128)[:, None, None]
    i = np.arange(4)[None, :, None]
    t = np.arange(512)[None, None, :]
    return (i * 128 + s <= t).astype(np.float32)


def mlstm_consts(NT):
    U = (np.arange(NT)[:, None] < np.arange(NT)[None, :]).astype(np.float32)
    I = np.eye(NT, dtype=np.float32)
    Sel = np.zeros((NT, NT, 128), np.float32)
    Sel[np.arange(NT), np.arange(NT), :] = 1.0
    return np.concatenate([U, I, Sel.reshape(NT, NT * 128)], axis=1)


MOBA_TW = 35 * 128


def moba_table(slope):
    c = np.arange(MOBA_TW)[None, :]
    jl = np.arange(128)[:, None]
    dist = c - 384 - jl
    return np.where(dist >= 0, -slope * dist, NEG).astype(np.float32)


def moba_consts():
    blk = np.arange(16)[:, None]
    n = np.arange(16)[None, :]
    past = (n < blk)
    pm = np.where(past, 0.0, NEG).reshape(-1)
    p01 = past.astype(np.float64).reshape(-1)
    own = (n == blk).astype(np.float64).reshape(-1)
    row = np.concatenate([pm, p01, own]).astype(np.float32)
    return np.ascontiguousarray(np.concatenate([np.tile(row[None, :], (128, 1)), np.eye(128, dtype=np.float32)], axis=1))


def moba_esel():
    E = np.zeros((16, 16, 128), np.float32)
    E[np.arange(16), np.arange(16), :] = 1.0
    return E.reshape(16, 16 * 128)


def emit_moba(p, NH, S, qT_d, kT_d, v_d, tbl_d, cst_d, esel_d, out_d, banks, ones_r, scale, qk, scr):
    NT = S // 128
    NQB = S // 512
    NB = S // 256
    q = qk.t[:, 0, :]
    k = qk.t[:, 1, :]
    v = qk.t[:, 2, :].rearrange("p (t d) -> p t d", d=128)
    tbl = p.sb('ctbl', [128, MOBA_TW])
    cst = p.sb('ccst', [128, 3 * 256 + 128])
    esel = p.sb('cesel', [16, 16 * 128], F32R)
    selT = p.sb('cselT', [16, S], F32R)
    kmean = p.sb('ckmean', [128, NB], F32R)
    ksum = p.sb('cksum', [128, NB])
    gm = [p.sb('cgm%d' % i, [128, 16]) for i in range(4)]
    top8 = [p.sb('ctop%d' % i, [128, 8]) for i in range(4)]
    sel = [p.sb('csel%d' % i, [128, 16]) for i in range(4)]
    gps = [banks[2], banks[3], banks[4], banks[5]]
    tps = [banks[6], banks[7]]
    LA = 2
    sc, pt, ob, rd, ex = scr['f'][0:2] + scr['f'][9:10], scr['r'][0:3], scr['f'][2:4], scr['f'][4], scr['f'][5:8]
    sring = [banks[0], banks[1], banks[6]]
    ident = cst.t[:, 768:896]
    p.dma('pool', cst[:], cst_d, writes=[cst])
    p.dma('pool', esel[:], esel_d, writes=[esel])
    misc = banks[7]
    for h in range(NH):
        p.dma('sp', q, qT_d[h], writes=[qk])
        p.dma('sp', k, kT_d[h], writes=[qk], partial=True)
        p.dma('sp', v, v_d[h], writes=[qk], partial=True)
        p.dma('sp', tbl[:], tbl_d[h], writes=[tbl])
        p.op('dve', lambda e: e.tensor_reduce(ksum[:], k.bitcast(F32).rearrange("p (n j) -> p n j", j=256), AX.X, ALU.add),
             reads=[qk], writes=[ksum])
        p.op('dve', lambda e: e.tensor_scalar_mul(kmean[:], ksum[:], 1.0 / 256), reads=[ksum], writes=[kmean])
        for it in range(NT + 2):
            if it < NT:
                qt = it
                gp = gps[qt % 4]
                p.op('pe', lambda e, qt=qt, gp=gp: e.matmul(gp[:, 0:16], q[:, qt * 128:(qt + 1) * 128], kmean[:], start=True, stop=True),
                     reads=[qk, kmean], writes=[gp])
            qt = it - 1
            if 0 <= qt < NT:
                blk = qt // 2
                gp = gps[qt % 4]
                g, t8, sl = gm[qt % 4], top8[qt % 4], sel[qt % 4]
                if blk < 3:
                    p.op('dve', lambda e, sl=sl, blk=blk: e.tensor_tensor(
                        sl[:], cst[:, 256 + blk * 16:256 + (blk + 1) * 16], cst[:, 512 + blk * 16:512 + (blk + 1) * 16], ALU.add),
                        reads=[cst, gp], writes=[sl])
                else:
                    p.op('dve', lambda e, g=g, gp=gp, blk=blk: e.tensor_tensor(g[:], gp[:, 0:16], cst[:, blk * 16:(blk + 1) * 16], ALU.add),
                         reads=[gp, cst], writes=[g])
                    p.op('dve', lambda e, g=g, t8=t8: e.max(t8[:], g[:]), reads=[g], writes=[t8])
                    p.op('dve', lambda e, g=g, t8=t8, sl=sl: e.tensor_scalar(sl[:], g[:], t8[:, 2:3], None, ALU.is_ge),
                         reads=[g, t8], writes=[sl])
                    p.op('dve', lambda e, sl=sl, blk=blk: e.tensor_tensor(
                        sl[:], sl[:], cst[:, 512 + blk * 16:512 + (blk + 1) * 16], ALU.add), reads=[sl, cst], writes=[sl])
            qt = it - 2
            if 0 <= qt < NT:
                sl = sel[qt % 4]
                tp = tps[qt % 2]
                p.op('pe', lambda e, sl=sl, tp=tp: e.matmul(tp[0:16, 0:128], sl[:], ident, start=True, stop=True),
                     reads=[sl, cst], writes=[tp])
                p.op('dve', lambda e, qt=qt, tp=tp: e.tensor_scalar(selT[:, qt * 128:(qt + 1) * 128], tp[0:16, 0:128], 1.0, -NEG / scale,
                                                                   ALU.subtract, ALU.mult),
                     reads=[tp], writes=[selT], partial=True)
        pairs = []
        for qb in range(NQB):
            nkt = 4 * qb + 4
            for kt in range(nkt):
                pairs.append((qb, kt, kt == 0, kt == nkt - 1))
        n = len(pairs)
        nblk = 0
        mbank = {}
        for it in range(n + 2):
            if it < n:
                qb, kt, first, last = pairs[it]
                s_ps = sring[it % 3]
                p.op('pe', lambda e, s_ps=s_ps, kt=kt, qb=qb: e.matmul(
                    s_ps[:], k[:, kt * 128:(kt + 1) * 128], q[:, qb * 512:(qb + 1) * 512], start=True, stop=False),
                    reads=[qk], writes=[s_ps])
                p.op('pe', lambda e, s_ps=s_ps, nb=kt // 2, qb=qb: e.matmul(
                    s_ps[:], esel[:, nb * 128:(nb + 1) * 128], selT[:, qb * 512:(qb + 1) * 512], start=False, stop=True),
                    reads=[esel, selT], writes=[s_ps], partial=True)
            j = it - 2
            if 0 <= j < n:
                qb, kt, first, last = pairs[j]
                s_ps = sring[j % 3]
                sci, pti = sc[j % 3], pt[j % 3]
                o_ps = banks[2 + (qb % 2) * 2]
                d_ps = banks[3 + (qb % 2) * 2]
                off = (4 * qb - kt + 3) * 128
                p.op('dve', lambda e, s_ps=s_ps, sci=sci, off=off: e.scalar_tensor_tensor(
                    sci[:], s_ps[:], scale, tbl[:, off:off + 512], ALU.mult, ALU.add), reads=[s_ps, tbl], writes=[sci])
                p.op('act', lambda e, sci=sci, pti=pti: e.activation(pti[:], sci[:], AF.Exp), reads=[sci], writes=[pti])
                p.op('pe', lambda e, o_ps=o_ps, kt=kt, pti=pti, first=first, last=last: e.matmul(
                    o_ps[:], v[:, kt, :], pti[:], start=first, stop=last), reads=[qk, pti], writes=[o_ps], partial=not first)
                p.op('pe', lambda e, d_ps=d_ps, pti=pti, first=first, last=last: e.matmul(
                    d_ps[:], ones_r[:], pti[:], start=first, stop=last), reads=[ones_r, pti], writes=[d_ps], partial=not first)
                if last:
                    obi = ob[qb % 2]
                    p.op('dve', lambda e, d_ps=d_ps: e.reciprocal(rd[:], d_ps[:]), reads=[d_ps], writes=[rd])
                    p.op('dve', lambda e, o_ps=o_ps, obi=obi: e.tensor_tensor(obi[:], o_ps[:], rd[:], ALU.mult),
                         reads=[o_ps, rd], writes=[obi])
                    p.dma('pool', out_d[h * 128:(h + 1) * 128, qb * 512:(qb + 1) * 512], obi[:], reads=[obi])
    return ob


def emit_conv(p, NTOK, NCH, da_d, db_d, cw_d, lv_d, idn_d, out_d, banks, ones_f, scr):
    W = 31
    HALO = W - 1
    TT = 512
    z = p.sb('dz', [128, NCH, TT])
    da = [p.sb('dda%d' % i, [128, TT + HALO]) for i in range(2)]
    db = [p.sb('ddb%d' % i, [128, TT + HALO]) for i in range(2)]
    gl = [p.sb('dgl%d' % i, [128, TT + HALO], F32R) for i in range(2)]
    dg = [p.sb('ddg%d' % i, [128, W, 128], F32R) for i in range(2)]
    idn = p.sb('didn', [128, 128])
    cw = p.sb('dcw', [128, NCH, W])
    lv = p.sb('dlv', [128, 2, NCH])
    mean = p.sb('dmean', [128, 512])
    rstd = p.sb('drstd', [128, 512])
    msq = scr['f'][4]
    sq, t1, ob = scr['f'][5:7], scr['f'][7:9], scr['f'][2:4]
    zring = [banks[2], banks[3]]
    p.dma('pool', cw[:], cw_d, writes=[cw])
    p.dma('pool', lv[:], lv_d, writes=[lv])
    p.dma('pool', idn[:], idn_d, writes=[idn])
    it = 0
    for hf in range(NTOK // TT):
        s1, s2 = banks[0], banks[1]
        for c in range(NCH):
            a_, b_, g_, d_ = da[it % 2], db[it % 2], gl[it % 2], dg[it % 2]
            z_ps = zring[it % 2]
            it += 1
            p.dma('sp', a_[:], da_d[c, :, hf * TT:hf * TT + TT + HALO], writes=[a_])
            p.dma('sp', b_[:], db_d[c, :, hf * TT:hf * TT + TT + HALO], writes=[b_])
            for j in range(W):
                if j % 3 == 2:
                    p.op('act', lambda e, d_=d_, c=c, j=j: e.activation(d_[:, j, :], idn[:], AF.Identity, scale=cw[:, c, j:j + 1]),
                         reads=[idn, cw], writes=[d_], partial=(j > 0))
                else:
                    p.op('dve', lambda e, d_=d_, c=c, j=j: e.tensor_scalar(d_[:, j, :], idn[:], cw[:, c, j:j + 1], None, ALU.mult),
                         reads=[idn, cw], writes=[d_], partial=(j > 0))
            p.op('act', lambda e, b_=b_: e.activation(b_[:], b_[:], AF.Sigmoid), reads=[b_], writes=[b_])
            p.op('dve', lambda e, a_=a_, b_=b_, g_=g_: e.tensor_tensor(g_[:], a_[:], b_[:], ALU.mult), reads=[a_, b_], writes=[g_])
            for j in range(W):
                p.op('pe', lambda e, z_ps=z_ps, d_=d_, g_=g_, j=j: e.matmul(z_ps[:], d_[:, j, :], g_[:, j:j + TT],
                                                                            start=(j == 0), stop=(j == W - 1)),
                     reads=[d_, g_], writes=[z_ps], partial=(j > 0))
            sqi = sq[c % 2]
            p.op('act', lambda e, z_ps=z_ps, c=c: e.copy(z[:, c, :], z_ps[:]), reads=[z_ps], writes=[z], partial=True)
            p.op('act', lambda e, sqi=sqi, z_ps=z_ps: e.activation(sqi[:], z_ps[:], AF.Square), reads=[z_ps], writes=[sqi])
            p.op('pe', lambda e, c=c: e.matmul(s1[:], ones_f[:], z[:, c, :], start=(c == 0), stop=(c == NCH - 1)),
                 reads=[ones_f, z], writes=[s1], partial=(c > 0))
            p.op('pe', lambda e, c=c, sqi=sqi: e.matmul(s2[:], ones_f[:], sqi[:], start=(c == 0), stop=(c == NCH - 1)),
                 reads=[ones_f, sqi], writes=[s2], partial=(c > 0))
        emit_ln_stats_finalize(p, s1, s2, mean, msq, rstd, NCH * 128, LN_EPS)
        for c in range(NCH):
            tb, obi = t1[c % 2], ob[c % 2]
            p.op('dve', lambda e, tb=tb, c=c: e.tensor_tensor(tb[:], z[:, c, :], mean[:], ALU.subtract),
                 reads=[z, mean], writes=[tb])
            p.op('dve', lambda e, tb=tb: e.tensor_tensor(tb[:], tb[:], rstd[:], ALU.mult), reads=[tb, rstd], writes=[tb])
            p.op('act', lambda e, tb=tb, obi=obi, c=c: e.activation(obi[:], tb[:], AF.Silu, bias=lv[:, 1, c:c + 1], scale=lv[:, 0, c:c + 1]),
                 reads=[tb, lv], writes=[obi])
            p.dma('pool', out_d[c * 128:(c + 1) * 128, hf * TT:(hf + 1) * TT], obi[:], reads=[obi])
    return ob


def build_mix1(S, NH, NTOK, NCH, with_c=True, with_d=True):
    nc = new_nc()
    NT = S // 128
    qT_d = nc.dram_tensor("cq", [NH, 128, S], F32R, kind="ExternalInput").ap()
    kT_d = nc.dram_tensor("ck", [NH, 128, S], F32R, kind="ExternalInput").ap()
    v_d = nc.dram_tensor("cv", [NH, 128, NT, 128], F32R, kind="ExternalInput").ap()
    tbl_d = nc.dram_tensor("ctbl", [NH, 128, MOBA_TW], F32, kind="ExternalInput").ap()
    cst_d = nc.dram_tensor("ccst", [128, 3 * 256 + 128], F32, kind="ExternalInput").ap()
    esel_d = nc.dram_tensor("cesel", [16, 16 * 128], F32R, kind="ExternalInput").ap()
    co_d = nc.dram_tensor("co", [NH * 128, S], F32, kind="ExternalOutput").ap()
    da_d = nc.dram_tensor("dda", [NCH, 128, NTOK + 30], F32, kind="ExternalInput").ap()
    db_d = nc.dram_tensor("ddb", [NCH, 128, NTOK + 30], F32, kind="ExternalInput").ap()
    cw_d = nc.dram_tensor("dcw", [128, NCH, 31], F32, kind="ExternalInput").ap()
    lv_d = nc.dram_tensor("dlv", [128, 2, NCH], F32, kind="ExternalInput").ap()
    do_d = nc.dram_tensor("dout", [NCH * 128, NTOK], F32, kind="ExternalOutput").ap()
    with contextlib.ExitStack() as es:
        p = Prog(nc, es)
        banks = [p.ps("bank%d" % i, [128, 512]) for i in range(8)]
        ones_r = p.sb("ones_r", [128, 128], F32R)
        ones_f = p.sb("ones_f", [128, 128], F32)
        p.op('dve', lambda e: e.memset(ones_f[:], 1.0), writes=[ones_f])
        p.op('dve', lambda e: e.tensor_copy(ones_r[:], ones_f[:]), reads=[ones_f], writes=[ones_r])
        qk = p.sb('qk', [128, 3, S], F32R)
        scr = {'f': [p.sb('scrf%d' % i, [128, 512]) for i in range(11)],
               'r': [p.sb('scrr%d' % i, [128, 512], F32R) for i in range(4)]}
        outs = []
        if with_d:
            outs += emit_conv(p, NTOK, NCH, da_d, db_d, cw_d, lv_d, cst_d[:, 768:896], do_d, banks, ones_f, scr)
        if with_c:
            outs += emit_moba(p, NH, S, qT_d, kT_d, v_d, tbl_d, cst_d, esel_d, co_d, banks, ones_r, 128 ** -0.5, qk, scr)
        p.finish('pool', outs)
        p.emit()
    return nc


TOK_PER_CORE = BATCH * SEQ // NCORES
CORES_PER_BATCH = NCORES // BATCH
FFN_T = 352
FFN_TILES = [(0, 352), (352, 352), (704, 320)]
PROJ_T = 512
OUT_T = 512


def _launch(nc, in_maps):
    res = run_bass_kernel_spmd(nc, in_maps, core_ids=list(range(NCORES)))
    return res.results


def _vecs(mod_l, b, j, ln_g, ln_b):
    return np.ascontiguousarray(np.stack([fm_vec(np.ascontiguousarray(v)) for v in
                                          (mod_l[b, 3 * j], mod_l[b, 3 * j + 1], mod_l[b, 3 * j + 2], ln_g[j], ln_b[j])], axis=1))


def _alibi(n):
    return 2.0 ** (-8.0 * np.arange(1, n + 1) / n)


def _run_ffn(xTs, mod_l, j, ln_g, ln_b, w_up, w_dn):
    wu = tile_w(np.asarray(w_up), 32)
    wd = tile_w(np.asarray(w_dn), 32)
    nc = build_ffn(D_MODEL, D_FF, TOK_PER_CORE, FFN_T, res_w=0.5, tiles=FFN_TILES)
    maps = [{"xT": xTs[c], "vecs": _vecs(mod_l, c // CORES_PER_BATCH, j, ln_g, ln_b), "w_up": wu, "w_dn": wd}
            for c in range(NCORES)]
    out = _launch(nc, maps)
    return [np.ascontiguousarray(o["oT"]) for o in out]


def _run_proj(xTs, mod_l, ln_g, ln_b, w_in):
    w_in = np.asarray(w_in)
    ncol = w_in.shape[1]
    nch = -(-ncol // 128)
    if nch * 128 != ncol:
        w_in = np.concatenate([w_in, np.zeros((w_in.shape[0], nch * 128 - ncol), np.float32)], axis=1)
    wt = tile_w(w_in, 32)
    nc = build_proj(D_MODEL, nch, TOK_PER_CORE, PROJ_T)
    maps = [{"xT": xTs[c], "vecs": _vecs(mod_l, c // CORES_PER_BATCH, 1, ln_g, ln_b), "w_in": wt} for c in range(NCORES)]
    out = _launch(nc, maps)
    return [np.concatenate([out[b * CORES_PER_BATCH + r]["oT"] for r in range(CORES_PER_BATCH)], axis=1) for b in range(BATCH)]


def _run_out(xTs, catTs, mod_l, ln_g, ln_b, w_out):
    wo = tile_w(np.asarray(w_out), 32)
    nc = build_out(D_MODEL, TOK_PER_CORE, OUT_T, res_w=1.0)
    maps = []
    for c in range(NCORES):
        b, r = divmod(c, CORES_PER_BATCH)
        maps.append({"xT": xTs[c], "cT": np.ascontiguousarray(catTs[b][:, r * TOK_PER_CORE:(r + 1) * TOK_PER_CORE]),
                     "vecs": _vecs(mod_l, b, 1, ln_g, ln_b), "w_o": wo})
    out = _launch(nc, maps)
    return [np.ascontiguousarray(o["oT"]) for o in out]


def _heads_T(pT, row0, nh):
    return np.ascontiguousarray(pT[row0:row0 + nh * 128].reshape(nh, 128, SEQ))


def _heads_V(pT, row0, nh):
    vt = pT[row0:row0 + nh * 128].reshape(nh, 128, SEQ // 128, 128)
    return np.ascontiguousarray(vt.transpose(0, 3, 2, 1))


def _run_mix0(projT, conv_qk, b_igate, b_fgate, norm_g):
    NH = 4
    slopes = _alibi(16)
    conv_qk = np.asarray(conv_qk)
    norm_g = np.asarray(norm_g)
    nc = build_mix0(SEQ, NH)
    mask = causal_mask_4x512()
    cst = mlstm_consts(SEQ // 128)
    maps = []
    for c in range(NCORES):
        b, r = divmod(c, CORES_PER_BATCH)
        pT = projT[b]
        m = {}
        m["aq"] = _heads_T(pT, 4 * r * 128, NH)
        m["ak"] = _heads_T(pT, 2048 + 4 * r * 128, NH)
        m["av"] = _heads_V(pT, 4096 + 4 * r * 128, NH)
        m["atbl"] = np.stack([dil_table(slopes[4 * r + i]) for i in range(NH)])
        pre = np.concatenate([pT[6144 + r * 256:6144 + (r + 1) * 256], pT[7168 + r * 256:7168 + (r + 1) * 256]], axis=0)
        pre = np.concatenate([np.zeros((512, 3), np.float32), pre], axis=1)
        m["bqk"] = np.ascontiguousarray(pre.reshape(4, 128, SEQ + 3))
        cw = np.concatenate([conv_qk[:, r * 256:(r + 1) * 256], conv_qk[:, 1024 + r * 256:1024 + (r + 1) * 256]], axis=1)
        m["bcw"] = np.ascontiguousarray(cw.T.reshape(4, 128, 4).transpose(1, 0, 2))
        vT = pT[8192 + r * 512:8192 + (r + 1) * 512]
        m["bv"] = np.ascontiguousarray(vT.reshape(512, SEQ // 128, 128).transpose(2, 1, 0))
        m["bo"] = np.ascontiguousarray(pT[10240 + r * 512:10240 + (r + 1) * 512].reshape(4, 128, SEQ))
        m["bgi"] = np.ascontiguousarray(pT[12288 + r].reshape(SEQ // 128, 128))
        m["bgf"] = np.ascontiguousarray(pT[12292 + r].reshape(SEQ // 128, 128))
        m["bgb"] = np.ascontiguousarray(np.tile(np.array([[b_igate[r], b_fgate[r]]], np.float32), (SEQ // 128, 1)))
        m["bng"] = fm_vec(np.ascontiguousarray(norm_g[r * 512:(r + 1) * 512]))
        m["bmask"] = mask
        m["bcst"] = cst
        maps.append(m)
    out = _launch(nc, maps)
    catTs = [np.empty((D_MODEL, SEQ), np.float32) for _ in range(BATCH)]
    for c in range(NCORES):
        b, r = divmod(c, CORES_PER_BATCH)
        catTs[b][4 * r * 128:(4 * r + 4) * 128] = out[c]["ao"]
        catTs[b][2048 + r * 512:2048 + (r + 1) * 512] = out[c]["bout"]
    return catTs


def _run_mix1(projT, conv_dw, conv_ln_g, conv_ln_b):
    NH = 4
    slopes = _alibi(16)
    conv_dw = np.asarray(conv_dw)
    nc = build_mix1(SEQ, NH, TOK_PER_CORE, 16)
    cst = moba_consts()
    esel = moba_esel()
    dcw = np.ascontiguousarray(conv_dw.T.reshape(16, 128, 31).transpose(1, 0, 2))
    dlv = np.ascontiguousarray(np.stack([fm_vec(np.asarray(conv_ln_g)), fm_vec(np.asarray(conv_ln_b))], axis=1))
    maps = []
    for c in range(NCORES):
        b, r = divmod(c, CORES_PER_BATCH)
        pT = projT[b]
        m = {}
        m["cq"] = _heads_T(pT, 4 * r * 128, NH)
        m["ck"] = _heads_T(pT, 2048 + 4 * r * 128, NH)
        m["cv"] = _heads_V(pT, 4096 + 4 * r * 128, NH)
        m["ctbl"] = np.stack([moba_table(slopes[4 * r + i]) for i in range(NH)])
        m["ccst"] = cst
        m["cesel"] = esel
        t0 = r * TOK_PER_CORE
        for name, row0 in (("dda", 6144), ("ddb", 8192)):
            blk = np.zeros((2048, TOK_PER_CORE + 30), np.float32)
            lo = max(0, t0 - 30)
            blk[:, 30 - (t0 - lo):] = pT[row0:row0 + 2048, lo:t0 + TOK_PER_CORE]
            m[name] = np.ascontiguousarray(blk.reshape(16, 128, TOK_PER_CORE + 30))
        m["dcw"] = dcw
        m["dlv"] = dlv
        maps.append(m)
    out = _launch(nc, maps)
    catTs = [np.empty((D_MODEL, SEQ), np.float32) for _ in range(BATCH)]
    for c in range(NCORES):
        b, r = divmod(c, CORES_PER_BATCH)
        catTs[b][4 * r * 128:(4 * r + 4) * 128] = out[c]["co"]
        catTs[b][2048:4096, r * TOK_PER_CORE:(r + 1) * TOK_PER_CORE] = out[c]["dout"]
    return catTs


def _run_ada(c, w_adas, b_adas):
    ncol = 9 * D_MODEL // NCORES
    cT = np.ascontiguousarray(np.asarray(c).T.reshape(D_MODEL // 128, 128, BATCH).transpose(1, 0, 2))
    nc = build_ada(D_MODEL, ncol, len(w_adas), BATCH)
    maps = []
    for k in range(NCORES):
        m = {"cT": cT}
        for l in range(len(w_adas)):
            m["w%d" % l] = np.ascontiguousarray(np.asarray(w_adas[l])[:, k * ncol:(k + 1) * ncol])
            m["b%d" % l] = np.ascontiguousarray(np.broadcast_to(np.asarray(b_adas[l])[k * ncol:(k + 1) * ncol], (BATCH, ncol)))
        maps.append(m)
    out = _launch(nc, maps)
    mods = []
    for l in range(len(w_adas)):
        full = np.concatenate([out[k]["m%d" % l] for k in range(NCORES)], axis=1)
        mods.append(full.reshape(BATCH, 9, D_MODEL))
    return mods


def _pad_w_in(w_in):
    w_in = np.asarray(w_in)
    ncol = w_in.shape[1]
    nch = -(-ncol // 128)
    if nch * 128 != ncol:
        w_in = np.concatenate([w_in, np.zeros((w_in.shape[0], nch * 128 - ncol), np.float32)], axis=1)
    return tile_w(w_in, 32), nch


def _run_chain(xTs, stages):
    spec = []
    shared = {}
    for i, st in enumerate(stages):
        if st['kind'] == 'proj':
            wt, nch = _pad_w_in(st['w_in'])
            shared["w_in%d" % i] = wt
            spec.append(('proj', nch))
        elif st['kind'] == 'out':
            shared["w_o%d" % i] = tile_w(np.asarray(st['w_out']), 32)
            spec.append(('out', None))
        else:
            shared["w_up%d" % i] = tile_w(np.asarray(st['w_up']), 32)
            shared["w_dn%d" % i] = tile_w(np.asarray(st['w_dn']), 32)
            spec.append(('ffn', None))
    nc = build_chain(D_MODEL, D_FF, TOK_PER_CORE, spec, OUT_T, FFN_T, FFN_TILES, PROJ_T)
    maps = []
    for c in range(NCORES):
        b, r = divmod(c, CORES_PER_BATCH)
        m = dict(shared)
        m["xT"] = xTs[c]
        for i, st in enumerate(stages):
            m["vecs%d" % i] = _vecs(st['mod_l'], b, st['j'], st['ln_g'], st['ln_b'])
            if st['kind'] == 'out':
                m["cT%d" % i] = np.ascontiguousarray(st['catTs'][b][:, r * TOK_PER_CORE:(r + 1) * TOK_PER_CORE])
        maps.append(m)
    out = _launch(nc, maps)
    xprod = [i for i, st in enumerate(stages) if st['kind'] != 'proj']
    xo = [np.ascontiguousarray(o["oT%d" % xprod[-1]]) for o in out]
    projT = None
    for i, st in enumerate(stages):
        if st['kind'] == 'proj':
            projT = [np.concatenate([out[b * CORES_PER_BATCH + r]["pT%d" % i] for r in range(CORES_PER_BATCH)], axis=1)
                     for b in range(BATCH)]
    return xo, projT


def kernel(x, c,
           l0_w_ada, l0_b_ada, l0_ln_g, l0_ln_b, l0_ffn1_w_up, l0_ffn1_w_down, l0_ffn2_w_up, l0_ffn2_w_down,
           l0_w_in, l0_conv_qk, l0_b_igate, l0_b_fgate, l0_mlstm_norm_g, l0_w_out,
           l1_w_ada, l1_b_ada, l1_ln_g, l1_ln_b, l1_ffn1_w_up, l1_ffn1_w_down, l1_ffn2_w_up, l1_ffn2_w_down,
           l1_w_in, l1_conv_dw, l1_conv_ln_g, l1_conv_ln_b, l1_w_out):
    x = np.asarray(x, dtype=np.float32)
    xTs = []
    for k in range(NCORES):
        b, r = divmod(k, CORES_PER_BATCH)
        xTs.append(np.ascontiguousarray(x[b, r * TOK_PER_CORE:(r + 1) * TOK_PER_CORE, :].T))
    mods = _run_ada(c, [l0_w_ada, l1_w_ada], [l0_b_ada, l1_b_ada])
    g0, b0, g1, b1 = (np.asarray(v) for v in (l0_ln_g, l0_ln_b, l1_ln_g, l1_ln_b))

    def st(kind, l, j, **kw):
        d = {'kind': kind, 'mod_l': mods[l], 'j': j, 'ln_g': (g0, g1)[l], 'ln_b': (b0, b1)[l]}
        d.update(kw)
        return d

    xTs, projT = _run_chain(xTs, [st('ffn', 0, 0, w_up=l0_ffn1_w_up, w_dn=l0_ffn1_w_down), st('proj', 0, 1, w_in=l0_w_in)])
    catTs = _run_mix0(projT, l0_conv_qk, np.asarray(l0_b_igate), np.asarray(l0_b_fgate), l0_mlstm_norm_g)
    del projT
    xTs, projT = _run_chain(xTs, [st('out', 0, 1, w_out=l0_w_out, catTs=catTs),
                                  st('ffn', 0, 2, w_up=l0_ffn2_w_up, w_dn=l0_ffn2_w_down),
                                  st('ffn', 1, 0, w_up=l1_ffn1_w_up, w_dn=l1_ffn1_w_down),
                                  st('proj', 1, 1, w_in=l1_w_in)])
    del catTs
    catTs = _run_mix1(projT, l1_conv_dw, l1_conv_ln_g, l1_conv_ln_b)
    del projT
    xTs, _ = _run_chain(xTs, [st('out', 1, 1, w_out=l1_w_out, catTs=catTs),
                              st('ffn', 1, 2, w_up=l1_ffn2_w_up, w_dn=l1_ffn2_w_down)])
    out = np.empty((BATCH, SEQ, D_MODEL), np.float32)
    for k in range(NCORES):
        b, r = divmod(k, CORES_PER_BATCH)
        out[b, r * TOK_PER_CORE:(r + 1) * TOK_PER_CORE, :] = xTs[k].T
    return out
```

```python
import contextlib
import numpy as np
import concourse.bass as bass
import concourse.mybir as mybir
from concourse.bass_utils import run_bass_kernel_spmd

F32 = mybir.dt.float32
F32R = mybir.dt.float32r
AF = mybir.ActivationFunctionType
ALU = mybir.AluOpType
AX = mybir.AxisListType
ENGS = ('pe', 'act', 'dve', 'pool', 'sp')

D_MODEL = 4096
SEQ = 4096
BATCH = 2
DEPTH = 2
D_FF = 2 * D_MODEL
ALPHA = (2 * DEPTH) ** 0.25
LN_EPS = 1e-5
NCORES = 8


class Buf:
    __slots__ = ('t', 'w', 'r', 'dsem', 'dcnt', 'name')

    def __init__(self, t, name):
        self.t = t
        self.name = name
        self.w = {}
        self.r = {}
        self.dsem = None
        self.dcnt = 0

    def __getitem__(self, k):
        return self.t[k]


class Prog:
    def __init__(self, nc, es):
        self.nc = nc
        self.top = es
        self.es = es
        self.q = {e: [] for e in ENGS}
        self.sem = {e: es.enter_context(nc.semaphore('c_' + e)) for e in ENGS}
        self.cnt = {e: 0 for e in ENGS}
        self.seen = {}
        self.ndsem = 0
        self.dpool = []
        self.dall = []
        self.pbufs = []
        self.prefix = ''

    def sb(self, name, shape, dt=F32):
        b = Buf(self.es.enter_context(self.nc.sbuf_tensor('s_' + self.prefix + name, list(shape), dt)), name)
        self.pbufs.append(b)
        return b

    def ps(self, name, shape, dt=F32):
        b = Buf(self.es.enter_context(self.nc.psum_tensor('p_' + self.prefix + name, list(shape), dt)), name)
        self.pbufs.append(b)
        return b

    def begin_phase(self, prefix):
        self.prefix = prefix
        self.es = contextlib.ExitStack()
        self.pbufs = []

    def end_phase(self):
        toks = [(self.sem[o], self.cnt[o], 'bar') for o in ENGS if self.cnt[o] > 0]
        toks += [(r[0], r[1], 'bar') for r in self.dall if r[1] > 0]
        for e in ENGS:
            self._waits(e, toks)
        self.emit()
        self.q = {e: [] for e in ENGS}
        for b in self.pbufs:
            if b.dsem is not None:
                self.dpool.append(b.dsem)
                b.dsem = None
        self.pbufs = []
        self.es.close()
        self.es = self.top
        self.prefix = ''

    def dram(self, name, shape, dt=F32, kind="Internal"):
        return Buf(self.nc.dram_tensor(name, list(shape), dt, kind=kind).ap(), name)

    def _waits(self, eng, toks):
        for sem, val, src in toks:
            if src == 'pe' and eng == 'pe':
                continue
            key = (eng, id(sem))
            if self.seen.get(key, 0) >= val:
                continue
            self.seen[key] = val
            self.q[eng].append(lambda e, sem=sem, val=val: e.wait_ge(sem, val))

    @staticmethod
    def _upd(d, tok):
        k = id(tok[0])
        if k not in d or d[k][1] < tok[1]:
            d[k] = tok

    @staticmethod
    def _deps(reads, writes):
        toks = []
        for b in reads:
            toks += list(b.w.values())
        for b in writes:
            toks += list(b.w.values()) + list(b.r.values())
        return toks

    def _record(self, tok, reads, writes, partial):
        for b in reads:
            self._upd(b.r, tok)
        for b in writes:
            if partial:
                self._upd(b.w, tok)
            else:
                b.w = {id(tok[0]): tok}
                b.r = {}

    def op(self, eng, fn, reads=(), writes=(), partial=False):
        self._waits(eng, self._deps(reads, writes))
        self.cnt[eng] += 1
        n = self.cnt[eng]
        sem = self.sem[eng]
        self.q[eng].append(lambda e: fn(e).then_inc(sem, 1))
        self._record((sem, n, eng), reads, writes, partial)

    def dma(self, eng, out, in_, reads=(), writes=(), partial=False, owner=None):
        self._waits(eng, self._deps(reads, writes))
        b = owner if owner is not None else (list(writes) + list(reads))[0]
        if b.dsem is None:
            if self.dpool:
                b.dsem = self.dpool.pop()
            else:
                b.dsem = [self.top.enter_context(self.nc.semaphore('d%d' % self.ndsem)), 0]
                self.ndsem += 1
                self.dall.append(b.dsem)
        b.dsem[1] += 16
        sem, val = b.dsem[0], b.dsem[1]
        self.q[eng].append(lambda e: e.dma_start(out=out, in_=in_).then_inc(sem, 16))
        self._record((sem, val, 'dma'), reads, writes, partial)

    def finish(self, eng, bufs):
        toks = []
        for b in bufs:
            toks += list(b.w.values()) + list(b.r.values())
        self._waits(eng, toks)

    def emit(self):
        q = self.q
        with self.nc.Block() as block:
            @block.tensor
            def _(e):
                for f in q['pe']:
                    f(e)

            @block.scalar
            def _(e):
                for f in q['act']:
                    f(e)

            @block.vector
            def _(e):
                for f in q['dve']:
                    f(e)

            @block.gpsimd
            def _(e):
                for f in q['pool']:
                    f(e)

            @block.sync
            def _(e):
                for f in q['sp']:
                    f(e)


def new_nc():
    nc = bass.Bass("TRN2", target_bir_lowering=False)
    nc.dge_precook = False
    return nc


class Ring:
    def __init__(self, p, name, n, width):
        self.p = p
        self.slots = [p.sb('%s%d' % (name, i), [128, width], F32R) for i in range(n)]
        self.i = 0

    def load(self, src_ap):
        b = self.slots[self.i % len(self.slots)]
        self.i += 1
        self.p.dma('sp', b[:], src_ap, writes=[b])
        return b


def tile_w(W, kcu):
    K, N = W.shape
    H = K // (128 * kcu)
    t = W.reshape(H, kcu, 128, N // 128, 128).transpose(3, 0, 2, 1, 4)
    return np.ascontiguousarray(t).reshape(N // 128, H, 128, kcu * 128)


def fm_vec(v):
    return np.ascontiguousarray(v.reshape(-1, 128).T)


def emit_ffn(p, D, DFF, NTOK, T, xT, vecs, w_up, w_dn, oT, res_w=0.5, tiles=None):
    KC = D // 128
    FC = DFF // 128
    KCU = min(32, KC)
    HU = KC // KCU
    HD = FC // KCU
    if tiles is None:
        tiles = [(t0, min(T, NTOK - t0)) for t0 in range(0, NTOK, T)]
    xv = xT.rearrange("(c p) t -> p c t", p=128)
    xvr = xT.bitcast(F32R).rearrange("(c p) t -> p c t", p=128)
    ov = oT.rearrange("(c p) t -> p c t", p=128)
    eps2 = LN_EPS / (ALPHA * ALPHA)
    u = p.sb("u", [128, KC, T], F32R)
    a = p.sb("a", [128, FC, T], F32R)
    ring = Ring(p, "wr", 3, KCU * 128)
    vs = p.sb("vs", [128, 5, KC])
    sc1 = p.sb("sc1", [128, KC])
    gwa = p.sb("gwa", [128, KC])
    ones = p.sb("ones", [128, 128])
    sil = [p.sb("sil%d" % i, [128, T]) for i in range(2)]
    xc = [p.sb("xc%d" % i, [128, T]) for i in range(2)]
    sq = [p.sb("sq%d" % i, [128, T]) for i in range(2)]
    oc = [p.sb("oc%d" % i, [128, T]) for i in range(2)]
    t1 = [p.sb("t1%d" % i, [128, T]) for i in range(2)]
    mean = p.sb("mean", [128, T])
    msq = p.sb("msq", [128, T])
    rstd = p.sb("rstd", [128, T])
    pg = [p.ps("pg%d" % i, [128, T]) for i in range(2)]
    pv = [p.ps("pv%d" % i, [128, T]) for i in range(2)]
    py = [p.ps("py%d" % i, [128, T]) for i in range(2)]
    s1 = p.ps("s1", [128, T])
    s2 = p.ps("s2", [128, T])

    p.dma('pool', vs[:], vecs, writes=[vs])
    p.op('dve', lambda e: e.memset(ones[:], 1.0), writes=[ones])
    p.op('dve', lambda e: e.tensor_scalar_add(sc1[:], vs[:, 1, :], 1.0), reads=[vs], writes=[sc1])
    p.op('dve', lambda e: e.tensor_scalar(gwa[:], vs[:, 2, :], 1.0, res_w / ALPHA, ALU.add, ALU.mult),
         reads=[vs], writes=[gwa])

    for (t0, tt) in tiles:
        p.dma('pool', u[:, :, 0:tt], xvr[:, :, t0:t0 + tt], writes=[u])
        for c in range(KC):
            p.op('dve', lambda e, c=c, tt=tt: e.tensor_scalar(u[:, c, 0:tt], u[:, c, 0:tt].bitcast(F32), sc1[:, c:c + 1],
                                                              vs[:, 0, c:c + 1], ALU.mult, ALU.add),
                 reads=[u, sc1, vs], writes=[u], partial=True)
        for f in range(FC):
            g_ps, v_ps = pg[f % 2], pv[f % 2]
            for (n, ps) in ((f, g_ps), (FC + f, v_ps)):
                for h in range(HU):
                    wb = ring.load(w_up[n, h])
                    for kc in range(KCU):
                        k = h * KCU + kc
                        p.op('pe', lambda e, ps=ps, wb=wb, kc=kc, k=k, tt=tt: e.matmul(
                            ps[:, 0:tt], wb[:, kc * 128:(kc + 1) * 128], u[:, k, 0:tt], start=(k == 0), stop=(k == KC - 1)),
                            reads=[wb, u], writes=[ps], partial=(k > 0))
            sb_ = sil[f % 2]
            p.op('act', lambda e, sb_=sb_, g_ps=g_ps, tt=tt: e.activation(sb_[:, 0:tt], g_ps[:, 0:tt], AF.Silu),
                 reads=[g_ps], writes=[sb_])
            p.op('dve', lambda e, sb_=sb_, v_ps=v_ps, f=f, tt=tt: e.tensor_tensor(a[:, f, 0:tt], sb_[:, 0:tt], v_ps[:, 0:tt], ALU.mult),
                 reads=[sb_, v_ps], writes=[a], partial=True)
        for n in range(KC):
            ps = py[n % 2]
            for h in range(HD):
                wb = ring.load(w_dn[n, h])
                for kc in range(KCU):
                    k = h * KCU + kc
                    p.op('pe', lambda e, ps=ps, wb=wb, kc=kc, k=k, tt=tt: e.matmul(
                        ps[:, 0:tt], wb[:, kc * 128:(kc + 1) * 128], a[:, k, 0:tt], start=(k == 0), stop=(k == FC - 1)),
                        reads=[wb, a], writes=[ps], partial=(k > 0))
            xb = xc[n % 2]
            p.dma('pool', xb[:, 0:tt], xv[:, n, t0:t0 + tt], writes=[xb])
            p.op('dve', lambda e, ps=ps, xb=xb, n=n, tt=tt: e.scalar_tensor_tensor(
                u[:, n, 0:tt], ps[:, 0:tt], gwa[:, n:n + 1], xb[:, 0:tt], ALU.mult, ALU.add),
                reads=[ps, xb, gwa], writes=[u], partial=True)
            qb = sq[n % 2]
            p.op('act', lambda e, qb=qb, n=n, tt=tt: e.activation(qb[:, 0:tt], u[:, n, 0:tt].bitcast(F32), AF.Square),
                 reads=[u], writes=[qb])
            p.op('pe', lambda e, n=n, tt=tt: e.matmul(s1[:, 0:tt], ones[:], u[:, n, 0:tt].bitcast(F32),
                                                      start=(n == 0), stop=(n == KC - 1)),
                 reads=[ones, u], writes=[s1], partial=(n > 0))
            p.op('pe', lambda e, n=n, qb=qb, tt=tt: e.matmul(s2[:, 0:tt], ones[:], qb[:, 0:tt], start=(n == 0), stop=(n == KC - 1)),
                 reads=[ones, qb], writes=[s2], partial=(n > 0))
        p.op('dve', lambda e, tt=tt: e.tensor_scalar_mul(mean[:, 0:tt], s1[:, 0:tt], 1.0 / D), reads=[s1], writes=[mean])
        p.op('dve', lambda e, tt=tt: e.tensor_tensor(msq[:, 0:tt], mean[:, 0:tt], mean[:, 0:tt], ALU.mult), reads=[mean], writes=[msq])
        p.op('dve', lambda e, tt=tt: e.scalar_tensor_tensor(rstd[:, 0:tt], s2[:, 0:tt], 1.0 / D, msq[:, 0:tt], ALU.mult, ALU.subtract),
             reads=[s2, msq], writes=[rstd])
        p.op('dve', lambda e, tt=tt: e.tensor_scalar_add(rstd[:, 0:tt], rstd[:, 0:tt], eps2), reads=[rstd], writes=[rstd])
        p.op('act', lambda e, tt=tt: e.activation(rstd[:, 0:tt], rstd[:, 0:tt], AF.Sqrt), reads=[rstd], writes=[rstd])
        p.op('dve', lambda e, tt=tt: e.reciprocal(rstd[:, 0:tt], rstd[:, 0:tt]), reads=[rstd], writes=[rstd])
        for n in range(KC):
            tb, ob = t1[n % 2], oc[n % 2]
            p.op('dve', lambda e, tb=tb, n=n, tt=tt: e.tensor_tensor(tb[:, 0:tt], u[:, n, 0:tt].bitcast(F32), mean[:, 0:tt], ALU.subtract),
                 reads=[u, mean], writes=[tb])
            p.op('dve', lambda e, tb=tb, tt=tt: e.tensor_tensor(tb[:, 0:tt], tb[:, 0:tt], rstd[:, 0:tt], ALU.mult),
                 reads=[tb, rstd], writes=[tb])
            p.op('act', lambda e, tb=tb, ob=ob, n=n, tt=tt: e.activation(ob[:, 0:tt], tb[:, 0:tt], AF.Identity,
                                                                       bias=vs[:, 4, n:n + 1], scale=vs[:, 3, n:n + 1]),
                 reads=[tb, vs], writes=[ob])
            p.dma('pool', ov[:, n, t0:t0 + tt], ob[:, 0:tt], reads=[ob])
    return oc


def build_ffn(D, DFF, NTOK, T, res_w=0.5, tiles=None):
    KC = D // 128
    FC = DFF // 128
    KCU = min(32, KC)
    nc = new_nc()
    xT = nc.dram_tensor("xT", [D, NTOK], F32, kind="ExternalInput").ap()
    vecs = nc.dram_tensor("vecs", [128, 5, KC], F32, kind="ExternalInput").ap()
    w_up = nc.dram_tensor("w_up", [2 * FC, KC // KCU, 128, KCU * 128], F32R, kind="ExternalInput").ap()
    w_dn = nc.dram_tensor("w_dn", [KC, FC // KCU, 128, KCU * 128], F32R, kind="ExternalInput").ap()
    oT = nc.dram_tensor("oT", [D, NTOK], F32, kind="ExternalOutput").ap()
    with contextlib.ExitStack() as es:
        p = Prog(nc, es)
        oc = emit_ffn(p, D, DFF, NTOK, T, xT, vecs, w_up, w_dn, oT, res_w, tiles)
        p.finish('pool', oc)
        p.emit()
    return nc


def ffn_inputs(xT_core, shift, scale, gate, ln_g, ln_b, w_up_t, w_dn_t):
    vecs = np.ascontiguousarray(np.stack([fm_vec(v) for v in (shift, scale, gate, ln_g, ln_b)], axis=1))
    return {"xT": np.ascontiguousarray(xT_core), "vecs": vecs, "w_up": w_up_t, "w_dn": w_dn_t}


def emit_gemm(p, ring, w_ap, n, HU, KCU, src, ps, KC):
    for h in range(HU):
        wb = ring.load(w_ap[n, h])
        for kc in range(KCU):
            k = h * KCU + kc
            p.op('pe', lambda e, ps=ps, wb=wb, kc=kc, k=k: e.matmul(
                ps[:], wb[:, kc * 128:(kc + 1) * 128], src[:, k, :], start=(k == 0), stop=(k == KC - 1)),
                reads=[wb, src], writes=[ps], partial=(k > 0))


def emit_ln_stats_finalize(p, s1, s2, mean, msq, rstd, nfeat, eps):
    p.op('dve', lambda e: e.tensor_scalar_mul(mean[:], s1[:], 1.0 / nfeat), reads=[s1], writes=[mean])
    p.op('dve', lambda e: e.tensor_tensor(msq[:], mean[:], mean[:], ALU.mult), reads=[mean], writes=[msq])
    p.op('dve', lambda e: e.scalar_tensor_tensor(rstd[:], s2[:], 1.0 / nfeat, msq[:], ALU.mult, ALU.subtract),
         reads=[s2, msq], writes=[rstd])
    p.op('dve', lambda e: e.tensor_scalar_add(rstd[:], rstd[:], eps), reads=[rstd], writes=[rstd])
    p.op('act', lambda e: e.activation(rstd[:], rstd[:], AF.Sqrt), reads=[rstd], writes=[rstd])
    p.op('dve', lambda e: e.reciprocal(rstd[:], rstd[:]), reads=[rstd], writes=[rstd])


def emit_proj(p, D, NCH, NTOK, T, xT, vecs, w_in, oT):
    KC = D // 128
    KCU = min(32, KC)
    HU = KC // KCU
    xvr = xT.bitcast(F32R).rearrange("(c p) t -> p c t", p=128)
    ov = oT.rearrange("(c p) t -> p c t", p=128)
    u = p.sb("u", [128, KC, T], F32R)
    ring = Ring(p, "wr", 4, KCU * 128)
    vs = p.sb("vs", [128, 5, KC])
    sc1 = p.sb("sc1", [128, KC])
    oc = [p.sb("oc%d" % i, [128, T]) for i in range(4)]
    pps = [p.ps("pp%d" % i, [128, T]) for i in range(4)]
    p.dma('pool', vs[:], vecs, writes=[vs])
    p.op('dve', lambda e: e.tensor_scalar_add(sc1[:], vs[:, 1, :], 1.0), reads=[vs], writes=[sc1])
    for t0 in range(0, NTOK, T):
        p.dma('pool', u[:], xvr[:, :, t0:t0 + T], writes=[u])
        for c in range(KC):
            p.op('dve', lambda e, c=c: e.tensor_scalar(u[:, c, :], u[:, c, :].bitcast(F32), sc1[:, c:c + 1],
                                                       vs[:, 0, c:c + 1], ALU.mult, ALU.add),
                 reads=[u, sc1, vs], writes=[u], partial=True)
        for n in range(NCH):
            ps = pps[n % 4]
            ob = oc[n % 4]
            emit_gemm(p, ring, w_in, n, HU, KCU, u, ps, KC)
            if n % 2 == 0:
                p.op('act', lambda e, ob=ob, ps=ps: e.copy(ob[:], ps[:]), reads=[ps], writes=[ob])
            else:
                p.op('dve', lambda e, ob=ob, ps=ps: e.tensor_copy(ob[:], ps[:]), reads=[ps], writes=[ob])
            p.dma('pool', ov[:, n, t0:t0 + T], ob[:], reads=[ob])
    return oc


def build_proj(D, NCH, NTOK, T):
    KC = D // 128
    KCU = min(32, KC)
    nc = new_nc()
    xT = nc.dram_tensor("xT", [D, NTOK], F32, kind="ExternalInput").ap()
    vecs = nc.dram_tensor("vecs", [128, 5, KC], F32, kind="ExternalInput").ap()
    w_in = nc.dram_tensor("w_in", [NCH, KC // KCU, 128, KCU * 128], F32R, kind="ExternalInput").ap()
    oT = nc.dram_tensor("oT", [NCH * 128, NTOK], F32, kind="ExternalOutput").ap()
    with contextlib.ExitStack() as es:
        p = Prog(nc, es)
        oc = emit_proj(p, D, NCH, NTOK, T, xT, vecs, w_in, oT)
        p.finish('pool', oc)
        p.emit()
    return nc


def emit_out(p, D, NTOK, T, xT, cT, vecs, w_o, oT, res_w=1.0):
    KC = D // 128
    KCU = min(32, KC)
    HD = KC // KCU
    xv = xT.rearrange("(c p) t -> p c t", p=128)
    cv = cT.rearrange("(c p) t -> p c t", p=128)
    ov = oT.rearrange("(c p) t -> p c t", p=128)
    eps2 = LN_EPS / (ALPHA * ALPHA)
    a = p.sb("a", [128, KC, T], F32R)
    z = p.sb("z", [128, KC, T], F32)
    ring = Ring(p, "wr", 3, KCU * 128)
    vs = p.sb("vs", [128, 5, KC])
    gwa = p.sb("gwa", [128, KC])
    ones = p.sb("ones", [128, 128])
    xc = [p.sb("xc%d" % i, [128, T]) for i in range(2)]
    sq = [p.sb("sq%d" % i, [128, T]) for i in range(2)]
    oc = [p.sb("oc%d" % i, [128, T]) for i in range(2)]
    t1 = [p.sb("t1%d" % i, [128, T]) for i in range(2)]
    mean = p.sb("mean", [128, T])
    msq = p.sb("msq", [128, T])
    rstd = p.sb("rstd", [128, T])
    py = [p.ps("py%d" % i, [128, T]) for i in range(2)]
    s1 = p.ps("s1", [128, T])
    s2 = p.ps("s2", [128, T])
    p.dma('pool', vs[:], vecs, writes=[vs])
    p.op('dve', lambda e: e.memset(ones[:], 1.0), writes=[ones])
    p.op('dve', lambda e: e.tensor_scalar(gwa[:], vs[:, 2, :], 1.0, res_w / ALPHA, ALU.add, ALU.mult),
         reads=[vs], writes=[gwa])
    for t0 in range(0, NTOK, T):
        p.dma('pool', a[:], cv[:, :, t0:t0 + T], writes=[a])
        emit_down_ln(p, ring, w_o, KC, HD, KCU, KC, a, z, py, s1, s2, xc, sq, t1, oc, mean, msq, rstd,
                     xv, ov, t0, T, gwa, vs, ones, D, eps2)
    return oc


def build_out(D, NTOK, T, res_w=1.0):
    KC = D // 128
    KCU = min(32, KC)
    nc = new_nc()
    xT = nc.dram_tensor("xT", [D, NTOK], F32, kind="ExternalInput").ap()
    cT = nc.dram_tensor("cT", [D, NTOK], F32R, kind="ExternalInput").ap()
    vecs = nc.dram_tensor("vecs", [128, 5, KC], F32, kind="ExternalInput").ap()
    w_o = nc.dram_tensor("w_o", [KC, KC // KCU, 128, KCU * 128], F32R, kind="ExternalInput").ap()
    oT = nc.dram_tensor("oT", [D, NTOK], F32, kind="ExternalOutput").ap()
    with contextlib.ExitStack() as es:
        p = Prog(nc, es)
        oc = emit_out(p, D, NTOK, T, xT, cT, vecs, w_o, oT, res_w)
        p.finish('pool', oc)
        p.emit()
    return nc


def build_chain(D, DFF, NTOK, stages, out_T, ffn_T, ffn_tiles, proj_T=512):
    KC = D // 128
    FC = DFF // 128
    KCU = min(32, KC)
    nc = new_nc()
    xT = nc.dram_tensor("xT", [D, NTOK], F32, kind="ExternalInput").ap()
    xprod = [i for i, (k, _) in enumerate(stages) if k != 'proj']
    with contextlib.ExitStack() as es:
        p = Prog(nc, es)
        cur = xT
        for i, (kind, arg) in enumerate(stages):
            vecs = nc.dram_tensor("vecs%d" % i, [128, 5, KC], F32, kind="ExternalInput").ap()
            p.begin_phase("ph%d_" % i)
            if kind == 'proj':
                w_in = nc.dram_tensor("w_in%d" % i, [arg, KC // KCU, 128, KCU * 128], F32R, kind="ExternalInput").ap()
                pT = nc.dram_tensor("pT%d" % i, [arg * 128, NTOK], F32, kind="ExternalOutput").ap()
                oc = emit_proj(p, D, arg, NTOK, proj_T, cur, vecs, w_in, pT)
            else:
                ext = (i == xprod[-1]) or (i + 1 < len(stages) and stages[i + 1][0] == 'proj')
                dst = nc.dram_tensor("oT%d" % i, [D, NTOK], F32, kind="ExternalOutput" if ext else "Internal").ap()
                if kind == 'out':
                    cT = nc.dram_tensor("cT%d" % i, [D, NTOK], F32R, kind="ExternalInput").ap()
                    w_o = nc.dram_tensor("w_o%d" % i, [KC, KC // KCU, 128, KCU * 128], F32R, kind="ExternalInput").ap()
                    oc = emit_out(p, D, NTOK, out_T, cur, cT, vecs, w_o, dst, 1.0)
                else:
                    w_up = nc.dram_tensor("w_up%d" % i, [2 * FC, KC // KCU, 128, KCU * 128], F32R, kind="ExternalInput").ap()
                    w_dn = nc.dram_tensor("w_dn%d" % i, [KC, FC // KCU, 128, KCU * 128], F32R, kind="ExternalInput").ap()
                    oc = emit_ffn(p, D, DFF, NTOK, ffn_T, cur, vecs, w_up, w_dn, dst, 0.5, ffn_tiles)
                cur = dst
            p.finish('pool', oc)
            p.end_phase()
    return nc


def emit_down_ln(p, ring, w_dn, KC, HD, KCU, FC, a, z, py, s1, s2, xc, sq, t1, oc, mean, msq, rstd,
                 xv, ov, t0, T, gwa, vs, ones, D, eps2):
    for n in range(KC):
        ps = py[n % 2]
        emit_gemm(p, ring, w_dn, n, HD, KCU, a, ps, FC)
        xb = xc[n % 2]
        p.dma('pool', xb[:], xv[:, n, t0:t0 + T], writes=[xb])
        p.op('dve', lambda e, ps=ps, xb=xb, n=n: e.scalar_tensor_tensor(
            z[:, n, :], ps[:], gwa[:, n:n + 1], xb[:], ALU.mult, ALU.add),
            reads=[ps, xb, gwa], writes=[z], partial=True)
        qb = sq[n % 2]
        p.op('act', lambda e, qb=qb, n=n: e.activation(qb[:], z[:, n, :], AF.Square), reads=[z], writes=[qb])
        p.op('pe', lambda e, n=n: e.matmul(s1[:], ones[:], z[:, n, :], start=(n == 0), stop=(n == KC - 1)),
             reads=[ones, z], writes=[s1], partial=(n > 0))
        p.op('pe', lambda e, n=n, qb=qb: e.matmul(s2[:], ones[:], qb[:], start=(n == 0), stop=(n == KC - 1)),
             reads=[ones, qb], writes=[s2], partial=(n > 0))
    emit_ln_stats_finalize(p, s1, s2, mean, msq, rstd, D, eps2)
    for n in range(KC):
        tb, ob = t1[n % 2], oc[n % 2]
        p.op('dve', lambda e, tb=tb, n=n: e.tensor_tensor(tb[:], z[:, n, :], mean[:], ALU.subtract),
             reads=[z, mean], writes=[tb])
        p.op('dve', lambda e, tb=tb: e.tensor_tensor(tb[:], tb[:], rstd[:], ALU.mult),
             reads=[tb, rstd], writes=[tb])
        p.op('act', lambda e, tb=tb, ob=ob, n=n: e.activation(ob[:], tb[:], AF.Identity,
                                                            bias=vs[:, 4, n:n + 1], scale=vs[:, 3, n:n + 1]),
             reads=[tb, vs], writes=[ob])
        p.dma('pool', ov[:, n, t0:t0 + T], ob[:], reads=[ob])


def build_ada(D, NCOL, NL, NB):
    KC = D // 128
    CB = NCOL // 512
    nc = new_nc()
    cT = nc.dram_tensor("cT", [128, KC, NB], F32, kind="ExternalInput").ap()
    ws = [nc.dram_tensor("w%d" % l, [D, NCOL], F32R, kind="ExternalInput").ap() for l in range(NL)]
    bs = [nc.dram_tensor("b%d" % l, [NB, NCOL], F32, kind="ExternalInput").ap() for l in range(NL)]
    outs = [nc.dram_tensor("m%d" % l, [NB, NCOL], F32, kind="ExternalOutput").ap() for l in range(NL)]
    with contextlib.ExitStack() as es:
        p = Prog(nc, es)
        cs = p.sb("cs", [128, KC, NB])
        sc = p.sb("sc", [128, KC, NB], F32R)
        ring = Ring(p, "wr", 6, 8 * 512)
        bsb = [p.sb("bsb%d" % l, [NB, NCOL]) for l in range(NL)]
        osb = [p.sb("osb%d" % l, [NB, NCOL]) for l in range(NL)]
        pps = [p.ps("pp%d" % i, [NB, 512]) for i in range(2)]
        p.dma('pool', cs[:], cT, writes=[cs])
        p.op('act', lambda e: e.activation(sc[:], cs[:], AF.Silu), reads=[cs], writes=[sc])
        for l in range(NL):
            p.dma('pool', bsb[l][:], bs[l], writes=[bsb[l]])
            wv = ws[l].rearrange("(k p) n -> p k n", p=128)
            for cb in range(CB):
                ps = pps[cb % 2]
                for h in range(KC // 8):
                    b = ring.slots[ring.i % len(ring.slots)]
                    ring.i += 1
                    p.dma('sp', b[:].rearrange("p (k n) -> p k n", k=8), wv[:, h * 8:(h + 1) * 8, cb * 512:(cb + 1) * 512],
                          writes=[b])
                    for kc in range(8):
                        k = h * 8 + kc
                        p.op('pe', lambda e, ps=ps, b=b, kc=kc, k=k: e.matmul(
                            ps[:], sc[:, k, :], b[:, kc * 512:(kc + 1) * 512], start=(k == 0), stop=(k == KC - 1)),
                            reads=[b, sc], writes=[ps], partial=(k > 0))
                p.op('dve', lambda e, ps=ps, l=l, cb=cb: e.tensor_tensor(
                    osb[l][:, cb * 512:(cb + 1) * 512], ps[:], bsb[l][:, cb * 512:(cb + 1) * 512], ALU.add),
                    reads=[ps, bsb[l]], writes=[osb[l]], partial=True)
            p.dma('pool', outs[l], osb[l][:], reads=[osb[l]])
        p.finish('pool', osb)
        p.emit()
    return nc


NEG = -30000.0


def emit_dilattn(p, NH, S, qT_d, kT_d, v_d, tbl_d, out_d, banks, ones_r, scale, qk, scr):
    NT = S // 128
    NQB = S // 512
    LA = 4
    q = qk.t[:, 0, :]
    k = qk.t[:, 1, :]
    v = qk.t[:, 2, :].rearrange("p (t d) -> p t d", d=128)
    tbl = p.sb('atbl', [128, 23 * 128])
    sring = [banks[0], banks[1], banks[6], banks[7], banks[4], banks[5]]
    sc = scr['f'][0:2] + scr['f'][5:7]
    pt = scr['r'][0:4]
    ob, rd = scr['f'][2:4], scr['f'][4]
    for h in range(NH):
        p.dma('sp', q, qT_d[h], writes=[qk])
        p.dma('sp', k, kT_d[h], writes=[qk], partial=True)
        p.dma('sp', v, v_d[h], writes=[qk], partial=True)
        p.dma('sp', tbl[:], tbl_d[h], writes=[tbl])
        pairs = []
        for qb in range(NQB):
            kts = list(range(max(0, 4 * qb - 16), 4 * qb + 4))
            for i, kt in enumerate(kts):
                pairs.append((qb, kt, i == 0, i == len(kts) - 1))
        n = len(pairs)
        for it in range(n + LA):
            if it < n:
                qb, kt, first, last = pairs[it]
                s_ps = sring[it % 6]
                p.op('pe', lambda e, s_ps=s_ps, kt=kt, qb=qb: e.matmul(
                    s_ps[:], k[:, kt * 128:(kt + 1) * 128], q[:, qb * 512:(qb + 1) * 512], start=True, stop=True),
                    reads=[qk], writes=[s_ps])
            j = it - LA
            if j < 0:
                continue
            qb, kt, first, last = pairs[j]
            s_ps = sring[j % 6]
            sci, pti = sc[j % 4], pt[j % 4]
            o_ps = banks[2]
            d_ps = banks[3]
            off = (4 * qb - kt + 3) * 128
            p.op('dve', lambda e, s_ps=s_ps, sci=sci, off=off: e.scalar_tensor_tensor(
                sci[:], s_ps[:], scale, tbl[:, off:off + 512], ALU.mult, ALU.add),
                reads=[s_ps, tbl], writes=[sci])
            p.op('act', lambda e, sci=sci, pti=pti: e.activation(pti[:], sci[:], AF.Exp), reads=[sci], writes=[pti])
            p.op('pe', lambda e, o_ps=o_ps, kt=kt, pti=pti, first=first, last=last: e.matmul(
                o_ps[:], v[:, kt, :], pti[:], start=first, stop=last), reads=[qk, pti], writes=[o_ps], partial=not first)
            p.op('pe', lambda e, d_ps=d_ps, pti=pti, first=first, last=last: e.matmul(
                d_ps[:], ones_r[:], pti[:], start=first, stop=last), reads=[ones_r, pti], writes=[d_ps], partial=not first)
            if last:
                obi = ob[qb % 2]
                p.op('dve', lambda e, d_ps=d_ps: e.reciprocal(rd[:], d_ps[:]), reads=[d_ps], writes=[rd])
                p.op('dve', lambda e, o_ps=o_ps, obi=obi: e.tensor_tensor(obi[:], o_ps[:], rd[:], ALU.mult),
                     reads=[o_ps, rd], writes=[obi])
                p.dma('pool', out_d[h * 128:(h + 1) * 128, qb * 512:(qb + 1) * 512], obi[:], reads=[obi])
    return ob


def dil_table(slope):
    c = np.arange(23 * 128)[None, :]
    jl = np.arange(128)[:, None]
    dist = c - 384 - jl
    cnt = ((dist <= 128).astype(np.float64) + ((dist % 4 == 0) & (dist <= 512)) + ((dist % 16 == 0) & (dist <= 2048)))
    ok = (dist >= 0) & (dist <= 2048) & (cnt > 0)
    val = np.where(ok, -slope * dist + np.log(np.maximum(cnt, 1.0)), NEG)
    return val.astype(np.float32)


def emit_mlstm(p, S, qkpre_d, cw_d, bv_d, bo_d, gi_d, gf_d, gb_d, ng_d, mask_d, cst_d, out_d, banks, ones_r, ones_f, qk, scr):
    NT = S // 128
    NQB = S // 512
    DK = 256
    PIECE = 512
    pre = [p.sb('bpre%d' % i, [128, PIECE + 3]) for i in range(2)]
    acc = [p.sb('bacc%d' % i, [128, PIECE]) for i in range(2)]
    cw = p.sb('bcw', [128, 4, 4])
    gb = p.sb('bgb', [32, 2])
    ngb = p.sb('bngb', [32, 1])
    ng = p.sb('bng', [128, 4])
    mask = p.sb('bmask', [128, 4, 512])
    bcol = p.sb('bbcol', [128, NT])
    fb = [p.sb('bfb%d' % i, [128, 512]) for i in range(2)]
    dt_ = scr['f'][0:2]
    at = scr['r'][0:2]
    bvt = [p.sb('bbvt%d' % i, [128, 512], F32R) for i in range(3)]
    dt_ = dt_ + [scr['f'][10]]
    dt_ = dt_[0:2]
    bo = [p.sb('bbo', [128, 4, 512])] * 2
    hT = p.sb('bhT', [128, 4, 512])
    sqh = scr['f'][5:7]
    rden = scr['f'][4]
    mean = p.sb('bmean', [128, 512])
    msq = p.sb('bmsq', [128, 512])
    rstd = p.sb('brstd', [128, 512])
    t1 = scr['f'][7:9]
    sg = scr['f'][9:11]
    ob = scr['f'][2:4]

    p.dma('pool', cw[:], cw_d, writes=[cw])
    p.dma('pool', gb[:], gb_d, writes=[gb])
    p.dma('pool', ng[:], ng_d, writes=[ng])
    p.dma('pool', mask[:], mask_d, writes=[mask])
    NTT = S // 128
    gi = p.sb('bgi', [NTT, 128])
    ga = p.sb('bga', [NTT, 128])
    gc = p.sb('bgc', [NTT, 128])
    F2d = p.sb('bF2d', [NTT, 128])
    b2d = p.sb('bb2d', [NTT, 128])
    offs = p.sb('boffs', [NTT, 1])
    cst = p.sb('bcst', [NTT, 2 * NTT + NTT * 128])
    p.dma('pool', cst[:], cst_d, writes=[cst])
    p.dma('pool', gi[:], gi_d, writes=[gi])
    p.dma('pool', ga[:], gf_d, writes=[ga])
    p.op('dve', lambda e: e.tensor_scalar_mul(ngb[:], gb[:, 1:2], -1.0), reads=[gb], writes=[ngb])
    p.op('act', lambda e: e.activation(gc[:], ga[:], AF.Exp, bias=ngb[:, 0:1], scale=-1.0), reads=[ga, ngb], writes=[gc])
    p.op('dve', lambda e: e.tensor_scalar_add(gc[:], gc[:], 1.0), reads=[gc], writes=[gc])
    p.op('act', lambda e: e.activation(ga[:], gc[:], AF.Ln), reads=[gc], writes=[ga])
    p.op('dve', lambda e: e.tensor_scalar_mul(ga[:], ga[:], -1.0), reads=[ga], writes=[ga])
    cur, oth = ga, gc
    sh = 1
    while sh < 128:
        p.op('dve', lambda e, cur=cur, oth=oth, sh=sh: e.tensor_copy(oth[:, 0:sh], cur[:, 0:sh]), reads=[cur], writes=[oth])
        p.op('dve', lambda e, cur=cur, oth=oth, sh=sh: e.tensor_tensor(oth[:, sh:128], cur[:, sh:128], cur[:, 0:128 - sh], ALU.add),
             reads=[cur], writes=[oth], partial=True)
        cur, oth = oth, cur
        sh *= 2
    misc = banks[7]
    p.op('pe', lambda e, cur=cur: e.matmul(misc[0:NTT, 0:1], cst[:, 0:NTT], cur[:, 127:128], start=True, stop=True),
         reads=[cst, cur], writes=[misc])
    p.op('dve', lambda e: e.tensor_copy(offs[:], misc[0:NTT, 0:1]), reads=[misc], writes=[offs])
    p.op('dve', lambda e, cur=cur: e.tensor_scalar(F2d[:], cur[:], offs[:, 0:1], None, ALU.add), reads=[cur, offs], writes=[F2d])
    p.op('dve', lambda e: e.scalar_tensor_tensor(b2d[:], gi[:], gb[:, 0:1], F2d[:], ALU.add, ALU.subtract),
         reads=[gi, gb, F2d], writes=[b2d])
    p.op('pe', lambda e: e.matmul(misc[:, 0:NTT], b2d[:], cst[:, NTT:2 * NTT], start=True, stop=True),
         reads=[b2d, cst], writes=[misc])
    p.op('dve', lambda e: e.tensor_copy(bcol[:], misc[:, 0:NT]), reads=[misc], writes=[bcol])
    for c in range(4):
        for pc in range(S // PIECE):
            pr, ac = pre[(c * (S // PIECE) + pc) % 2], acc[(c * (S // PIECE) + pc) % 2]
            p.dma('sp', pr[:], qkpre_d[c, :, pc * PIECE:pc * PIECE + PIECE + 3], writes=[pr])
            p.op('dve', lambda e, pr=pr, ac=ac, c=c: e.tensor_scalar(ac[:], pr[:, 0:PIECE], cw[:, c, 0:1], None, ALU.mult),
                 reads=[pr, cw], writes=[ac])
            for j in range(1, 4):
                p.op('dve', lambda e, pr=pr, ac=ac, c=c, j=j: e.scalar_tensor_tensor(
                    ac[:], pr[:, j:j + PIECE], cw[:, c, j:j + 1], ac[:], ALU.mult, ALU.add), reads=[pr, cw, ac], writes=[ac])
            p.op('act', lambda e, ac=ac, c=c, pc=pc: e.activation(qk[:, c, pc * PIECE:(pc + 1) * PIECE], ac[:], AF.Silu),
                 reads=[ac], writes=[qk], partial=True)
    nld = 0
    for qb in range(NQB):
        fbi = fb[qb % 2]
        for i4 in range(4):
            tl = 4 * qb + i4
            p.op('pe', lambda e, tl=tl, i4=i4: e.matmul(misc[:, i4 * 128:(i4 + 1) * 128],
                                                         cst[:, 2 * NTT + tl * 128:2 * NTT + (tl + 1) * 128], F2d[:],
                                                         start=True, stop=True),
                 reads=[cst, F2d], writes=[misc], partial=(i4 > 0))
        p.op('act', lambda e, fbi=fbi: e.copy(fbi[:], misc[:]), reads=[misc], writes=[fbi])
        p.dma('pool', bo[qb % 2][:], bo_d[:, :, qb * 512:(qb + 1) * 512].rearrange("c p t -> p c t"), writes=[bo[qb % 2]])
        nkt = 4 * qb + 4
        bvs = {}
        for it in range(nkt + 1):
            if it < nkt:
                kt = it
                s_ps = banks[kt % 2]
                dti = dt_[kt % 2]
                bvb = bvt[nld % 3]
                nld += 1
                bvs[kt] = bvb
                p.dma('sp', bvb[:], bv_d[:, kt, :], writes=[bvb])
                for c in range(2):
                    p.op('pe', lambda e, s_ps=s_ps, c=c, kt=kt, qb=qb: e.matmul(
                        s_ps[:], qk[:, 2 + c, kt * 128:(kt + 1) * 128], qk[:, c, qb * 512:(qb + 1) * 512],
                        start=(c == 0), stop=(c == 1)), reads=[qk], writes=[s_ps], partial=(c > 0))
                p.op('act', lambda e, dti=dti, fbi=fbi, kt=kt: e.activation(dti[:], fbi[:], AF.Exp, bias=bcol[:, kt:kt + 1]),
                     reads=[fbi, bcol], writes=[dti])
                if kt >= 4 * qb:
                    p.op('dve', lambda e, dti=dti, kt=kt, qb=qb: e.tensor_tensor(dti[:], dti[:], mask[:, kt - 4 * qb, :], ALU.mult),
                         reads=[dti, mask], writes=[dti])
            kt = it - 1
            if kt < 0:
                continue
            s_ps = banks[kt % 2]
            dti, ati = dt_[kt % 2], at[kt % 2]
            bvb = bvs[kt]
            p.op('dve', lambda e, s_ps=s_ps, dti=dti, ati=ati: e.scalar_tensor_tensor(
                ati[:], s_ps[:], DK ** -0.5, dti[:], ALU.mult, ALU.mult), reads=[s_ps, dti], writes=[ati])
            first, last = (kt == 0), (kt == nkt - 1)
            for ec in range(4):
                p.op('pe', lambda e, ec=ec, bvb=bvb, ati=ati, first=first, last=last: e.matmul(
                    banks[2 + ec][:], bvb[:, ec * 128:(ec + 1) * 128], ati[:], start=first, stop=last),
                    reads=[bvb, ati], writes=[banks[2 + ec]], partial=not first)
            p.op('pe', lambda e, ati=ati, first=first, last=last: e.matmul(
                banks[6][:], ones_r[:], ati[:], start=first, stop=last), reads=[ones_r, ati], writes=[banks[6]], partial=not first)
        p.op('act', lambda e: e.activation(rden[:], banks[6][:], AF.Abs), reads=[banks[6]], writes=[rden])
        p.op('dve', lambda e: e.tensor_scalar_max(rden[:], rden[:], 1.0), reads=[rden], writes=[rden])
        p.op('dve', lambda e: e.reciprocal(rden[:], rden[:]), reads=[rden], writes=[rden])
        for ec in range(4):
            p.op('dve', lambda e, ec=ec: e.tensor_tensor(hT[:, ec, :], banks[2 + ec][:], rden[:], ALU.mult),
                 reads=[banks[2 + ec], rden], writes=[hT], partial=True)
            sqi = sqh[ec % 2]
            p.op('act', lambda e, ec=ec, sqi=sqi: e.activation(sqi[:], hT[:, ec, :], AF.Square), reads=[hT], writes=[sqi])
            p.op('pe', lambda e, ec=ec: e.matmul(banks[0][:], ones_f[:], hT[:, ec, :], start=(ec == 0), stop=(ec == 3)),
                 reads=[ones_f, hT], writes=[banks[0]], partial=(ec > 0))
            p.op('pe', lambda e, ec=ec, sqi=sqi: e.matmul(banks[1][:], ones_f[:], sqi[:], start=(ec == 0), stop=(ec == 3)),
                 reads=[ones_f, sqi], writes=[banks[1]], partial=(ec > 0))
        emit_ln_stats_finalize(p, banks[0], banks[1], mean, msq, rstd, 512, LN_EPS)
        for ec in range(4):
            tb, sgi, obi = t1[ec % 2], sg[ec % 2], ob[ec % 2]
            p.op('dve', lambda e, tb=tb, ec=ec: e.tensor_tensor(tb[:], hT[:, ec, :], mean[:], ALU.subtract),
                 reads=[hT, mean], writes=[tb])
            p.op('dve', lambda e, tb=tb: e.tensor_tensor(tb[:], tb[:], rstd[:], ALU.mult), reads=[tb, rstd], writes=[tb])
            p.op('act', lambda e, sgi=sgi, ec=ec, qb=qb: e.activation(sgi[:], bo[qb % 2][:, ec, :], AF.Sigmoid),
                 reads=[bo[qb % 2]], writes=[sgi])
            p.op('dve', lambda e, tb=tb, sgi=sgi, obi=obi, ec=ec: e.scalar_tensor_tensor(
                obi[:], tb[:], ng[:, ec:ec + 1], sgi[:], ALU.mult, ALU.mult), reads=[tb, ng, sgi], writes=[obi])
            p.dma('pool', out_d[ec * 128:(ec + 1) * 128, qb * 512:(qb + 1) * 512], obi[:], reads=[obi])
    return ob


def build_mix0(S, NH, with_a=True, with_b=True):
    nc = new_nc()
    NT = S // 128
    qT_d = nc.dram_tensor("aq", [NH, 128, S], F32R, kind="ExternalInput").ap()
    kT_d = nc.dram_tensor("ak", [NH, 128, S], F32R, kind="ExternalInput").ap()
    v_d = nc.dram_tensor("av", [NH, 128, NT, 128], F32R, kind="ExternalInput").ap()
    tbl_d = nc.dram_tensor("atbl", [NH, 128, 23 * 128], F32, kind="ExternalInput").ap()
    ao_d = nc.dram_tensor("ao", [NH * 128, S], F32, kind="ExternalOutput").ap()
    qkpre_d = nc.dram_tensor("bqk", [4, 128, S + 3], F32, kind="ExternalInput").ap()
    cw_d = nc.dram_tensor("bcw", [128, 4, 4], F32, kind="ExternalInput").ap()
    bv_d = nc.dram_tensor("bv", [128, NT, 512], F32R, kind="ExternalInput").ap()
    bo_d = nc.dram_tensor("bo", [4, 128, S], F32, kind="ExternalInput").ap()
    gi_d = nc.dram_tensor("bgi", [NT, 128], F32, kind="ExternalInput").ap()
    gf_d = nc.dram_tensor("bgf", [NT, 128], F32, kind="ExternalInput").ap()
    gb_d = nc.dram_tensor("bgb", [NT, 2], F32, kind="ExternalInput").ap()
    cst_d = nc.dram_tensor("bcst", [NT, 2 * NT + NT * 128], F32, kind="ExternalInput").ap()
    ng_d = nc.dram_tensor("bng", [128, 4], F32, kind="ExternalInput").ap()
    mask_d = nc.dram_tensor("bmask", [128, 4, 512], F32, kind="ExternalInput").ap()
    bout_d = nc.dram_tensor("bout", [512, S], F32, kind="ExternalOutput").ap()
    with contextlib.ExitStack() as es:
        p = Prog(nc, es)
        banks = [p.ps("bank%d" % i, [128, 512]) for i in range(8)]
        ones_r = p.sb("ones_r", [128, 128], F32R)
        ones_f = p.sb("ones_f", [128, 128], F32)
        p.op('dve', lambda e: e.memset(ones_f[:], 1.0), writes=[ones_f])
        p.op('dve', lambda e: e.tensor_copy(ones_r[:], ones_f[:]), reads=[ones_f], writes=[ones_r])
        outs = []
        qk = p.sb('qk', [128, 4, S], F32R)
        scr = {'f': [p.sb('scrf%d' % i, [128, 512]) for i in range(11)],
               'r': [p.sb('scrr%d' % i, [128, 512], F32R) for i in range(4)]}
        if with_a:
            outs += emit_dilattn(p, NH, S, qT_d, kT_d, v_d, tbl_d, ao_d, banks, ones_r, 128 ** -0.5, qk, scr)
        if with_b:
            outs += emit_mlstm(p, S, qkpre_d, cw_d, bv_d, bo_d, gi_d, gf_d, gb_d, ng_d, mask_d, cst_d, bout_d, banks, ones_r, ones_f, qk, scr)
        p.finish('pool', outs)
        p.emit()
    return nc


def causal_mask_4x512():
    s = np.arange(128)[:, None, None]
    i = np.arange(4)[None, :, None]
    t = np.arange(512)[None, None, :]
    return (i * 128 + s <= t).astype(np.float32)


def mlstm_consts(NT):
    U = (np.arange(NT)[:, None] < np.arange(NT)[None, :]).astype(np.float32)
    I = np.eye(NT, dtype=np.float32)
    Sel = np.zeros((NT, NT, 128), np.float32)
    Sel[np.arange(NT), np.arange(NT), :] = 1.0
    return np.concatenate([U, I, Sel.reshape(NT, NT * 128)], axis=1)


MOBA_TW = 35 * 128


def moba_table(slope):
    c = np.arange(MOBA_TW)[None, :]
    jl = np.arange(128)[:, None]
    dist = c - 384 - jl
    return np.where(dist >= 0, -slope * dist, NEG).astype(np.float32)


def moba_consts():
    blk = np.arange(16)[:, None]
    n = np.arange(16)[None, :]
    past = (n < blk)
    pm = np.where(past, 0.0, NEG).reshape(-1)
    p01 = past.astype(np.float64).reshape(-1)
    own = (n == blk).astype(np.float64).reshape(-1)
    row = np.concatenate([pm, p01, own]).astype(np.float32)
    return np.ascontiguousarray(np.concatenate([np.tile(row[None, :], (128, 1)), np.eye(128, dtype=np.float32)], axis=1))


def moba_esel():
    E = np.zeros((16, 16, 128), np.float32)
    E[np.arange(16), np.arange(16), :] = 1.0
    return E.reshape(16, 16 * 128)


def emit_moba(p, NH, S, qT_d, kT_d, v_d, tbl_d, cst_d, esel_d, out_d, banks, ones_r, scale, qk, scr):
    NT = S // 128
    NQB = S // 512
    NB = S // 256
    q = qk.t[:, 0, :]
    k = qk.t[:, 1, :]
    v = qk.t[:, 2, :].rearrange("p (t d) -> p t d", d=128)
    tbl = p.sb('ctbl', [128, MOBA_TW])
    cst = p.sb('ccst', [128, 3 * 256 + 128])
    BF16 = mybir.dt.bfloat16
    esel_f = p.sb('cesel', [16, 16 * 128], F32R)
    esel = p.sb('ceselb', [16, 16 * 128], BF16)
    selT = p.sb('cselT', [16, S], BF16)
    kmean = p.sb('ckmean', [128, NB], F32R)
    ksum = p.sb('cksum', [128, NB])
    gm = [p.sb('cgm%d' % i, [128, 64]) for i in range(4)]
    top8 = [p.sb('ctop%d' % i, [128, 32]) for i in range(4)]
    sel = [p.sb('csel%d' % i, [128, 64]) for i in range(4)]
    gps = [banks[2], banks[3], banks[4], banks[5]]
    tps = [banks[6], banks[7]]
    LA = 2
    sc, pt, ob, rd, ex = scr['f'][0:2] + scr['f'][9:10], scr['r'][0:4], scr['f'][2:4], scr['f'][4], scr['f'][5:8]
    sring = [banks[0], banks[1], banks[6], banks[7], banks[4], banks[5]]
    ident = cst.t[:, 768:896]
    p.dma('pool', cst[:], cst_d, writes=[cst])
    p.dma('pool', esel_f[:], esel_d, writes=[esel_f])
    p.op('dve', lambda e: e.tensor_copy(esel[:], esel_f[:].bitcast(F32)), reads=[esel_f], writes=[esel])
    misc = banks[7]
    for h in range(NH):
        p.dma('sp', q, qT_d[h], writes=[qk])
        p.dma('sp', k, kT_d[h], writes=[qk], partial=True)
        p.dma('sp', v, v_d[h], writes=[qk], partial=True)
        p.dma('sp', tbl[:], tbl_d[h], writes=[tbl])
        p.op('dve', lambda e: e.tensor_reduce(ksum[:], k.bitcast(F32).rearrange("p (n j) -> p n j", j=256), AX.X, ALU.add),
             reads=[qk], writes=[ksum])
        p.op('dve', lambda e: e.tensor_scalar_mul(kmean[:], ksum[:], 1.0 / 256), reads=[ksum], writes=[kmean])
        NG = NT // 4
        for it in range(NG + 2):
            if it < NG:
                G = it
                gp = gps[G % 4]
                for i4 in range(4):
                    qt = 4 * G + i4
                    p.op('pe', lambda e, qt=qt, gp=gp, i4=i4: e.matmul(gp[:, i4 * 16:(i4 + 1) * 16], q[:, qt * 128:(qt + 1) * 128], kmean[:],
                                                                       start=True, stop=True),
                         reads=[qk, kmean], writes=[gp], partial=(i4 > 0))
            G = it - 1
            if 0 <= G < NG:
                gp = gps[G % 4]
                g, t8, sl = gm[G % 4], top8[G % 4], sel[G % 4]

                def tb4(base, G=G):
                    return cst.t[:, base + 2 * G * 16:base + (2 * G + 2) * 16].rearrange("p (b n) -> p b n", b=2) \
                        .unsqueeze(2).broadcast_to([128, 2, 2, 16])

                v4 = lambda ap: ap.rearrange("p (b r n) -> p b r n", b=2, r=2)
                p.op('dve', lambda e, g=g, gp=gp, tb4=tb4, v4=v4: e.tensor_tensor(v4(g[:]), v4(gp[:, 0:64]), tb4(0), ALU.add),
                     reads=[gp, cst], writes=[g])
                for i4 in range(4):
                    p.op('dve', lambda e, g=g, t8=t8, i4=i4: e.max(t8[:, i4 * 8:(i4 + 1) * 8], g[:, i4 * 16:(i4 + 1) * 16]),
                         reads=[g], writes=[t8], partial=(i4 > 0))
                p.op('dve', lambda e, g=g, t8=t8, sl=sl: e.tensor_tensor(
                    sl[:].rearrange("p (t n) -> p t n", n=16), g[:].rearrange("p (t n) -> p t n", n=16),
                    t8[:].rearrange("p (t n) -> p t n", n=8)[:, :, 2:3].broadcast_to([128, 4, 16]), ALU.is_ge),
                    reads=[g, t8], writes=[sl])
                p.op('dve', lambda e, sl=sl, tb4=tb4, v4=v4: e.tensor_tensor(v4(sl[:]), v4(sl[:]), tb4(256), ALU.mult),
                     reads=[sl, cst], writes=[sl])
                p.op('dve', lambda e, sl=sl, tb4=tb4, v4=v4: e.tensor_tensor(v4(sl[:]), v4(sl[:]), tb4(512), ALU.add),
                     reads=[sl, cst], writes=[sl])
            G = it - 2
            if 0 <= G < NG:
                sl = sel[G % 4]
                tp = tps[G % 2]
                for i4 in range(4):
                    p.op('pe', lambda e, sl=sl, tp=tp, i4=i4: e.matmul(tp[0:16, i4 * 128:(i4 + 1) * 128], sl[:, i4 * 16:(i4 + 1) * 16], ident,
                                                                       start=True, stop=True),
                         reads=[sl, cst], writes=[tp], partial=(i4 > 0))
                p.op('dve', lambda e, G=G, tp=tp: e.tensor_scalar(selT[:, G * 512:(G + 1) * 512], tp[0:16, 0:512], 1.0, -NEG / scale,
                                                                 ALU.subtract, ALU.mult),
                     reads=[tp], writes=[selT], partial=True)
        pairs = []
        for qb in range(NQB):
            nkt = 4 * qb + 4
            for kt in range(nkt):
                pairs.append((qb, kt, kt == 0, kt == nkt - 1))
        n = len(pairs)
        nblk = 0
        mbank = {}
        for it in range(n + 4):
            if it < n:
                qb, kt, first, last = pairs[it]
                s_ps = sring[it % 6]
                p.op('pe', lambda e, s_ps=s_ps, kt=kt, qb=qb: e.matmul(
                    s_ps[:], k[:, kt * 128:(kt + 1) * 128], q[:, qb * 512:(qb + 1) * 512], start=True, stop=False),
                    reads=[qk], writes=[s_ps])
                p.op('pe', lambda e, s_ps=s_ps, nb=kt // 2, qb=qb: e.matmul(
                    s_ps[:], esel[:, nb * 128:(nb + 1) * 128], selT[:, qb * 512:(qb + 1) * 512], start=False, stop=True),
                    reads=[esel, selT], writes=[s_ps], partial=True)
            j = it - 4
            if 0 <= j < n:
                qb, kt, first, last = pairs[j]
                s_ps = sring[j % 6]
                sci, pti = sc[j % 3], pt[j % 4]
                o_ps = banks[2]
                d_ps = banks[3]
                off = (4 * qb - kt + 3) * 128
                p.op('dve', lambda e, s_ps=s_ps, sci=sci, off=off: e.scalar_tensor_tensor(
                    sci[:], s_ps[:], scale, tbl[:, off:off + 512], ALU.mult, ALU.add), reads=[s_ps, tbl], writes=[sci])
                p.op('act', lambda e, sci=sci, pti=pti: e.activation(pti[:], sci[:], AF.Exp), reads=[sci], writes=[pti])
                p.op('pe', lambda e, o_ps=o_ps, kt=kt, pti=pti, first=first, last=last: e.matmul(
                    o_ps[:], v[:, kt, :], pti[:], start=first, stop=last), reads=[qk, pti], writes=[o_ps], partial=not first)
                p.op('pe', lambda e, d_ps=d_ps, pti=pti, first=first, last=last: e.matmul(
                    d_ps[:], ones_r[:], pti[:], start=first, stop=last), reads=[ones_r, pti], writes=[d_ps], partial=not first)
                if last:
                    obi = ob[qb % 2]
                    p.op('dve', lambda e, d_ps=d_ps: e.reciprocal(rd[:], d_ps[:]), reads=[d_ps], writes=[rd])
                    p.op('dve', lambda e, o_ps=o_ps, obi=obi: e.tensor_tensor(obi[:], o_ps[:], rd[:], ALU.mult),
                         reads=[o_ps, rd], writes=[obi])
                    p.dma('pool', out_d[h * 128:(h + 1) * 128, qb * 512:(qb + 1) * 512], obi[:], reads=[obi])
    return ob


def emit_conv(p, NTOK, NCH, da_d, db_d, cw_d, lv_d, idn_d, out_d, banks, ones_f, scr):
    W = 31
    HALO = W - 1
    TT = 512
    z = p.sb('dz', [128, NCH, TT])
    da = [p.sb('dda%d' % i, [128, TT + HALO]) for i in range(2)]
    db = [p.sb('ddb%d' % i, [128, TT + HALO]) for i in range(2)]
    gl = [p.sb('dgl%d' % i, [128, TT + HALO], F32R) for i in range(2)]
    dg = [p.sb('ddg%d' % i, [128, W, 128], F32R) for i in range(2)]
    idn = p.sb('didn', [128, 128])
    cw = p.sb('dcw', [128, NCH, W])
    lv = p.sb('dlv', [128, 2, NCH])
    mean = p.sb('dmean', [128, 512])
    rstd = p.sb('drstd', [128, 512])
    msq = scr['f'][4]
    sq, t1, ob = scr['f'][5:7], scr['f'][7:9], scr['f'][2:4]
    zring = [banks[2], banks[3]]
    p.dma('pool', cw[:], cw_d, writes=[cw])
    p.dma('pool', lv[:], lv_d, writes=[lv])
    p.dma('pool', idn[:], idn_d, writes=[idn])
    it = 0
    for hf in range(NTOK // TT):
        s1, s2 = banks[0], banks[1]
        for c in range(NCH):
            a_, b_, g_, d_ = da[it % 2], db[it % 2], gl[it % 2], dg[it % 2]
            z_ps = zring[it % 2]
            it += 1
            p.dma('sp', a_[:], da_d[c, :, hf * TT:hf * TT + TT + HALO], writes=[a_])
            p.dma('sp', b_[:], db_d[c, :, hf * TT:hf * TT + TT + HALO], writes=[b_])
            for j in range(W):
                if j % 3 == 2:
                    p.op('act', lambda e, d_=d_, c=c, j=j: e.activation(d_[:, j, :], idn[:], AF.Identity, scale=cw[:, c, j:j + 1]),
                         reads=[idn, cw], writes=[d_], partial=(j > 0))
                else:
                    p.op('dve', lambda e, d_=d_, c=c, j=j: e.tensor_scalar(d_[:, j, :], idn[:], cw[:, c, j:j + 1], None, ALU.mult),
                         reads=[idn, cw], writes=[d_], partial=(j > 0))
            p.op('act', lambda e, b_=b_: e.activation(b_[:], b_[:], AF.Sigmoid), reads=[b_], writes=[b_])
            p.op('dve', lambda e, a_=a_, b_=b_, g_=g_: e.tensor_tensor(g_[:], a_[:], b_[:], ALU.mult), reads=[a_, b_], writes=[g_])
            for j in range(W):
                p.op('pe', lambda e, z_ps=z_ps, d_=d_, g_=g_, j=j: e.matmul(z_ps[:], d_[:, j, :], g_[:, j:j + TT],
                                                                            start=(j == 0), stop=(j == W - 1)),
                     reads=[d_, g_], writes=[z_ps], partial=(j > 0))
            sqi = sq[c % 2]
            p.op('act', lambda e, z_ps=z_ps, c=c: e.copy(z[:, c, :], z_ps[:]), reads=[z_ps], writes=[z], partial=True)
            p.op('act', lambda e, sqi=sqi, z_ps=z_ps: e.activation(sqi[:], z_ps[:], AF.Square), reads=[z_ps], writes=[sqi])
            p.op('pe', lambda e, c=c: e.matmul(s1[:], ones_f[:], z[:, c, :], start=(c == 0), stop=(c == NCH - 1)),
                 reads=[ones_f, z], writes=[s1], partial=(c > 0))
            p.op('pe', lambda e, c=c, sqi=sqi: e.matmul(s2[:], ones_f[:], sqi[:], start=(c == 0), stop=(c == NCH - 1)),
                 reads=[ones_f, sqi], writes=[s2], partial=(c > 0))
        emit_ln_stats_finalize(p, s1, s2, mean, msq, rstd, NCH * 128, LN_EPS)
        for c in range(NCH):
            tb, obi = t1[c % 2], ob[c % 2]
            p.op('dve', lambda e, tb=tb, c=c: e.tensor_tensor(tb[:], z[:, c, :], mean[:], ALU.subtract),
                 reads=[z, mean], writes=[tb])
            p.op('dve', lambda e, tb=tb: e.tensor_tensor(tb[:], tb[:], rstd[:], ALU.mult), reads=[tb, rstd], writes=[tb])
            p.op('act', lambda e, tb=tb, obi=obi, c=c: e.activation(obi[:], tb[:], AF.Silu, bias=lv[:, 1, c:c + 1], scale=lv[:, 0, c:c + 1]),
                 reads=[tb, lv], writes=[obi])
            p.dma('pool', out_d[c * 128:(c + 1) * 128, hf * TT:(hf + 1) * TT], obi[:], reads=[obi])
    return ob


def build_mix1(S, NH, NTOK, NCH, with_c=True, with_d=True):
    nc = new_nc()
    NT = S // 128
    qT_d = nc.dram_tensor("cq", [NH, 128, S], F32R, kind="ExternalInput").ap()
    kT_d = nc.dram_tensor("ck", [NH, 128, S], F32R, kind="ExternalInput").ap()
    v_d = nc.dram_tensor("cv", [NH, 128, NT, 128], F32R, kind="ExternalInput").ap()
    tbl_d = nc.dram_tensor("ctbl", [NH, 128, MOBA_TW], F32, kind="ExternalInput").ap()
    cst_d = nc.dram_tensor("ccst", [128, 3 * 256 + 128], F32, kind="ExternalInput").ap()
    esel_d = nc.dram_tensor("cesel", [16, 16 * 128], F32R, kind="ExternalInput").ap()
    co_d = nc.dram_tensor("co", [NH * 128, S], F32, kind="ExternalOutput").ap()
    da_d = nc.dram_tensor("dda", [NCH, 128, NTOK + 30], F32, kind="ExternalInput").ap()
    db_d = nc.dram_tensor("ddb", [NCH, 128, NTOK + 30], F32, kind="ExternalInput").ap()
    cw_d = nc.dram_tensor("dcw", [128, NCH, 31], F32, kind="ExternalInput").ap()
    lv_d = nc.dram_tensor("dlv", [128, 2, NCH], F32, kind="ExternalInput").ap()
    do_d = nc.dram_tensor("dout", [NCH * 128, NTOK], F32, kind="ExternalOutput").ap()
    with contextlib.ExitStack() as es:
        p = Prog(nc, es)
        banks = [p.ps("bank%d" % i, [128, 512]) for i in range(8)]
        ones_r = p.sb("ones_r", [128, 128], F32R)
        ones_f = p.sb("ones_f", [128, 128], F32)
        p.op('dve', lambda e: e.memset(ones_f[:], 1.0), writes=[ones_f])
        p.op('dve', lambda e: e.tensor_copy(ones_r[:], ones_f[:]), reads=[ones_f], writes=[ones_r])
        qk = p.sb('qk', [128, 3, S], F32R)
        scr = {'f': [p.sb('scrf%d' % i, [128, 512]) for i in range(11)],
               'r': [p.sb('scrr%d' % i, [128, 512], F32R) for i in range(4)]}
        outs = []
        if with_d:
            outs += emit_conv(p, NTOK, NCH, da_d, db_d, cw_d, lv_d, cst_d[:, 768:896], do_d, banks, ones_f, scr)
        if with_c:
            outs += emit_moba(p, NH, S, qT_d, kT_d, v_d, tbl_d, cst_d, esel_d, co_d, banks, ones_r, 128 ** -0.5, qk, scr)
        p.finish('pool', outs)
        p.emit()
    return nc


TOK_PER_CORE = BATCH * SEQ // NCORES
CORES_PER_BATCH = NCORES // BATCH
FFN_T = 352
FFN_TILES = [(0, 352), (352, 352), (704, 320)]
PROJ_T = 512
OUT_T = 512


def _launch(nc, in_maps):
    res = run_bass_kernel_spmd(nc, in_maps, core_ids=list(range(NCORES)))
    return res.results


def _vecs(mod_l, b, j, ln_g, ln_b):
    return np.ascontiguousarray(np.stack([fm_vec(np.ascontiguousarray(v)) for v in
                                          (mod_l[b, 3 * j], mod_l[b, 3 * j + 1], mod_l[b, 3 * j + 2], ln_g[j], ln_b[j])], axis=1))


def _alibi(n):
    return 2.0 ** (-8.0 * np.arange(1, n + 1) / n)


def _run_ffn(xTs, mod_l, j, ln_g, ln_b, w_up, w_dn):
    wu = tile_w(np.asarray(w_up), 32)
    wd = tile_w(np.asarray(w_dn), 32)
    nc = build_ffn(D_MODEL, D_FF, TOK_PER_CORE, FFN_T, res_w=0.5, tiles=FFN_TILES)
    maps = [{"xT": xTs[c], "vecs": _vecs(mod_l, c // CORES_PER_BATCH, j, ln_g, ln_b), "w_up": wu, "w_dn": wd}
            for c in range(NCORES)]
    out = _launch(nc, maps)
    return [np.ascontiguousarray(o["oT"]) for o in out]


def _run_proj(xTs, mod_l, ln_g, ln_b, w_in):
    w_in = np.asarray(w_in)
    ncol = w_in.shape[1]
    nch = -(-ncol // 128)
    if nch * 128 != ncol:
        w_in = np.concatenate([w_in, np.zeros((w_in.shape[0], nch * 128 - ncol), np.float32)], axis=1)
    wt = tile_w(w_in, 32)
    nc = build_proj(D_MODEL, nch, TOK_PER_CORE, PROJ_T)
    maps = [{"xT": xTs[c], "vecs": _vecs(mod_l, c // CORES_PER_BATCH, 1, ln_g, ln_b), "w_in": wt} for c in range(NCORES)]
    out = _launch(nc, maps)
    return [np.concatenate([out[b * CORES_PER_BATCH + r]["oT"] for r in range(CORES_PER_BATCH)], axis=1) for b in range(BATCH)]


def _run_out(xTs, catTs, mod_l, ln_g, ln_b, w_out):
    wo = tile_w(np.asarray(w_out), 32)
    nc = build_out(D_MODEL, TOK_PER_CORE, OUT_T, res_w=1.0)
    maps = []
    for c in range(NCORES):
        b, r = divmod(c, CORES_PER_BATCH)
        maps.append({"xT": xTs[c], "cT": np.ascontiguousarray(catTs[b][:, r * TOK_PER_CORE:(r + 1) * TOK_PER_CORE]),
                     "vecs": _vecs(mod_l, b, 1, ln_g, ln_b), "w_o": wo})
    out = _launch(nc, maps)
    return [np.ascontiguousarray(o["oT"]) for o in out]


def _heads_T(pT, row0, nh):
    return np.ascontiguousarray(pT[row0:row0 + nh * 128].reshape(nh, 128, SEQ))


def _heads_V(pT, row0, nh):
    vt = pT[row0:row0 + nh * 128].reshape(nh, 128, SEQ // 128, 128)
    return np.ascontiguousarray(vt.transpose(0, 3, 2, 1))


def _run_mix0(projT, conv_qk, b_igate, b_fgate, norm_g):
    NH = 4
    slopes = _alibi(16)
    conv_qk = np.asarray(conv_qk)
    norm_g = np.asarray(norm_g)
    nc = build_mix0(SEQ, NH)
    mask = causal_mask_4x512()
    cst = mlstm_consts(SEQ // 128)
    maps = []
    for c in range(NCORES):
        b, r = divmod(c, CORES_PER_BATCH)
        pT = projT[b]
        m = {}
        m["aq"] = _heads_T(pT, 4 * r * 128, NH)
        m["ak"] = _heads_T(pT, 2048 + 4 * r * 128, NH)
        m["av"] = _heads_V(pT, 4096 + 4 * r * 128, NH)
        m["atbl"] = np.stack([dil_table(slopes[4 * r + i]) for i in range(NH)])
        pre = np.concatenate([pT[6144 + r * 256:6144 + (r + 1) * 256], pT[7168 + r * 256:7168 + (r + 1) * 256]], axis=0)
        pre = np.concatenate([np.zeros((512, 3), np.float32), pre], axis=1)
        m["bqk"] = np.ascontiguousarray(pre.reshape(4, 128, SEQ + 3))
        cw = np.concatenate([conv_qk[:, r * 256:(r + 1) * 256], conv_qk[:, 1024 + r * 256:1024 + (r + 1) * 256]], axis=1)
        m["bcw"] = np.ascontiguousarray(cw.T.reshape(4, 128, 4).transpose(1, 0, 2))
        vT = pT[8192 + r * 512:8192 + (r + 1) * 512]
        m["bv"] = np.ascontiguousarray(vT.reshape(512, SEQ // 128, 128).transpose(2, 1, 0))
        m["bo"] = np.ascontiguousarray(pT[10240 + r * 512:10240 + (r + 1) * 512].reshape(4, 128, SEQ))
        m["bgi"] = np.ascontiguousarray(pT[12288 + r].reshape(SEQ // 128, 128))
        m["bgf"] = np.ascontiguousarray(pT[12292 + r].reshape(SEQ // 128, 128))
        m["bgb"] = np.ascontiguousarray(np.tile(np.array([[b_igate[r], b_fgate[r]]], np.float32), (SEQ // 128, 1)))
        m["bng"] = fm_vec(np.ascontiguousarray(norm_g[r * 512:(r + 1) * 512]))
        m["bmask"] = mask
        m["bcst"] = cst
        maps.append(m)
    out = _launch(nc, maps)
    catTs = [np.empty((D_MODEL, SEQ), np.float32) for _ in range(BATCH)]
    for c in range(NCORES):
        b, r = divmod(c, CORES_PER_BATCH)
        catTs[b][4 * r * 128:(4 * r + 4) * 128] = out[c]["ao"]
        catTs[b][2048 + r * 512:2048 + (r + 1) * 512] = out[c]["bout"]
    return catTs


def _run_mix1(projT, conv_dw, conv_ln_g, conv_ln_b):
    NH = 4
    slopes = _alibi(16)
    conv_dw = np.asarray(conv_dw)
    nc = build_mix1(SEQ, NH, TOK_PER_CORE, 16)
    cst = moba_consts()
    esel = moba_esel()
    dcw = np.ascontiguousarray(conv_dw.T.reshape(16, 128, 31).transpose(1, 0, 2))
    dlv = np.ascontiguousarray(np.stack([fm_vec(np.asarray(conv_ln_g)), fm_vec(np.asarray(conv_ln_b))], axis=1))
    maps = []
    for c in range(NCORES):
        b, r = divmod(c, CORES_PER_BATCH)
        pT = projT[b]
        m = {}
        m["cq"] = _heads_T(pT, 4 * r * 128, NH)
        m["ck"] = _heads_T(pT, 2048 + 4 * r * 128, NH)
        m["cv"] = _heads_V(pT, 4096 + 4 * r * 128, NH)
        m["ctbl"] = np.stack([moba_table(slopes[4 * r + i]) for i in range(NH)])
        m["ccst"] = cst
        m["cesel"] = esel
        t0 = r * TOK_PER_CORE
        for name, row0 in (("dda", 6144), ("ddb", 8192)):
            blk = np.zeros((2048, TOK_PER_CORE + 30), np.float32)
            lo = max(0, t0 - 30)
            blk[:, 30 - (t0 - lo):] = pT[row0:row0 + 2048, lo:t0 + TOK_PER_CORE]
            m[name] = np.ascontiguousarray(blk.reshape(16, 128, TOK_PER_CORE + 30))
        m["dcw"] = dcw
        m["dlv"] = dlv
        maps.append(m)
    out = _launch(nc, maps)
    catTs = [np.empty((D_MODEL, SEQ), np.float32) for _ in range(BATCH)]
    for c in range(NCORES):
        b, r = divmod(c, CORES_PER_BATCH)
        catTs[b][4 * r * 128:(4 * r + 4) * 128] = out[c]["co"]
        catTs[b][2048:4096, r * TOK_PER_CORE:(r + 1) * TOK_PER_CORE] = out[c]["dout"]
    return catTs


def _run_ada(c, w_adas, b_adas):
    ncol = 9 * D_MODEL // NCORES
    cT = np.ascontiguousarray(np.asarray(c).T.reshape(D_MODEL // 128, 128, BATCH).transpose(1, 0, 2))
    nc = build_ada(D_MODEL, ncol, len(w_adas), BATCH)
    maps = []
    for k in range(NCORES):
        m = {"cT": cT}
        for l in range(len(w_adas)):
            m["w%d" % l] = np.ascontiguousarray(np.asarray(w_adas[l])[:, k * ncol:(k + 1) * ncol])
            m["b%d" % l] = np.ascontiguousarray(np.broadcast_to(np.asarray(b_adas[l])[k * ncol:(k + 1) * ncol], (BATCH, ncol)))
        maps.append(m)
    out = _launch(nc, maps)
    mods = []
    for l in range(len(w_adas)):
        full = np.concatenate([out[k]["m%d" % l] for k in range(NCORES)], axis=1)
        mods.append(full.reshape(BATCH, 9, D_MODEL))
    return mods


def _pad_w_in(w_in):
    w_in = np.asarray(w_in)
    ncol = w_in.shape[1]
    nch = -(-ncol // 128)
    if nch * 128 != ncol:
        w_in = np.concatenate([w_in, np.zeros((w_in.shape[0], nch * 128 - ncol), np.float32)], axis=1)
    return tile_w(w_in, 32), nch


def _run_chain(xTs, stages):
    spec = []
    shared = {}
    for i, st in enumerate(stages):
        if st['kind'] == 'proj':
            wt, nch = _pad_w_in(st['w_in'])
            shared["w_in%d" % i] = wt
            spec.append(('proj', nch))
        elif st['kind'] == 'out':
            shared["w_o%d" % i] = tile_w(np.asarray(st['w_out']), 32)
            spec.append(('out', None))
        else:
            shared["w_up%d" % i] = tile_w(np.asarray(st['w_up']), 32)
            shared["w_dn%d" % i] = tile_w(np.asarray(st['w_dn']), 32)
            spec.append(('ffn', None))
    nc = build_chain(D_MODEL, D_FF, TOK_PER_CORE, spec, OUT_T, FFN_T, FFN_TILES, PROJ_T)
    maps = []
    for c in range(NCORES):
        b, r = divmod(c, CORES_PER_BATCH)
        m = dict(shared)
        m["xT"] = xTs[c]
        for i, st in enumerate(stages):
            m["vecs%d" % i] = _vecs(st['mod_l'], b, st['j'], st['ln_g'], st['ln_b'])
            if st['kind'] == 'out':
                m["cT%d" % i] = np.ascontiguousarray(st['catTs'][b][:, r * TOK_PER_CORE:(r + 1) * TOK_PER_CORE])
        maps.append(m)
    out = _launch(nc, maps)
    xprod = [i for i, st in enumerate(stages) if st['kind'] != 'proj']
    xo = [np.ascontiguousarray(o["oT%d" % xprod[-1]]) for o in out]
    projT = None
    for i, st in enumerate(stages):
        if st['kind'] == 'proj':
            projT = [np.concatenate([out[b * CORES_PER_BATCH + r]["pT%d" % i] for r in range(CORES_PER_BATCH)], axis=1)
                     for b in range(BATCH)]
    return xo, projT


def kernel(x, c,
           l0_w_ada, l0_b_ada, l0_ln_g, l0_ln_b, l0_ffn1_w_up, l0_ffn1_w_down, l0_ffn2_w_up, l0_ffn2_w_down,
           l0_w_in, l0_conv_qk, l0_b_igate, l0_b_fgate, l0_mlstm_norm_g, l0_w_out,
           l1_w_ada, l1_b_ada, l1_ln_g, l1_ln_b, l1_ffn1_w_up, l1_ffn1_w_down, l1_ffn2_w_up, l1_ffn2_w_down,
           l1_w_in, l1_conv_dw, l1_conv_ln_g, l1_conv_ln_b, l1_w_out):
    x = np.asarray(x, dtype=np.float32)
    xTs = []
    for k in range(NCORES):
        b, r = divmod(k, CORES_PER_BATCH)
        xTs.append(np.ascontiguousarray(x[b, r * TOK_PER_CORE:(r + 1) * TOK_PER_CORE, :].T))
    mods = _run_ada(c, [l0_w_ada, l1_w_ada], [l0_b_ada, l1_b_ada])
    g0, b0, g1, b1 = (np.asarray(v) for v in (l0_ln_g, l0_ln_b, l1_ln_g, l1_ln_b))

    def st(kind, l, j, **kw):
        d = {'kind': kind, 'mod_l': mods[l], 'j': j, 'ln_g': (g0, g1)[l], 'ln_b': (b0, b1)[l]}
        d.update(kw)
        return d

    xTs, projT = _run_chain(xTs, [st('ffn', 0, 0, w_up=l0_ffn1_w_up, w_dn=l0_ffn1_w_down), st('proj', 0, 1, w_in=l0_w_in)])
    catTs = _run_mix0(projT, l0_conv_qk, np.asarray(l0_b_igate), np.asarray(l0_b_fgate), l0_mlstm_norm_g)
    del projT
    xTs, projT = _run_chain(xTs, [st('out', 0, 1, w_out=l0_w_out, catTs=catTs),
                                  st('ffn', 0, 2, w_up=l0_ffn2_w_up, w_dn=l0_ffn2_w_down),
                                  st('ffn', 1, 0, w_up=l1_ffn1_w_up, w_dn=l1_ffn1_w_down),
                                  st('proj', 1, 1, w_in=l1_w_in)])
    del catTs
    catTs = _run_mix1(projT, l1_conv_dw, l1_conv_ln_g, l1_conv_ln_b)
    del projT
    xTs, _ = _run_chain(xTs, [st('out', 1, 1, w_out=l1_w_out, catTs=catTs),
                              st('ffn', 1, 2, w_up=l1_ffn2_w_up, w_dn=l1_ffn2_w_down)])
    out = np.empty((BATCH, SEQ, D_MODEL), np.float32)
    for k in range(NCORES):
        b, r = divmod(k, CORES_PER_BATCH)
        out[b, r * TOK_PER_CORE:(r + 1) * TOK_PER_CORE, :] = xTs[k].T
    return out
```

```python
import contextlib
import numpy as np
import concourse.bass as bass
import concourse.mybir as mybir
from concourse.bass_utils import run_bass_kernel_spmd

F32 = mybir.dt.float32
F32R = mybir.dt.float32r
AF = mybir.ActivationFunctionType
ALU = mybir.AluOpType
AX = mybir.AxisListType
ENGS = ('pe', 'act', 'dve', 'pool', 'sp')

D_MODEL = 4096
SEQ = 4096
BATCH = 2
DEPTH = 2
D_FF = 2 * D_MODEL
ALPHA = (2 * DEPTH) ** 0.25
LN_EPS = 1e-5
NCORES = 8


class Buf:
    __slots__ = ('t', 'w', 'r', 'dsem', 'dcnt', 'name')

    def __init__(self, t, name):
        self.t = t
        self.name = name
        self.w = {}
        self.r = {}
        self.dsem = None
        self.dcnt = 0

    def __getitem__(self, k):
        return self.t[k]


class Prog:
    def __init__(self, nc, es):
        self.nc = nc
        self.top = es
        self.es = es
        self.q = {e: [] for e in ENGS}
        self.sem = {e: es.enter_context(nc.semaphore('c_' + e)) for e in ENGS}
        self.cnt = {e: 0 for e in ENGS}
        self.seen = {}
        self.ndsem = 0
        self.dpool = []
        self.dall = []
        self.pbufs = []
        self.prefix = ''

    def sb(self, name, shape, dt=F32):
        b = Buf(self.es.enter_context(self.nc.sbuf_tensor('s_' + self.prefix + name, list(shape), dt)), name)
        self.pbufs.append(b)
        return b

    def ps(self, name, shape, dt=F32):
        b = Buf(self.es.enter_context(self.nc.psum_tensor('p_' + self.prefix + name, list(shape), dt)), name)
        self.pbufs.append(b)
        return b

    def begin_phase(self, prefix):
        self.prefix = prefix
        self.es = contextlib.ExitStack()
        self.pbufs = []

    def end_phase(self):
        toks = [(self.sem[o], self.cnt[o], 'bar') for o in ENGS if self.cnt[o] > 0]
        toks += [(r[0], r[1], 'bar') for r in self.dall if r[1] > 0]
        for e in ENGS:
            self._waits(e, toks)
        self.emit()
        self.q = {e: [] for e in ENGS}
        for b in self.pbufs:
            if b.dsem is not None:
                self.dpool.append(b.dsem)
                b.dsem = None
        self.pbufs = []
        self.es.close()
        self.es = self.top
        self.prefix = ''

    def dram(self, name, shape, dt=F32, kind="Internal"):
        return Buf(self.nc.dram_tensor(name, list(shape), dt, kind=kind).ap(), name)

    def _waits(self, eng, toks):
        for sem, val, src in toks:
            if src == 'pe' and eng == 'pe':
                continue
            key = (eng, id(sem))
            if self.seen.get(key, 0) >= val:
                continue
            self.seen[key] = val
            self.q[eng].append(lambda e, sem=sem, val=val: e.wait_ge(sem, val))

    @staticmethod
    def _upd(d, tok):
        k = id(tok[0])
        if k not in d or d[k][1] < tok[1]:
            d[k] = tok

    @staticmethod
    def _deps(reads, writes):
        toks = []
        for b in reads:
            toks += list(b.w.values())
        for b in writes:
            toks += list(b.w.values()) + list(b.r.values())
        return toks

    def _record(self, tok, reads, writes, partial):
        for b in reads:
            self._upd(b.r, tok)
        for b in writes:
            if partial:
                self._upd(b.w, tok)
            else:
                b.w = {id(tok[0]): tok}
                b.r = {}

    def op(self, eng, fn, reads=(), writes=(), partial=False):
        self._waits(eng, self._deps(reads, writes))
        self.cnt[eng] += 1
        n = self.cnt[eng]
        sem = self.sem[eng]
        self.q[eng].append(lambda e: fn(e).then_inc(sem, 1))
        self._record((sem, n, eng), reads, writes, partial)

    def dma(self, eng, out, in_, reads=(), writes=(), partial=False, owner=None):
        self._waits(eng, self._deps(reads, writes))
        b = owner if owner is not None else (list(writes) + list(reads))[0]
        if b.dsem is None:
            if self.dpool:
                b.dsem = self.dpool.pop()
            else:
                b.dsem = [self.top.enter_context(self.nc.semaphore('d%d' % self.ndsem)), 0]
                self.ndsem += 1
                self.dall.append(b.dsem)
        b.dsem[1] += 16
        sem, val = b.dsem[0], b.dsem[1]
        self.q[eng].append(lambda e: e.dma_start(out=out, in_=in_).then_inc(sem, 16))
        self._record((sem, val, 'dma'), reads, writes, partial)

    def finish(self, eng, bufs):
        toks = []
        for b in bufs:
            toks += list(b.w.values()) + list(b.r.values())
        self._waits(eng, toks)

    def emit(self):
        q = self.q
        with self.nc.Block() as block:
            @block.tensor
            def _(e):
                for f in q['pe']:
                    f(e)

            @block.scalar
            def _(e):
                for f in q['act']:
                    f(e)

            @block.vector
            def _(e):
                for f in q['dve']:
                    f(e)

            @block.gpsimd
            def _(e):
                for f in q['pool']:
                    f(e)

            @block.sync
            def _(e):
                for f in q['sp']:
                    f(e)


def new_nc():
    nc = bass.Bass("TRN2", target_bir_lowering=False)
    nc.dge_precook = False
    return nc


class Ring:
    def __init__(self, p, name, n, width):
        self.p = p
        self.slots = [p.sb('%s%d' % (name, i), [128, width], F32R) for i in range(n)]
        self.i = 0

    def load(self, src_ap):
        b = self.slots[self.i % len(self.slots)]
        self.i += 1
        self.p.dma('sp', b[:], src_ap, writes=[b])
        return b


def tile_w(W, kcu):
    K, N = W.shape
    H = K // (128 * kcu)
    t = W.reshape(H, kcu, 128, N // 128, 128).transpose(3, 0, 2, 1, 4)
    return np.ascontiguousarray(t).reshape(N // 128, H, 128, kcu * 128)


def fm_vec(v):
    return np.ascontiguousarray(v.reshape(-1, 128).T)


def emit_ffn(p, D, DFF, NTOK, T, xT, vecs, w_up, w_dn, oT, res_w=0.5, tiles=None):
    KC = D // 128
    FC = DFF // 128
    KCU = min(32, KC)
    HU = KC // KCU
    HD = FC // KCU
    if tiles is None:
        tiles = [(t0, min(T, NTOK - t0)) for t0 in range(0, NTOK, T)]
    xv = xT.rearrange("(c p) t -> p c t", p=128)
    xvr = xT.bitcast(F32R).rearrange("(c p) t -> p c t", p=128)
    ov = oT.rearrange("(c p) t -> p c t", p=128)
    eps2 = LN_EPS / (ALPHA * ALPHA)
    u = p.sb("u", [128, KC, T], F32R)
    a = p.sb("a", [128, FC, T], F32R)
    ring = Ring(p, "wr", 3, KCU * 128)
    vs = p.sb("vs", [128, 5, KC])
    sc1 = p.sb("sc1", [128, KC])
    gwa = p.sb("gwa", [128, KC])
    ones = p.sb("ones", [128, 128])
    sil = [p.sb("sil%d" % i, [128, T]) for i in range(2)]
    xc = [p.sb("xc%d" % i, [128, T]) for i in range(2)]
    sq = [p.sb("sq%d" % i, [128, T]) for i in range(2)]
    oc = [p.sb("oc%d" % i, [128, T]) for i in range(2)]
    t1 = [p.sb("t1%d" % i, [128, T]) for i in range(2)]
    mean = p.sb("mean", [128, T])
    msq = p.sb("msq", [128, T])
    rstd = p.sb("rstd", [128, T])
    pg = [p.ps("pg%d" % i, [128, T]) for i in range(2)]
    pv = [p.ps("pv%d" % i, [128, T]) for i in range(2)]
    py = [p.ps("py%d" % i, [128, T]) for i in range(2)]
    s1 = p.ps("s1", [128, T])
    s2 = p.ps("s2", [128, T])

    p.dma('pool', vs[:], vecs, writes=[vs])
    p.op('dve', lambda e: e.memset(ones[:], 1.0), writes=[ones])
    p.op('dve', lambda e: e.tensor_scalar_add(sc1[:], vs[:, 1, :], 1.0), reads=[vs], writes=[sc1])
    p.op('dve', lambda e: e.tensor_scalar(gwa[:], vs[:, 2, :], 1.0, res_w / ALPHA, ALU.add, ALU.mult),
         reads=[vs], writes=[gwa])

    for (t0, tt) in tiles:
        p.dma('pool', u[:, :, 0:tt], xvr[:, :, t0:t0 + tt], writes=[u])
        for c in range(KC):
            p.op('dve', lambda e, c=c, tt=tt: e.tensor_scalar(u[:, c, 0:tt], u[:, c, 0:tt].bitcast(F32), sc1[:, c:c + 1],
                                                              vs[:, 0, c:c + 1], ALU.mult, ALU.add),
                 reads=[u, sc1, vs], writes=[u], partial=True)
        for f in range(FC):
            g_ps, v_ps = pg[f % 2], pv[f % 2]
            for (n, ps) in ((f, g_ps), (FC + f, v_ps)):
                for h in range(HU):
                    wb = ring.load(w_up[n, h])
                    for kc in range(KCU):
                        k = h * KCU + kc
                        p.op('pe', lambda e, ps=ps, wb=wb, kc=kc, k=k, tt=tt: e.matmul(
                            ps[:, 0:tt], wb[:, kc * 128:(kc + 1) * 128], u[:, k, 0:tt], start=(k == 0), stop=(k == KC - 1)),
                            reads=[wb, u], writes=[ps], partial=(k > 0))
            sb_ = sil[f % 2]
            p.op('act', lambda e, sb_=sb_, g_ps=g_ps, tt=tt: e.activation(sb_[:, 0:tt], g_ps[:, 0:tt], AF.Silu),
                 reads=[g_ps], writes=[sb_])
            p.op('dve', lambda e, sb_=sb_, v_ps=v_ps, f=f, tt=tt: e.tensor_tensor(a[:, f, 0:tt], sb_[:, 0:tt], v_ps[:, 0:tt], ALU.mult),
                 reads=[sb_, v_ps], writes=[a], partial=True)
        for n in range(KC):
            ps = py[n % 2]
            for h in range(HD):
                wb = ring.load(w_dn[n, h])
                for kc in range(KCU):
                    k = h * KCU + kc
                    p.op('pe', lambda e, ps=ps, wb=wb, kc=kc, k=k, tt=tt: e.matmul(
                        ps[:, 0:tt], wb[:, kc * 128:(kc + 1) * 128], a[:, k, 0:tt], start=(k == 0), stop=(k == FC - 1)),
                        reads=[wb, a], writes=[ps], partial=(k > 0))
            xb = xc[n % 2]
            p.dma('pool', xb[:, 0:tt], xv[:, n, t0:t0 + tt], writes=[xb])
            p.op('dve', lambda e, ps=ps, xb=xb, n=n, tt=tt: e.scalar_tensor_tensor(
                u[:, n, 0:tt], ps[:, 0:tt], gwa[:, n:n + 1], xb[:, 0:tt], ALU.mult, ALU.add),
                reads=[ps, xb, gwa], writes=[u], partial=True)
            qb = sq[n % 2]
            p.op('act', lambda e, qb=qb, n=n, tt=tt: e.activation(qb[:, 0:tt], u[:, n, 0:tt].bitcast(F32), AF.Square),
                 reads=[u], writes=[qb])
            p.op('pe', lambda e, n=n, tt=tt: e.matmul(s1[:, 0:tt], ones[:], u[:, n, 0:tt].bitcast(F32),
                                                      start=(n == 0), stop=(n == KC - 1)),
                 reads=[ones, u], writes=[s1], partial=(n > 0))
            p.op('pe', lambda e, n=n, qb=qb, tt=tt: e.matmul(s2[:, 0:tt], ones[:], qb[:, 0:tt], start=(n == 0), stop=(n == KC - 1)),
                 reads=[ones, qb], writes=[s2], partial=(n > 0))
        p.op('dve', lambda e, tt=tt: e.tensor_scalar_mul(mean[:, 0:tt], s1[:, 0:tt], 1.0 / D), reads=[s1], writes=[mean])
        p.op('dve', lambda e, tt=tt: e.tensor_tensor(msq[:, 0:tt], mean[:, 0:tt], mean[:, 0:tt], ALU.mult), reads=[mean], writes=[msq])
        p.op('dve', lambda e, tt=tt: e.scalar_tensor_tensor(rstd[:, 0:tt], s2[:, 0:tt], 1.0 / D, msq[:, 0:tt], ALU.mult, ALU.subtract),
             reads=[s2, msq], writes=[rstd])
        p.op('dve', lambda e, tt=tt: e.tensor_scalar_add(rstd[:, 0:tt], rstd[:, 0:tt], eps2), reads=[rstd], writes=[rstd])
        p.op('act', lambda e, tt=tt: e.activation(rstd[:, 0:tt], rstd[:, 0:tt], AF.Sqrt), reads=[rstd], writes=[rstd])
        p.op('dve', lambda e, tt=tt: e.reciprocal(rstd[:, 0:tt], rstd[:, 0:tt]), reads=[rstd], writes=[rstd])
        for n in range(KC):
            tb, ob = t1[n % 2], oc[n % 2]
            p.op('dve', lambda e, tb=tb, n=n, tt=tt: e.tensor_tensor(tb[:, 0:tt], u[:, n, 0:tt].bitcast(F32), mean[:, 0:tt], ALU.subtract),
                 reads=[u, mean], writes=[tb])
            p.op('dve', lambda e, tb=tb, tt=tt: e.tensor_tensor(tb[:, 0:tt], tb[:, 0:tt], rstd[:, 0:tt], ALU.mult),
                 reads=[tb, rstd], writes=[tb])
            p.op('act', lambda e, tb=tb, ob=ob, n=n, tt=tt: e.activation(ob[:, 0:tt], tb[:, 0:tt], AF.Identity,
                                                                       bias=vs[:, 4, n:n + 1], scale=vs[:, 3, n:n + 1]),
                 reads=[tb, vs], writes=[ob])
            p.dma('pool', ov[:, n, t0:t0 + tt], ob[:, 0:tt], reads=[ob])
    return oc


def build_ffn(D, DFF, NTOK, T, res_w=0.5, tiles=None):
    KC = D // 128
    FC = DFF // 128
    KCU = min(32, KC)
    nc = new_nc()
    xT = nc.dram_tensor("xT", [D, NTOK], F32, kind="ExternalInput").ap()
    vecs = nc.dram_tensor("vecs", [128, 5, KC], F32, kind="ExternalInput").ap()
    w_up = nc.dram_tensor("w_up", [2 * FC, KC // KCU, 128, KCU * 128], F32R, kind="ExternalInput").ap()
    w_dn = nc.dram_tensor("w_dn", [KC, FC // KCU, 128, KCU * 128], F32R, kind="ExternalInput").ap()
    oT = nc.dram_tensor("oT", [D, NTOK], F32, kind="ExternalOutput").ap()
    with contextlib.ExitStack() as es:
        p = Prog(nc, es)
        oc = emit_ffn(p, D, DFF, NTOK, T, xT, vecs, w_up, w_dn, oT, res_w, tiles)
        p.finish('pool', oc)
        p.emit()
    return nc


def ffn_inputs(xT_core, shift, scale, gate, ln_g, ln_b, w_up_t, w_dn_t):
    vecs = np.ascontiguousarray(np.stack([fm_vec(v) for v in (shift, scale, gate, ln_g, ln_b)], axis=1))
    return {"xT": np.ascontiguousarray(xT_core), "vecs": vecs, "w_up": w_up_t, "w_dn": w_dn_t}


def emit_gemm(p, ring, w_ap, n, HU, KCU, src, ps, KC):
    for h in range(HU):
        wb = ring.load(w_ap[n, h])
        for kc in range(KCU):
            k = h * KCU + kc
            p.op('pe', lambda e, ps=ps, wb=wb, kc=kc, k=k: e.matmul(
                ps[:], wb[:, kc * 128:(kc + 1) * 128], src[:, k, :], start=(k == 0), stop=(k == KC - 1)),
                reads=[wb, src], writes=[ps], partial=(k > 0))


def emit_ln_stats_finalize(p, s1, s2, mean, msq, rstd, nfeat, eps):
    p.op('dve', lambda e: e.tensor_scalar_mul(mean[:], s1[:], 1.0 / nfeat), reads=[s1], writes=[mean])
    p.op('dve', lambda e: e.tensor_tensor(msq[:], mean[:], mean[:], ALU.mult), reads=[mean], writes=[msq])
    p.op('dve', lambda e: e.scalar_tensor_tensor(rstd[:], s2[:], 1.0 / nfeat, msq[:], ALU.mult, ALU.subtract),
         reads=[s2, msq], writes=[rstd])
    p.op('dve', lambda e: e.tensor_scalar_add(rstd[:], rstd[:], eps), reads=[rstd], writes=[rstd])
    p.op('act', lambda e: e.activation(rstd[:], rstd[:], AF.Sqrt), reads=[rstd], writes=[rstd])
    p.op('dve', lambda e: e.reciprocal(rstd[:], rstd[:]), reads=[rstd], writes=[rstd])


def emit_proj(p, D, NCH, NTOK, T, xT, vecs, w_in, oT):
    KC = D // 128
    KCU = min(32, KC)
    HU = KC // KCU
    xvr = xT.bitcast(F32R).rearrange("(c p) t -> p c t", p=128)
    ov = oT.rearrange("(c p) t -> p c t", p=128)
    u = p.sb("u", [128, KC, T], F32R)
    ring = Ring(p, "wr", 4, KCU * 128)
    vs = p.sb("vs", [128, 5, KC])
    sc1 = p.sb("sc1", [128, KC])
    oc = [p.sb("oc%d" % i, [128, T]) for i in range(4)]
    pps = [p.ps("pp%d" % i, [128, T]) for i in range(4)]
    p.dma('pool', vs[:], vecs, writes=[vs])
    p.op('dve', lambda e: e.tensor_scalar_add(sc1[:], vs[:, 1, :], 1.0), reads=[vs], writes=[sc1])
    for t0 in range(0, NTOK, T):
        p.dma('pool', u[:], xvr[:, :, t0:t0 + T], writes=[u])
        for c in range(KC):
            p.op('dve', lambda e, c=c: e.tensor_scalar(u[:, c, :], u[:, c, :].bitcast(F32), sc1[:, c:c + 1],
                                                       vs[:, 0, c:c + 1], ALU.mult, ALU.add),
                 reads=[u, sc1, vs], writes=[u], partial=True)
        for n in range(NCH):
            ps = pps[n % 4]
            ob = oc[n % 4]
            emit_gemm(p, ring, w_in, n, HU, KCU, u, ps, KC)
            if n % 2 == 0:
                p.op('act', lambda e, ob=ob, ps=ps: e.copy(ob[:], ps[:]), reads=[ps], writes=[ob])
            else:
                p.op('dve', lambda e, ob=ob, ps=ps: e.tensor_copy(ob[:], ps[:]), reads=[ps], writes=[ob])
            p.dma('pool', ov[:, n, t0:t0 + T], ob[:], reads=[ob])
    return oc


def build_proj(D, NCH, NTOK, T):
    KC = D // 128
    KCU = min(32, KC)
    nc = new_nc()
    xT = nc.dram_tensor("xT", [D, NTOK], F32, kind="ExternalInput").ap()
    vecs = nc.dram_tensor("vecs", [128, 5, KC], F32, kind="ExternalInput").ap()
    w_in = nc.dram_tensor("w_in", [NCH, KC // KCU, 128, KCU * 128], F32R, kind="ExternalInput").ap()
    oT = nc.dram_tensor("oT", [NCH * 128, NTOK], F32, kind="ExternalOutput").ap()
    with contextlib.ExitStack() as es:
        p = Prog(nc, es)
        oc = emit_proj(p, D, NCH, NTOK, T, xT, vecs, w_in, oT)
        p.finish('pool', oc)
        p.emit()
    return nc


def emit_out(p, D, NTOK, T, xT, cT, vecs, w_o, oT, res_w=1.0):
    KC = D // 128
    KCU = min(32, KC)
    HD = KC // KCU
    xv = xT.rearrange("(c p) t -> p c t", p=128)
    cv = cT.rearrange("(c p) t -> p c t", p=128)
    ov = oT.rearrange("(c p) t -> p c t", p=128)
    eps2 = LN_EPS / (ALPHA * ALPHA)
    a = p.sb("a", [128, KC, T], F32R)
    z = p.sb("z", [128, KC, T], F32)
    ring = Ring(p, "wr", 3, KCU * 128)
    vs = p.sb("vs", [128, 5, KC])
    gwa = p.sb("gwa", [128, KC])
    ones = p.sb("ones", [128, 128])
    xc = [p.sb("xc%d" % i, [128, T]) for i in range(2)]
    sq = [p.sb("sq%d" % i, [128, T]) for i in range(2)]
    oc = [p.sb("oc%d" % i, [128, T]) for i in range(2)]
    t1 = [p.sb("t1%d" % i, [128, T]) for i in range(2)]
    mean = p.sb("mean", [128, T])
    msq = p.sb("msq", [128, T])
    rstd = p.sb("rstd", [128, T])
    py = [p.ps("py%d" % i, [128, T]) for i in range(2)]
    s1 = p.ps("s1", [128, T])
    s2 = p.ps("s2", [128, T])
    p.dma('pool', vs[:], vecs, writes=[vs])
    p.op('dve', lambda e: e.memset(ones[:], 1.0), writes=[ones])
    p.op('dve', lambda e: e.tensor_scalar(gwa[:], vs[:, 2, :], 1.0, res_w / ALPHA, ALU.add, ALU.mult),
         reads=[vs], writes=[gwa])
    for t0 in range(0, NTOK, T):
        p.dma('pool', a[:], cv[:, :, t0:t0 + T], writes=[a])
        emit_down_ln(p, ring, w_o, KC, HD, KCU, KC, a, z, py, s1, s2, xc, sq, t1, oc, mean, msq, rstd,
                     xv, ov, t0, T, gwa, vs, ones, D, eps2)
    return oc


def build_out(D, NTOK, T, res_w=1.0):
    KC = D // 128
    KCU = min(32, KC)
    nc = new_nc()
    xT = nc.dram_tensor("xT", [D, NTOK], F32, kind="ExternalInput").ap()
    cT = nc.dram_tensor("cT", [D, NTOK], F32R, kind="ExternalInput").ap()
    vecs = nc.dram_tensor("vecs", [128, 5, KC], F32, kind="ExternalInput").ap()
    w_o = nc.dram_tensor("w_o", [KC, KC // KCU, 128, KCU * 128], F32R, kind="ExternalInput").ap()
    oT = nc.dram_tensor("oT", [D, NTOK], F32, kind="ExternalOutput").ap()
    with contextlib.ExitStack() as es:
        p = Prog(nc, es)
        oc = emit_out(p, D, NTOK, T, xT, cT, vecs, w_o, oT, res_w)
        p.finish('pool', oc)
        p.emit()
    return nc


def build_chain(D, DFF, NTOK, stages, out_T, ffn_T, ffn_tiles, proj_T=512):
    KC = D // 128
    FC = DFF // 128
    KCU = min(32, KC)
    nc = new_nc()
    xT = nc.dram_tensor("xT", [D, NTOK], F32, kind="ExternalInput").ap()
    xprod = [i for i, (k, _) in enumerate(stages) if k != 'proj']
    with contextlib.ExitStack() as es:
        p = Prog(nc, es)
        cur = xT
        for i, (kind, arg) in enumerate(stages):
            vecs = nc.dram_tensor("vecs%d" % i, [128, 5, KC], F32, kind="ExternalInput").ap()
            p.begin_phase("ph%d_" % i)
            if kind == 'proj':
                w_in = nc.dram_tensor("w_in%d" % i, [arg, KC // KCU, 128, KCU * 128], F32R, kind="ExternalInput").ap()
                pT = nc.dram_tensor("pT%d" % i, [arg * 128, NTOK], F32, kind="ExternalOutput").ap()
                oc = emit_proj(p, D, arg, NTOK, proj_T, cur, vecs, w_in, pT)
            else:
                ext = (i == xprod[-1]) or (i + 1 < len(stages) and stages[i + 1][0] == 'proj')
                dst = nc.dram_tensor("oT%d" % i, [D, NTOK], F32, kind="ExternalOutput" if ext else "Internal").ap()
                if kind == 'out':
                    cT = nc.dram_tensor("cT%d" % i, [D, NTOK], F32R, kind="ExternalInput").ap()
                    w_o = nc.dram_tensor("w_o%d" % i, [KC, KC // KCU, 128, KCU * 128], F32R, kind="ExternalInput").ap()
                    oc = emit_out(p, D, NTOK, out_T, cur, cT, vecs, w_o, dst, 1.0)
                else:
                    w_up = nc.dram_tensor("w_up%d" % i, [2 * FC, KC // KCU, 128, KCU * 128], F32R, kind="ExternalInput").ap()
                    w_dn = nc.dram_tensor("w_dn%d" % i, [KC, FC // KCU, 128, KCU * 128], F32R, kind="ExternalInput").ap()
                    oc = emit_ffn(p, D, DFF, NTOK, ffn_T, cur, vecs, w_up, w_dn, dst, 0.5, ffn_tiles)
                cur = dst
            p.finish('pool', oc)
            p.end_phase()
    return nc


def emit_down_ln(p, ring, w_dn, KC, HD, KCU, FC, a, z, py, s1, s2, xc, sq, t1, oc, mean, msq, rstd,
                 xv, ov, t0, T, gwa, vs, ones, D, eps2):
    for n in range(KC):
        ps = py[n % 2]
        emit_gemm(p, ring, w_dn, n, HD, KCU, a, ps, FC)
        xb = xc[n % 2]
        p.dma('pool', xb[:], xv[:, n, t0:t0 + T], writes=[xb])
        p.op('dve', lambda e, ps=ps, xb=xb, n=n: e.scalar_tensor_tensor(
            z[:, n, :], ps[:], gwa[:, n:n + 1], xb[:], ALU.mult, ALU.add),
            reads=[ps, xb, gwa], writes=[z], partial=True)
        qb = sq[n % 2]
        p.op('act', lambda e, qb=qb, n=n: e.activation(qb[:], z[:, n, :], AF.Square), reads=[z], writes=[qb])
        p.op('pe', lambda e, n=n: e.matmul(s1[:], ones[:], z[:, n, :], start=(n == 0), stop=(n == KC - 1)),
             reads=[ones, z], writes=[s1], partial=(n > 0))
        p.op('pe', lambda e, n=n, qb=qb: e.matmul(s2[:], ones[:], qb[:], start=(n == 0), stop=(n == KC - 1)),
             reads=[ones, qb], writes=[s2], partial=(n > 0))
    emit_ln_stats_finalize(p, s1, s2, mean, msq, rstd, D, eps2)
    for n in range(KC):
        tb, ob = t1[n % 2], oc[n % 2]
        p.op('dve', lambda e, tb=tb, n=n: e.tensor_tensor(tb[:], z[:, n, :], mean[:], ALU.subtract),
             reads=[z, mean], writes=[tb])
        p.op('dve', lambda e, tb=tb: e.tensor_tensor(tb[:], tb[:], rstd[:], ALU.mult),
             reads=[tb, rstd], writes=[tb])
        p.op('act', lambda e, tb=tb, ob=ob, n=n: e.activation(ob[:], tb[:], AF.Identity,
                                                            bias=vs[:, 4, n:n + 1], scale=vs[:, 3, n:n + 1]),
             reads=[tb, vs], writes=[ob])
        p.dma('pool', ov[:, n, t0:t0 + T], ob[:], reads=[ob])


def build_ada(D, NCOL, NL, NB):
    KC = D // 128
    CB = NCOL // 512
    nc = new_nc()
    cT = nc.dram_tensor("cT", [128, KC, NB], F32, kind="ExternalInput").ap()
    ws = [nc.dram_tensor("w%d" % l, [D, NCOL], F32R, kind="ExternalInput").ap() for l in range(NL)]
    bs = [nc.dram_tensor("b%d" % l, [NB, NCOL], F32, kind="ExternalInput").ap() for l in range(NL)]
    outs = [nc.dram_tensor("m%d" % l, [NB, NCOL], F32, kind="ExternalOutput").ap() for l in range(NL)]
    with contextlib.ExitStack() as es:
        p = Prog(nc, es)
        cs = p.sb("cs", [128, KC, NB])
        sc = p.sb("sc", [128, KC, NB], F32R)
        ring = Ring(p, "wr", 6, 8 * 512)
        bsb = [p.sb("bsb%d" % l, [NB, NCOL]) for l in range(NL)]
        osb = [p.sb("osb%d" % l, [NB, NCOL]) for l in range(NL)]
        pps = [p.ps("pp%d" % i, [NB, 512]) for i in range(2)]
        p.dma('pool', cs[:], cT, writes=[cs])
        p.op('act', lambda e: e.activation(sc[:], cs[:], AF.Silu), reads=[cs], writes=[sc])
        for l in range(NL):
            p.dma('pool', bsb[l][:], bs[l], writes=[bsb[l]])
            wv = ws[l].rearrange("(k p) n -> p k n", p=128)
            for cb in range(CB):
                ps = pps[cb % 2]
                for h in range(KC // 8):
                    b = ring.slots[ring.i % len(ring.slots)]
                    ring.i += 1
                    p.dma('sp', b[:].rearrange("p (k n) -> p k n", k=8), wv[:, h * 8:(h + 1) * 8, cb * 512:(cb + 1) * 512],
                          writes=[b])
                    for kc in range(8):
                        k = h * 8 + kc
                        p.op('pe', lambda e, ps=ps, b=b, kc=kc, k=k: e.matmul(
                            ps[:], sc[:, k, :], b[:, kc * 512:(kc + 1) * 512], start=(k == 0), stop=(k == KC - 1)),
                            reads=[b, sc], writes=[ps], partial=(k > 0))
                p.op('dve', lambda e, ps=ps, l=l, cb=cb: e.tensor_tensor(
                    osb[l][:, cb * 512:(cb + 1) * 512], ps[:], bsb[l][:, cb * 512:(cb + 1) * 512], ALU.add),
                    reads=[ps, bsb[l]], writes=[osb[l]], partial=True)
            p.dma('pool', outs[l], osb[l][:], reads=[osb[l]])
        p.finish('pool', osb)
        p.emit()
    return nc


NEG = -30000.0


def emit_dilattn(p, NH, S, qT_d, kT_d, v_d, tbl_d, out_d, banks, ones_r, scale, qk, scr):
    NT = S // 128
    NQB = S // 512
    LA = 4
    q = qk.t[:, 0, :]
    k = qk.t[:, 1, :]
    v = qk.t[:, 2, :].rearrange("p (t d) -> p t d", d=128)
    tbl = p.sb('atbl', [128, 23 * 128])
    sring = [banks[0], banks[1], banks[6], banks[7], banks[4], banks[5]]
    sc = scr['f'][0:2] + scr['f'][5:7]
    pt = scr['r'][0:4]
    ob, rd = scr['f'][2:4], scr['f'][4]
    for h in range(NH):
        p.dma('sp', q, qT_d[h], writes=[qk])
        p.dma('sp', k, kT_d[h], writes=[qk], partial=True)
        p.dma('sp', v, v_d[h], writes=[qk], partial=True)
        p.dma('sp', tbl[:], tbl_d[h], writes=[tbl])
        pairs = []
        for qb in range(NQB):
            kts = list(range(max(0, 4 * qb - 16), 4 * qb + 4))
            for i, kt in enumerate(kts):
                pairs.append((qb, kt, i == 0, i == len(kts) - 1))
        n = len(pairs)
        for it in range(n + LA):
            if it < n:
                qb, kt, first, last = pairs[it]
                s_ps = sring[it % 6]
                p.op('pe', lambda e, s_ps=s_ps, kt=kt, qb=qb: e.matmul(
                    s_ps[:], k[:, kt * 128:(kt + 1) * 128], q[:, qb * 512:(qb + 1) * 512], start=True, stop=True),
                    reads=[qk], writes=[s_ps])
            j = it - LA
            if j < 0:
                continue
            qb, kt, first, last = pairs[j]
            s_ps = sring[j % 6]
            sci, pti = sc[j % 4], pt[j % 4]
            o_ps = banks[2]
            d_ps = banks[3]
            off = (4 * qb - kt + 3) * 128
            p.op('dve', lambda e, s_ps=s_ps, sci=sci, off=off: e.scalar_tensor_tensor(
                sci[:], s_ps[:], scale, tbl[:, off:off + 512], ALU.mult, ALU.add),
                reads=[s_ps, tbl], writes=[sci])
            p.op('act', lambda e, sci=sci, pti=pti: e.activation(pti[:], sci[:], AF.Exp), reads=[sci], writes=[pti])
            p.op('pe', lambda e, o_ps=o_ps, kt=kt, pti=pti, first=first, last=last: e.matmul(
                o_ps[:], v[:, kt, :], pti[:], start=first, stop=last), reads=[qk, pti], writes=[o_ps], partial=not first)
            p.op('pe', lambda e, d_ps=d_ps, pti=pti, first=first, last=last: e.matmul(
                d_ps[:], ones_r[:], pti[:], start=first, stop=last), reads=[ones_r, pti], writes=[d_ps], partial=not first)
            if last:
                obi = ob[qb % 2]
                p.op('dve', lambda e, d_ps=d_ps: e.reciprocal(rd[:], d_ps[:]), reads=[d_ps], writes=[rd])
                p.op('dve', lambda e, o_ps=o_ps, obi=obi: e.tensor_tensor(obi[:], o_ps[:], rd[:], ALU.mult),
                     reads=[o_ps, rd], writes=[obi])
                p.dma('pool', out_d[h * 128:(h + 1) * 128, qb * 512:(qb + 1) * 512], obi[:], reads=[obi])
    return ob


def dil_table(slope):
    c = np.arange(23 * 128)[None, :]
    jl = np.arange(128)[:, None]
    dist = c - 384 - jl
    cnt = ((dist <= 128).astype(np.float64) + ((dist % 4 == 0) & (dist <= 512)) + ((dist % 16 == 0) & (dist <= 2048)))
    ok = (dist >= 0) & (dist <= 2048) & (cnt > 0)
    val = np.where(ok, -slope * dist + np.log(np.maximum(cnt, 1.0)), NEG)
    return val.astype(np.float32)


def emit_mlstm(p, S, qkpre_d, cw_d, bv_d, bo_d, gi_d, gf_d, gb_d, ng_d, mask_d, cst_d, out_d, banks, ones_r, ones_f, qk, scr):
    NT = S // 128
    NQB = S // 512
    DK = 256
    PIECE = 512
    pre = [p.sb('bpre%d' % i, [128, PIECE + 3]) for i in range(2)]
    acc = [p.sb('bacc%d' % i, [128, PIECE]) for i in range(2)]
    cw = p.sb('bcw', [128, 4, 4])
    gb = p.sb('bgb', [32, 2])
    ngb = p.sb('bngb', [32, 1])
    ng = p.sb('bng', [128, 4])
    mask = p.sb('bmask', [128, 4, 512])
    bcol = p.sb('bbcol', [128, NT])
    fb = [p.sb('bfb%d' % i, [128, 512]) for i in range(2)]
    dt_ = scr['f'][0:2]
    at = scr['r'][0:2]
    bvt = [p.sb('bbvt%d' % i, [128, 512], F32R) for i in range(3)]
    dt_ = dt_ + [scr['f'][10]]
    dt_ = dt_[0:2]
    bo = [p.sb('bbo', [128, 4, 512])] * 2
    hT = p.sb('bhT', [128, 4, 512])
    sqh = scr['f'][5:7]
    rden = scr['f'][4]
    mean = p.sb('bmean', [128, 512])
    msq = p.sb('bmsq', [128, 512])
    rstd = p.sb('brstd', [128, 512])
    t1 = scr['f'][7:9]
    sg = scr['f'][9:11]
    ob = scr['f'][2:4]

    p.dma('pool', cw[:], cw_d, writes=[cw])
    p.dma('pool', gb[:], gb_d, writes=[gb])
    p.dma('pool', ng[:], ng_d, writes=[ng])
    p.dma('pool', mask[:], mask_d, writes=[mask])
    NTT = S // 128
    gi = p.sb('bgi', [NTT, 128])
    ga = p.sb('bga', [NTT, 128])
    gc = p.sb('bgc', [NTT, 128])
    F2d = p.sb('bF2d', [NTT, 128])
    b2d = p.sb('bb2d', [NTT, 128])
    offs = p.sb('boffs', [NTT, 1])
    cst = p.sb('bcst', [NTT, 2 * NTT + NTT * 128])
    p.dma('pool', cst[:], cst_d, writes=[cst])
    p.dma('pool', gi[:], gi_d, writes=[gi])
    p.dma('pool', ga[:], gf_d, writes=[ga])
    p.op('dve', lambda e: e.tensor_scalar_mul(ngb[:], gb[:, 1:2], -1.0), reads=[gb], writes=[ngb])
    p.op('act', lambda e: e.activation(gc[:], ga[:], AF.Exp, bias=ngb[:, 0:1], scale=-1.0), reads=[ga, ngb], writes=[gc])
    p.op('dve', lambda e: e.tensor_scalar_add(gc[:], gc[:], 1.0), reads=[gc], writes=[gc])
    p.op('act', lambda e: e.activation(ga[:], gc[:], AF.Ln), reads=[gc], writes=[ga])
    p.op('dve', lambda e: e.tensor_scalar_mul(ga[:], ga[:], -1.0), reads=[ga], writes=[ga])
    cur, oth = ga, gc
    sh = 1
    while sh < 128:
        p.op('dve', lambda e, cur=cur, oth=oth, sh=sh: e.tensor_copy(oth[:, 0:sh], cur[:, 0:sh]), reads=[cur], writes=[oth])
        p.op('dve', lambda e, cur=cur, oth=oth, sh=sh: e.tensor_tensor(oth[:, sh:128], cur[:, sh:128], cur[:, 0:128 - sh], ALU.add),
             reads=[cur], writes=[oth], partial=True)
        cur, oth = oth, cur
        sh *= 2
    misc = banks[7]
    p.op('pe', lambda e, cur=cur: e.matmul(misc[0:NTT, 0:1], cst[:, 0:NTT], cur[:, 127:128], start=True, stop=True),
         reads=[cst, cur], writes=[misc])
    p.op('dve', lambda e: e.tensor_copy(offs[:], misc[0:NTT, 0:1]), reads=[misc], writes=[offs])
    p.op('dve', lambda e, cur=cur: e.tensor_scalar(F2d[:], cur[:], offs[:, 0:1], None, ALU.add), reads=[cur, offs], writes=[F2d])
    p.op('dve', lambda e: e.scalar_tensor_tensor(b2d[:], gi[:], gb[:, 0:1], F2d[:], ALU.add, ALU.subtract),
         reads=[gi, gb, F2d], writes=[b2d])
    p.op('pe', lambda e: e.matmul(misc[:, 0:NTT], b2d[:], cst[:, NTT:2 * NTT], start=True, stop=True),
         reads=[b2d, cst], writes=[misc])
    p.op('dve', lambda e: e.tensor_copy(bcol[:], misc[:, 0:NT]), reads=[misc], writes=[bcol])
    for c in range(4):
        for pc in range(S // PIECE):
            pr, ac = pre[(c * (S // PIECE) + pc) % 2], acc[(c * (S // PIECE) + pc) % 2]
            p.dma('sp', pr[:], qkpre_d[c, :, pc * PIECE:pc * PIECE + PIECE + 3], writes=[pr])
            p.op('dve', lambda e, pr=pr, ac=ac, c=c: e.tensor_scalar(ac[:], pr[:, 0:PIECE], cw[:, c, 0:1], None, ALU.mult),
                 reads=[pr, cw], writes=[ac])
            for j in range(1, 4):
                p.op('dve', lambda e, pr=pr, ac=ac, c=c, j=j: e.scalar_tensor_tensor(
                    ac[:], pr[:, j:j + PIECE], cw[:, c, j:j + 1], ac[:], ALU.mult, ALU.add), reads=[pr, cw, ac], writes=[ac])
            p.op('act', lambda e, ac=ac, c=c, pc=pc: e.activation(qk[:, c, pc * PIECE:(pc + 1) * PIECE], ac[:], AF.Silu),
                 reads=[ac], writes=[qk], partial=True)
    nld = 0
    for qb in range(NQB):
        fbi = fb[qb % 2]
        for i4 in range(4):
            tl = 4 * qb + i4
            p.op('pe', lambda e, tl=tl, i4=i4: e.matmul(misc[:, i4 * 128:(i4 + 1) * 128],
                                                         cst[:, 2 * NTT + tl * 128:2 * NTT + (tl + 1) * 128], F2d[:],
                                                         start=True, stop=True),
                 reads=[cst, F2d], writes=[misc], partial=(i4 > 0))
        p.op('act', lambda e, fbi=fbi: e.copy(fbi[:], misc[:]), reads=[misc], writes=[fbi])
        p.dma('pool', bo[qb % 2][:], bo_d[:, :, qb * 512:(qb + 1) * 512].rearrange("c p t -> p c t"), writes=[bo[qb % 2]])
        nkt = 4 * qb + 4
        bvs = {}
        for it in range(nkt + 1):
            if it < nkt:
                kt = it
                s_ps = banks[kt % 2]
                dti = dt_[kt % 2]
                bvb = bvt[nld % 3]
                nld += 1
                bvs[kt] = bvb
                p.dma('sp', bvb[:], bv_d[:, kt, :], writes=[bvb])
                for c in range(2):
                    p.op('pe', lambda e, s_ps=s_ps, c=c, kt=kt, qb=qb: e.matmul(
                        s_ps[:], qk[:, 2 + c, kt * 128:(kt + 1) * 128], qk[:, c, qb * 512:(qb + 1) * 512],
                        start=(c == 0), stop=(c == 1)), reads=[qk], writes=[s_ps], partial=(c > 0))
                p.op('act', lambda e, dti=dti, fbi=fbi, kt=kt: e.activation(dti[:], fbi[:], AF.Exp, bias=bcol[:, kt:kt + 1]),
                     reads=[fbi, bcol], writes=[dti])
                if kt >= 4 * qb:
                    p.op('dve', lambda e, dti=dti, kt=kt, qb=qb: e.tensor_tensor(dti[:], dti[:], mask[:, kt - 4 * qb, :], ALU.mult),
                         reads=[dti, mask], writes=[dti])
            kt = it - 1
            if kt < 0:
                continue
            s_ps = banks[kt % 2]
            dti, ati = dt_[kt % 2], at[kt % 2]
            bvb = bvs[kt]
            p.op('dve', lambda e, s_ps=s_ps, dti=dti, ati=ati: e.scalar_tensor_tensor(
                ati[:], s_ps[:], DK ** -0.5, dti[:], ALU.mult, ALU.mult), reads=[s_ps, dti], writes=[ati])
            first, last = (kt == 0), (kt == nkt - 1)
            for ec in range(4):
                p.op('pe', lambda e, ec=ec, bvb=bvb, ati=ati, first=first, last=last: e.matmul(
                    banks[2 + ec][:], bvb[:, ec * 128:(ec + 1) * 128], ati[:], start=first, stop=last),
                    reads=[bvb, ati], writes=[banks[2 + ec]], partial=not first)
            p.op('pe', lambda e, ati=ati, first=first, last=last: e.matmul(
                banks[6][:], ones_r[:], ati[:], start=first, stop=last), reads=[ones_r, ati], writes=[banks[6]], partial=not first)
        p.op('act', lambda e: e.activation(rden[:], banks[6][:], AF.Abs), reads=[banks[6]], writes=[rden])
        p.op('dve', lambda e: e.tensor_scalar_max(rden[:], rden[:], 1.0), reads=[rden], writes=[rden])
        p.op('dve', lambda e: e.reciprocal(rden[:], rden[:]), reads=[rden], writes=[rden])
        for ec in range(4):
            p.op('dve', lambda e, ec=ec: e.tensor_tensor(hT[:, ec, :], banks[2 + ec][:], rden[:], ALU.mult),
                 reads=[banks[2 + ec], rden], writes=[hT], partial=True)
            sqi = sqh[ec % 2]
            p.op('act', lambda e, ec=ec, sqi=sqi: e.activation(sqi[:], hT[:, ec, :], AF.Square), reads=[hT], writes=[sqi])
            p.op('pe', lambda e, ec=ec: e.matmul(banks[0][:], ones_f[:], hT[:, ec, :], start=(ec == 0), stop=(ec == 3)),
                 reads=[ones_f, hT], writes=[banks[0]], partial=(ec > 0))
            p.op('pe', lambda e, ec=ec, sqi=sqi: e.matmul(banks[1][:], ones_f[:], sqi[:], start=(ec == 0), stop=(ec == 3)),
                 reads=[ones_f, sqi], writes=[banks[1]], partial=(ec > 0))
        emit_ln_stats_finalize(p, banks[0], banks[1], mean, msq, rstd, 512, LN_EPS)
        for ec in range(4):
            tb, sgi, obi = t1[ec % 2], sg[ec % 2], ob[ec % 2]
            p.op('dve', lambda e, tb=tb, ec=ec: e.tensor_tensor(tb[:], hT[:, ec, :], mean[:], ALU.subtract),
                 reads=[hT, mean], writes=[tb])
            p.op('dve', lambda e, tb=tb: e.tensor_tensor(tb[:], tb[:], rstd[:], ALU.mult), reads=[tb, rstd], writes=[tb])
            p.op('act', lambda e, sgi=sgi, ec=ec, qb=qb: e.activation(sgi[:], bo[qb % 2][:, ec, :], AF.Sigmoid),
                 reads=[bo[qb % 2]], writes=[sgi])
            p.op('dve', lambda e, tb=tb, sgi=sgi, obi=obi, ec=ec: e.scalar_tensor_tensor(
                obi[:], tb[:], ng[:, ec:ec + 1], sgi[:], ALU.mult, ALU.mult), reads=[tb, ng, sgi], writes=[obi])
            p.dma('pool', out_d[ec * 128:(ec + 1) * 128, qb * 512:(qb + 1) * 512], obi[:], reads=[obi])
    return ob


def build_mix0(S, NH, with_a=True, with_b=True):
    nc = new_nc()
    NT = S // 128
    qT_d = nc.dram_tensor("aq", [NH, 128, S], F32R, kind="ExternalInput").ap()
    kT_d = nc.dram_tensor("ak", [NH, 128, S], F32R, kind="ExternalInput").ap()
    v_d = nc.dram_tensor("av", [NH, 128, NT, 128], F32R, kind="ExternalInput").ap()
    tbl_d = nc.dram_tensor("atbl", [NH, 128, 23 * 128], F32, kind="ExternalInput").ap()
    ao_d = nc.dram_tensor("ao", [NH * 128, S], F32, kind="ExternalOutput").ap()
    qkpre_d = nc.dram_tensor("bqk", [4, 128, S + 3], F32, kind="ExternalInput").ap()
    cw_d = nc.dram_tensor("bcw", [128, 4, 4], F32, kind="ExternalInput").ap()
    bv_d = nc.dram_tensor("bv", [128, NT, 512], F32R, kind="ExternalInput").ap()
    bo_d = nc.dram_tensor("bo", [4, 128, S], F32, kind="ExternalInput").ap()
    gi_d = nc.dram_tensor("bgi", [NT, 128], F32, kind="ExternalInput").ap()
    gf_d = nc.dram_tensor("bgf", [NT, 128], F32, kind="ExternalInput").ap()
    gb_d = nc.dram_tensor("bgb", [NT, 2], F32, kind="ExternalInput").ap()
    cst_d = nc.dram_tensor("bcst", [NT, 2 * NT + NT * 128], F32, kind="ExternalInput").ap()
    ng_d = nc.dram_tensor("bng", [128, 4], F32, kind="ExternalInput").ap()
    mask_d = nc.dram_tensor("bmask", [128, 4, 512], F32, kind="ExternalInput").ap()
    bout_d = nc.dram_tensor("bout", [512, S], F32, kind="ExternalOutput").ap()
    with contextlib.ExitStack() as es:
        p = Prog(nc, es)
        banks = [p.ps("bank%d" % i, [128, 512]) for i in range(8)]
        ones_r = p.sb("ones_r", [128, 128], F32R)
        ones_f = p.sb("ones_f", [128, 128], F32)
        p.op('dve', lambda e: e.memset(ones_f[:], 1.0), writes=[ones_f])
        p.op('dve', lambda e: e.tensor_copy(ones_r[:], ones_f[:]), reads=[ones_f], writes=[ones_r])
        outs = []
        qk = p.sb('qk', [128, 4, S], F32R)
        scr = {'f': [p.sb('scrf%d' % i, [128, 512]) for i in range(11)],
               'r': [p.sb('scrr%d' % i, [128, 512], F32R) for i in range(4)]}
        if with_a:
            outs += emit_dilattn(p, NH, S, qT_d, kT_d, v_d, tbl_d, ao_d, banks, ones_r, 128 ** -0.5, qk, scr)
        if with_b:
            outs += emit_mlstm(p, S, qkpre_d, cw_d, bv_d, bo_d, gi_d, gf_d, gb_d, ng_d, mask_d, cst_d, bout_d, banks, ones_r, ones_f, qk, scr)
        p.finish('pool', outs)
        p.emit()
    return nc


def causal_mask_4x512():
    s = np.arange(128)[:, None, None]
    i = np.arange(4)[None, :, None]
    t = np.arange(512)[None, None, :]
    return (i * 128 + s <= t).astype(np.float32)


def mlstm_consts(NT):
    U = (np.arange(NT)[:, None] < np.arange(NT)[None, :]).astype(np.float32)
    I = np.eye(NT, dtype=np.float32)
    Sel = np.zeros((NT, NT, 128), np.float32)
    Sel[np.arange(NT), np.arange(NT), :] = 1.0
    return np.concatenate([U, I, Sel.reshape(NT, NT * 128)], axis=1)


MOBA_TW = 35 * 128


def moba_table(slope):
    c = np.arange(MOBA_TW)[None, :]
    jl = np.arange(128)[:, None]
    dist = c - 384 - jl
    return np.where(dist >= 0, -slope * dist, NEG).astype(np.float32)


def moba_consts():
    blk = np.arange(16)[:, None]
    n = np.arange(16)[None, :]
    past = (n < blk)
    pm = np.where(past, 0.0, NEG).reshape(-1)
    p01 = past.astype(np.float64).reshape(-1)
    own = (n == blk).astype(np.float64).reshape(-1)
    row = np.concatenate([pm, p01, own]).astype(np.float32)
    return np.ascontiguousarray(np.concatenate([np.tile(row[None, :], (128, 1)), np.eye(128, dtype=np.float32)], axis=1))


def moba_esel():
    E = np.zeros((16, 16, 128), np.float32)
    E[np.arange(16), np.arange(16), :] = 1.0
    return E.reshape(16, 16 * 128)


def emit_moba(p, NH, S, qT_d, kT_d, v_d, tbl_d, cst_d, esel_d, out_d, banks, ones_r, scale, qk, scr):
    NT = S // 128
    NQB = S // 512
    NB = S // 256
    q = qk.t[:, 0, :]
    k = qk.t[:, 1, :]
    v = qk.t[:, 2, :].rearrange("p (t d) -> p t d", d=128)
    tbl = p.sb('ctbl', [128, MOBA_TW])
    cst = p.sb('ccst', [128, 3 * 256 + 128])
    BF16 = mybir.dt.bfloat16
    esel_f = p.sb('cesel', [16, 16 * 128], F32R)
    esel = p.sb('ceselb', [16, 16 * 128], BF16)
    selT = p.sb('cselT', [16, S], BF16)
    kmean = p.sb('ckmean', [128, NB], F32R)
    ksum = p.sb('cksum', [128, NB])
    gm = [p.sb('cgm%d' % i, [128, 64]) for i in range(4)]
    top8 = [p.sb('ctop%d' % i, [128, 32]) for i in range(4)]
    sel = [p.sb('csel%d' % i, [128, 64]) for i in range(4)]
    gps = [banks[2], banks[3], banks[4], banks[5]]
    tps = [banks[6], banks[7]]
    LA = 2
    sc, pt, ob, rd, ex = scr['f'][0:2] + scr['f'][9:10], scr['r'][0:4], scr['f'][2:4], scr['f'][4], scr['f'][5:8]
    sring = [banks[0], banks[1], banks[6], banks[7], banks[4], banks[5]]
    ident = cst.t[:, 768:896]
    p.dma('pool', cst[:], cst_d, writes=[cst])
    p.dma('pool', esel_f[:], esel_d, writes=[esel_f])
    p.op('dve', lambda e: e.tensor_copy(esel[:], esel_f[:].bitcast(F32)), reads=[esel_f], writes=[esel])
    misc = banks[7]
    for h in range(NH):
        p.dma('sp', q, qT_d[h], writes=[qk])
        p.dma('sp', k, kT_d[h], writes=[qk], partial=True)
        p.dma('sp', v, v_d[h], writes=[qk], partial=True)
        p.dma('sp', tbl[:], tbl_d[h], writes=[tbl])
        p.op('dve', lambda e: e.tensor_reduce(ksum[:], k.bitcast(F32).rearrange("p (n j) -> p n j", j=256), AX.X, ALU.add),
             reads=[qk], writes=[ksum])
        p.op('dve', lambda e: e.tensor_scalar_mul(kmean[:], ksum[:], 1.0 / 256), reads=[ksum], writes=[kmean])
        NG = NT // 4
        for it in range(NG + 2):
            if it < NG:
                G = it
                gp = gps[G % 4]
                for i4 in range(4):
                    qt = 4 * G + i4
                    p.op('pe', lambda e, qt=qt, gp=gp, i4=i4: e.matmul(gp[:, i4 * 16:(i4 + 1) * 16], q[:, qt * 128:(qt + 1) * 128], kmean[:],
                                                                       start=True, stop=True),
                         reads=[qk, kmean], writes=[gp], partial=(i4 > 0))
            G = it - 1
            if 0 <= G < NG:
                gp = gps[G % 4]
                g, t8, sl = gm[G % 4], top8[G % 4], sel[G % 4]

                def tb4(base, G=G):
                    return cst.t[:, base + 2 * G * 16:base + (2 * G + 2) * 16].rearrange("p (b n) -> p b n", b=2) \
                        .unsqueeze(2).broadcast_to([128, 2, 2, 16])

                v4 = lambda ap: ap.rearrange("p (b r n) -> p b r n", b=2, r=2)
                p.op('dve', lambda e, g=g, gp=gp, tb4=tb4, v4=v4: e.tensor_tensor(v4(g[:]), v4(gp[:, 0:64]), tb4(0), ALU.add),
                     reads=[gp, cst], writes=[g])
                for i4 in range(4):
                    p.op('dve', lambda e, g=g, t8=t8, i4=i4: e.max(t8[:, i4 * 8:(i4 + 1) * 8], g[:, i4 * 16:(i4 + 1) * 16]),
                         reads=[g], writes=[t8], partial=(i4 > 0))
                p.op('dve', lambda e, g=g, t8=t8, sl=sl: e.tensor_tensor(
                    sl[:].rearrange("p (t n) -> p t n", n=16), g[:].rearrange("p (t n) -> p t n", n=16),
                    t8[:].rearrange("p (t n) -> p t n", n=8)[:, :, 2:3].broadcast_to([128, 4, 16]), ALU.is_ge),
                    reads=[g, t8], writes=[sl])
                p.op('dve', lambda e, sl=sl, tb4=tb4, v4=v4: e.tensor_tensor(v4(sl[:]), v4(sl[:]), tb4(256), ALU.mult),
                     reads=[sl, cst], writes=[sl])
                p.op('dve', lambda e, sl=sl, tb4=tb4, v4=v4: e.tensor_tensor(v4(sl[:]), v4(sl[:]), tb4(512), ALU.add),
                     reads=[sl, cst], writes=[sl])
            G = it - 2
            if 0 <= G < NG:
                sl = sel[G % 4]
                tp = tps[G % 2]
                for i4 in range(4):
                    p.op('pe', lambda e, sl=sl, tp=tp, i4=i4: e.matmul(tp[0:16, i4 * 128:(i4 + 1) * 128], sl[:, i4 * 16:(i4 + 1) * 16], ident,
                                                                       start=True, stop=True),
                         reads=[sl, cst], writes=[tp], partial=(i4 > 0))
                p.op('dve', lambda e, G=G, tp=tp: e.tensor_scalar(selT[:, G * 512:(G + 1) * 512], tp[0:16, 0:512], 1.0, -NEG / scale,
                                                                 ALU.subtract, ALU.mult),
                     reads=[tp], writes=[selT], partial=True)
        pairs = []
        for qb in range(NQB):
            nkt = 4 * qb + 4
            for kt in range(nkt):
                pairs.append((qb, kt, kt == 0, kt == nkt - 1))
        n = len(pairs)
        nblk = 0
        mbank = {}
        for it in range(n + 4):
            if it < n:
                qb, kt, first, last = pairs[it]
                s_ps = sring[it % 6]
                p.op('pe', lambda e, s_ps=s_ps, kt=kt, qb=qb: e.matmul(
                    s_ps[:], k[:, kt * 128:(kt + 1) * 128], q[:, qb * 512:(qb + 1) * 512], start=True, stop=False),
                    reads=[qk], writes=[s_ps])
                p.op('pe', lambda e, s_ps=s_ps, nb=kt // 2, qb=qb: e.matmul(
                    s_ps[:], esel[:, nb * 128:(nb + 1) * 128], selT[:, qb * 512:(qb + 1) * 512], start=False, stop=True),
                    reads=[esel, selT], writes=[s_ps], partial=True)
            j = it - 4
            if 0 <= j < n:
                qb, kt, first, last = pairs[j]
                s_ps = sring[j % 6]
                sci, pti = sc[j % 3], pt[j % 4]
                o_ps = banks[2]
                d_ps = banks[3]
                off = (4 * qb - kt + 3) * 128
                p.op('dve', lambda e, s_ps=s_ps, sci=sci, off=off: e.scalar_tensor_tensor(
                    sci[:], s_ps[:], scale, tbl[:, off:off + 512], ALU.mult, ALU.add), reads=[s_ps, tbl], writes=[sci])
                p.op('act', lambda e, sci=sci, pti=pti: e.activation(pti[:], sci[:], AF.Exp), reads=[sci], writes=[pti])
                p.op('pe', lambda e, o_ps=o_ps, kt=kt, pti=pti, first=first, last=last: e.matmul(
                    o_ps[:], v[:, kt, :], pti[:], start=first, stop=last), reads=[qk, pti], writes=[o_ps], partial=not first)
                p.op('pe', lambda e, d_ps=d_ps, pti=pti, first=first, last=last: e.matmul(
                    d_ps[:], ones_r[:], pti[:], start=first, stop=last), reads=[ones_r, pti], writes=[d_ps], partial=not first)
                if last:
                    obi = ob[qb % 2]
                    p.op('dve', lambda e, d_ps=d_ps: e.reciprocal(rd[:], d_ps[:]), reads=[d_ps], writes=[rd])
                    p.op('dve', lambda e, o_ps=o_ps, obi=obi: e.tensor_tensor(obi[:], o_ps[:], rd[:], ALU.mult),
                         reads=[o_ps, rd], writes=[obi])
                    p.dma('pool', out_d[h * 128:(h + 1) * 128, qb * 512:(qb + 1) * 512], obi[:], reads=[obi])
    return ob


def emit_conv(p, NTOK, NCH, da_d, db_d, dg_d, lv_d, out_d, banks, ones_f, scr):
    W = 31
    HALO = W - 1
    TT = 512
    z = p.sb('dz', [128, NCH, TT])
    da = [p.sb('dda%d' % i, [128, TT + HALO]) for i in range(2)]
    db = [p.sb('ddb%d' % i, [128, TT + HALO]) for i in range(2)]
    gl = [p.sb('dgl%d' % i, [128, TT + HALO], F32R) for i in range(2)]
    dg = [p.sb('ddg%d' % i, [128, W, 128], F32R) for i in range(2)]
    lv = p.sb('dlv', [128, 2, NCH])
    mean = p.sb('dmean', [128, 512])
    rstd = p.sb('drstd', [128, 512])
    msq = scr['f'][4]
    sq, t1, ob = scr['f'][5:7], scr['f'][7:9], scr['f'][2:4]
    zring = [banks[2], banks[3]]
    p.dma('pool', lv[:], lv_d, writes=[lv])
    it = 0
    for hf in range(NTOK // TT):
        s1, s2 = banks[0], banks[1]
        for c in range(NCH):
            a_, b_, g_, d_ = da[it % 2], db[it % 2], gl[it % 2], dg[it % 2]
            z_ps = zring[it % 2]
            it += 1
            p.dma('sp', a_[:], da_d[c, :, hf * TT:hf * TT + TT + HALO], writes=[a_])
            p.dma('sp', b_[:], db_d[c, :, hf * TT:hf * TT + TT + HALO], writes=[b_])
            p.dma('sp', d_[:], dg_d[c].rearrange("p (j m) -> p j m", m=128), writes=[d_])
            p.op('act', lambda e, b_=b_: e.activation(b_[:], b_[:], AF.Sigmoid), reads=[b_], writes=[b_])
            p.op('dve', lambda e, a_=a_, b_=b_, g_=g_: e.tensor_tensor(g_[:], a_[:], b_[:], ALU.mult), reads=[a_, b_], writes=[g_])
            for j in range(W):
                p.op('pe', lambda e, z_ps=z_ps, d_=d_, g_=g_, j=j: e.matmul(z_ps[:], d_[:, j, :], g_[:, j:j + TT],
                                                                            start=(j == 0), stop=(j == W - 1)),
                     reads=[d_, g_], writes=[z_ps], partial=(j > 0))
            sqi = sq[c % 2]
            p.op('act', lambda e, z_ps=z_ps, c=c: e.copy(z[:, c, :], z_ps[:]), reads=[z_ps], writes=[z], partial=True)
            p.op('act', lambda e, sqi=sqi, z_ps=z_ps: e.activation(sqi[:], z_ps[:], AF.Square), reads=[z_ps], writes=[sqi])
            p.op('pe', lambda e, c=c: e.matmul(s1[:], ones_f[:], z[:, c, :], start=(c == 0), stop=(c == NCH - 1)),
                 reads=[ones_f, z], writes=[s1], partial=(c > 0))
            p.op('pe', lambda e, c=c, sqi=sqi: e.matmul(s2[:], ones_f[:], sqi[:], start=(c == 0), stop=(c == NCH - 1)),
                 reads=[ones_f, sqi], writes=[s2], partial=(c > 0))
        emit_ln_stats_finalize(p, s1, s2, mean, msq, rstd, NCH * 128, LN_EPS)
        for c in range(NCH):
            tb, obi = t1[c % 2], ob[c % 2]
            p.op('dve', lambda e, tb=tb, c=c: e.tensor_tensor(tb[:], z[:, c, :], mean[:], ALU.subtract),
                 reads=[z, mean], writes=[tb])
            p.op('dve', lambda e, tb=tb: e.tensor_tensor(tb[:], tb[:], rstd[:], ALU.mult), reads=[tb, rstd], writes=[tb])
            p.op('act', lambda e, tb=tb, obi=obi, c=c: e.activation(obi[:], tb[:], AF.Silu, bias=lv[:, 1, c:c + 1], scale=lv[:, 0, c:c + 1]),
                 reads=[tb, lv], writes=[obi])
            p.dma('pool', out_d[c * 128:(c + 1) * 128, hf * TT:(hf + 1) * TT], obi[:], reads=[obi])
    return ob


def build_mix1(S, NH, NTOK, NCH, with_c=True, with_d=True):
    nc = new_nc()
    NT = S // 128
    qT_d = nc.dram_tensor("cq", [NH, 128, S], F32R, kind="ExternalInput").ap()
    kT_d = nc.dram_tensor("ck", [NH, 128, S], F32R, kind="ExternalInput").ap()
    v_d = nc.dram_tensor("cv", [NH, 128, NT, 128], F32R, kind="ExternalInput").ap()
    tbl_d = nc.dram_tensor("ctbl", [NH, 128, MOBA_TW], F32, kind="ExternalInput").ap()
    cst_d = nc.dram_tensor("ccst", [128, 3 * 256 + 128], F32, kind="ExternalInput").ap()
    esel_d = nc.dram_tensor("cesel", [16, 16 * 128], F32R, kind="ExternalInput").ap()
    co_d = nc.dram_tensor("co", [NH * 128, S], F32, kind="ExternalOutput").ap()
    da_d = nc.dram_tensor("dda", [NCH, 128, NTOK + 30], F32, kind="ExternalInput").ap()
    db_d = nc.dram_tensor("ddb", [NCH, 128, NTOK + 30], F32, kind="ExternalInput").ap()
    dg_d = nc.dram_tensor("ddg", [NCH, 128, 31 * 128], F32R, kind="ExternalInput").ap()
    lv_d = nc.dram_tensor("dlv", [128, 2, NCH], F32, kind="ExternalInput").ap()
    do_d = nc.dram_tensor("dout", [NCH * 128, NTOK], F32, kind="ExternalOutput").ap()
    with contextlib.ExitStack() as es:
        p = Prog(nc, es)
        banks = [p.ps("bank%d" % i, [128, 512]) for i in range(8)]
        ones_r = p.sb("ones_r", [128, 128], F32R)
        ones_f = p.sb("ones_f", [128, 128], F32)
        p.op('dve', lambda e: e.memset(ones_f[:], 1.0), writes=[ones_f])
        p.op('dve', lambda e: e.tensor_copy(ones_r[:], ones_f[:]), reads=[ones_f], writes=[ones_r])
        qk = p.sb('qk', [128, 3, S], F32R)
        scr = {'f': [p.sb('scrf%d' % i, [128, 512]) for i in range(11)],
               'r': [p.sb('scrr%d' % i, [128, 512], F32R) for i in range(4)]}
        outs = []
        if with_d:
            outs += emit_conv(p, NTOK, NCH, da_d, db_d, dg_d, lv_d, do_d, banks, ones_f, scr)
        if with_c:
            outs += emit_moba(p, NH, S, qT_d, kT_d, v_d, tbl_d, cst_d, esel_d, co_d, banks, ones_r, 128 ** -0.5, qk, scr)
        p.finish('pool', outs)
        p.emit()
    return nc


TOK_PER_CORE = BATCH * SEQ // NCORES
CORES_PER_BATCH = NCORES // BATCH
FFN_T = 352
FFN_TILES = [(0, 352), (352, 352), (704, 320)]
PROJ_T = 512
OUT_T = 512


def _launch(nc, in_maps):
    res = run_bass_kernel_spmd(nc, in_maps, core_ids=list(range(NCORES)))
    return res.results


def _vecs(mod_l, b, j, ln_g, ln_b):
    return np.ascontiguousarray(np.stack([fm_vec(np.ascontiguousarray(v)) for v in
                                          (mod_l[b, 3 * j], mod_l[b, 3 * j + 1], mod_l[b, 3 * j + 2], ln_g[j], ln_b[j])], axis=1))


def _alibi(n):
    return 2.0 ** (-8.0 * np.arange(1, n + 1) / n)


def _run_ffn(xTs, mod_l, j, ln_g, ln_b, w_up, w_dn):
    wu = tile_w(np.asarray(w_up), 32)
    wd = tile_w(np.asarray(w_dn), 32)
    nc = build_ffn(D_MODEL, D_FF, TOK_PER_CORE, FFN_T, res_w=0.5, tiles=FFN_TILES)
    maps = [{"xT": xTs[c], "vecs": _vecs(mod_l, c // CORES_PER_BATCH, j, ln_g, ln_b), "w_up": wu, "w_dn": wd}
            for c in range(NCORES)]
    out = _launch(nc, maps)
    return [np.ascontiguousarray(o["oT"]) for o in out]


def _run_proj(xTs, mod_l, ln_g, ln_b, w_in):
    w_in = np.asarray(w_in)
    ncol = w_in.shape[1]
    nch = -(-ncol // 128)
    if nch * 128 != ncol:
        w_in = np.concatenate([w_in, np.zeros((w_in.shape[0], nch * 128 - ncol), np.float32)], axis=1)
    wt = tile_w(w_in, 32)
    nc = build_proj(D_MODEL, nch, TOK_PER_CORE, PROJ_T)
    maps = [{"xT": xTs[c], "vecs": _vecs(mod_l, c // CORES_PER_BATCH, 1, ln_g, ln_b), "w_in": wt} for c in range(NCORES)]
    out = _launch(nc, maps)
    return [np.concatenate([out[b * CORES_PER_BATCH + r]["oT"] for r in range(CORES_PER_BATCH)], axis=1) for b in range(BATCH)]


def _run_out(xTs, catTs, mod_l, ln_g, ln_b, w_out):
    wo = tile_w(np.asarray(w_out), 32)
    nc = build_out(D_MODEL, TOK_PER_CORE, OUT_T, res_w=1.0)
    maps = []
    for c in range(NCORES):
        b, r = divmod(c, CORES_PER_BATCH)
        maps.append({"xT": xTs[c], "cT": np.ascontiguousarray(catTs[b][:, r * TOK_PER_CORE:(r + 1) * TOK_PER_CORE]),
                     "vecs": _vecs(mod_l, b, 1, ln_g, ln_b), "w_o": wo})
    out = _launch(nc, maps)
    return [np.ascontiguousarray(o["oT"]) for o in out]


def _heads_T(pT, row0, nh):
    return np.ascontiguousarray(pT[row0:row0 + nh * 128].reshape(nh, 128, SEQ))


def _heads_V(pT, row0, nh):
    vt = pT[row0:row0 + nh * 128].reshape(nh, 128, SEQ // 128, 128)
    return np.ascontiguousarray(vt.transpose(0, 3, 2, 1))


def _run_mix0(projT, conv_qk, b_igate, b_fgate, norm_g):
    NH = 4
    slopes = _alibi(16)
    conv_qk = np.asarray(conv_qk)
    norm_g = np.asarray(norm_g)
    nc = build_mix0(SEQ, NH)
    mask = causal_mask_4x512()
    cst = mlstm_consts(SEQ // 128)
    maps = []
    for c in range(NCORES):
        b, r = divmod(c, CORES_PER_BATCH)
        pT = projT[b]
        m = {}
        m["aq"] = _heads_T(pT, 4 * r * 128, NH)
        m["ak"] = _heads_T(pT, 2048 + 4 * r * 128, NH)
        m["av"] = _heads_V(pT, 4096 + 4 * r * 128, NH)
        m["atbl"] = np.stack([dil_table(slopes[4 * r + i]) for i in range(NH)])
        pre = np.concatenate([pT[6144 + r * 256:6144 + (r + 1) * 256], pT[7168 + r * 256:7168 + (r + 1) * 256]], axis=0)
        pre = np.concatenate([np.zeros((512, 3), np.float32), pre], axis=1)
        m["bqk"] = np.ascontiguousarray(pre.reshape(4, 128, SEQ + 3))
        cw = np.concatenate([conv_qk[:, r * 256:(r + 1) * 256], conv_qk[:, 1024 + r * 256:1024 + (r + 1) * 256]], axis=1)
        m["bcw"] = np.ascontiguousarray(cw.T.reshape(4, 128, 4).transpose(1, 0, 2))
        vT = pT[8192 + r * 512:8192 + (r + 1) * 512]
        m["bv"] = np.ascontiguousarray(vT.reshape(512, SEQ // 128, 128).transpose(2, 1, 0))
        m["bo"] = np.ascontiguousarray(pT[10240 + r * 512:10240 + (r + 1) * 512].reshape(4, 128, SEQ))
        m["bgi"] = np.ascontiguousarray(pT[12288 + r].reshape(SEQ // 128, 128))
        m["bgf"] = np.ascontiguousarray(pT[12292 + r].reshape(SEQ // 128, 128))
        m["bgb"] = np.ascontiguousarray(np.tile(np.array([[b_igate[r], b_fgate[r]]], np.float32), (SEQ // 128, 1)))
        m["bng"] = fm_vec(np.ascontiguousarray(norm_g[r * 512:(r + 1) * 512]))
        m["bmask"] = mask
        m["bcst"] = cst
        maps.append(m)
    out = _launch(nc, maps)
    catTs = [np.empty((D_MODEL, SEQ), np.float32) for _ in range(BATCH)]
    for c in range(NCORES):
        b, r = divmod(c, CORES_PER_BATCH)
        catTs[b][4 * r * 128:(4 * r + 4) * 128] = out[c]["ao"]
        catTs[b][2048 + r * 512:2048 + (r + 1) * 512] = out[c]["bout"]
    return catTs


def _run_mix1(projT, conv_dw, conv_ln_g, conv_ln_b):
    NH = 4
    slopes = _alibi(16)
    conv_dw = np.asarray(conv_dw)
    nc = build_mix1(SEQ, NH, TOK_PER_CORE, 16)
    cst = moba_consts()
    esel = moba_esel()
    dcw = conv_diag(conv_dw)
    dlv = np.ascontiguousarray(np.stack([fm_vec(np.asarray(conv_ln_g)), fm_vec(np.asarray(conv_ln_b))], axis=1))
    maps = []
    for c in range(NCORES):
        b, r = divmod(c, CORES_PER_BATCH)
        pT = projT[b]
        m = {}
        m["cq"] = _heads_T(pT, 4 * r * 128, NH)
        m["ck"] = _heads_T(pT, 2048 + 4 * r * 128, NH)
        m["cv"] = _heads_V(pT, 4096 + 4 * r * 128, NH)
        m["ctbl"] = np.stack([moba_table(slopes[4 * r + i]) for i in range(NH)])
        m["ccst"] = cst
        m["cesel"] = esel
        t0 = r * TOK_PER_CORE
        for name, row0 in (("dda", 6144), ("ddb", 8192)):
            blk = np.zeros((2048, TOK_PER_CORE + 30), np.float32)
            lo = max(0, t0 - 30)
            blk[:, 30 - (t0 - lo):] = pT[row0:row0 + 2048, lo:t0 + TOK_PER_CORE]
            m[name] = np.ascontiguousarray(blk.reshape(16, 128, TOK_PER_CORE + 30))
        m["ddg"] = dcw
        m["dlv"] = dlv
        maps.append(m)
    out = _launch(nc, maps)
    catTs = [np.empty((D_MODEL, SEQ), np.float32) for _ in range(BATCH)]
    for c in range(NCORES):
        b, r = divmod(c, CORES_PER_BATCH)
        catTs[b][4 * r * 128:(4 * r + 4) * 128] = out[c]["co"]
        catTs[b][2048:4096, r * TOK_PER_CORE:(r + 1) * TOK_PER_CORE] = out[c]["dout"]
    return catTs


def _run_ada(c, w_adas, b_adas):
    ncol = 9 * D_MODEL // NCORES
    cT = np.ascontiguousarray(np.asarray(c).T.reshape(D_MODEL // 128, 128, BATCH).transpose(1, 0, 2))
    nc = build_ada(D_MODEL, ncol, len(w_adas), BATCH)
    maps = []
    for k in range(NCORES):
        m = {"cT": cT}
        for l in range(len(w_adas)):
            m["w%d" % l] = np.ascontiguousarray(np.asarray(w_adas[l])[:, k * ncol:(k + 1) * ncol])
            m["b%d" % l] = np.ascontiguousarray(np.broadcast_to(np.asarray(b_adas[l])[k * ncol:(k + 1) * ncol], (BATCH, ncol)))
        maps.append(m)
    out = _launch(nc, maps)
    mods = []
    for l in range(len(w_adas)):
        full = np.concatenate([out[k]["m%d" % l] for k in range(NCORES)], axis=1)
        mods.append(full.reshape(BATCH, 9, D_MODEL))
    return mods


def _pad_w_in(w_in):
    w_in = np.asarray(w_in)
    ncol = w_in.shape[1]
    nch = -(-ncol // 128)
    if nch * 128 != ncol:
        w_in = np.concatenate([w_in, np.zeros((w_in.shape[0], nch * 128 - ncol), np.float32)], axis=1)
    return tile_w(w_in, 32), nch


def _run_chain(xTs, stages):
    spec = []
    shared = {}
    for i, st in enumerate(stages):
        if st['kind'] == 'proj':
            wt, nch = _pad_w_in(st['w_in'])
            shared["w_in%d" % i] = wt
            spec.append(('proj', nch))
        elif st['kind'] == 'out':
            shared["w_o%d" % i] = tile_w(np.asarray(st['w_out']), 32)
            spec.append(('out', None))
        else:
            shared["w_up%d" % i] = tile_w(np.asarray(st['w_up']), 32)
            shared["w_dn%d" % i] = tile_w(np.asarray(st['w_dn']), 32)
            spec.append(('ffn', None))
    nc = build_chain(D_MODEL, D_FF, TOK_PER_CORE, spec, OUT_T, FFN_T, FFN_TILES, PROJ_T)
    maps = []
    for c in range(NCORES):
        b, r = divmod(c, CORES_PER_BATCH)
        m = dict(shared)
        m["xT"] = xTs[c]
        for i, st in enumerate(stages):
            m["vecs%d" % i] = _vecs(st['mod_l'], b, st['j'], st['ln_g'], st['ln_b'])
            if st['kind'] == 'out':
                m["cT%d" % i] = np.ascontiguousarray(st['catTs'][b][:, r * TOK_PER_CORE:(r + 1) * TOK_PER_CORE])
        maps.append(m)
    out = _launch(nc, maps)
    xprod = [i for i, st in enumerate(stages) if st['kind'] != 'proj']
    xo = [np.ascontiguousarray(o["oT%d" % xprod[-1]]) for o in out]
    projT = None
    for i, st in enumerate(stages):
        if st['kind'] == 'proj':
            projT = [np.concatenate([out[b * CORES_PER_BATCH + r]["pT%d" % i] for r in range(CORES_PER_BATCH)], axis=1)
                     for b in range(BATCH)]
    return xo, projT


def kernel(x, c,
           l0_w_ada, l0_b_ada, l0_ln_g, l0_ln_b, l0_ffn1_w_up, l0_ffn1_w_down, l0_ffn2_w_up, l0_ffn2_w_down,
           l0_w_in, l0_conv_qk, l0_b_igate, l0_b_fgate, l0_mlstm_norm_g, l0_w_out,
           l1_w_ada, l1_b_ada, l1_ln_g, l1_ln_b, l1_ffn1_w_up, l1_ffn1_w_down, l1_ffn2_w_up, l1_ffn2_w_down,
           l1_w_in, l1_conv_dw, l1_conv_ln_g, l1_conv_ln_b, l1_w_out):
    x = np.asarray(x, dtype=np.float32)
    xTs = []
    for k in range(NCORES):
        b, r = divmod(k, CORES_PER_BATCH)
        xTs.append(np.ascontiguousarray(x[b, r * TOK_PER_CORE:(r + 1) * TOK_PER_CORE, :].T))
    mods = _run_ada(c, [l0_w_ada, l1_w_ada], [l0_b_ada, l1_b_ada])
    g0, b0, g1, b1 = (np.asarray(v) for v in (l0_ln_g, l0_ln_b, l1_ln_g, l1_ln_b))

    def st(kind, l, j, **kw):
        d = {'kind': kind, 'mod_l': mods[l], 'j': j, 'ln_g': (g0, g1)[l], 'ln_b': (b0, b1)[l]}
        d.update(kw)
        return d

    xTs, projT = _run_chain(xTs, [st('ffn', 0, 0, w_up=l0_ffn1_w_up, w_dn=l0_ffn1_w_down), st('proj', 0, 1, w_in=l0_w_in)])
    catTs = _run_mix0(projT, l0_conv_qk, np.asarray(l0_b_igate), np.asarray(l0_b_fgate), l0_mlstm_norm_g)
    del projT
    xTs, projT = _run_chain(xTs, [st('out', 0, 1, w_out=l0_w_out, catTs=catTs),
                                  st('ffn', 0, 2, w_up=l0_ffn2_w_up, w_dn=l0_ffn2_w_down),
                                  st('ffn', 1, 0, w_up=l1_ffn1_w_up, w_dn=l1_ffn1_w_down),
                                  st('proj', 1, 1, w_in=l1_w_in)])
    del catTs
    catTs = _run_mix1(projT, l1_conv_dw, l1_conv_ln_g, l1_conv_ln_b)
    del projT
    xTs, _ = _run_chain(xTs, [st('out', 1, 1, w_out=l1_w_out, catTs=catTs),
                              st('ffn', 1, 2, w_up=l1_ffn2_w_up, w_dn=l1_ffn2_w_down)])
    out = np.empty((BATCH, SEQ, D_MODEL), np.float32)
    for k in range(NCORES):
        b, r = divmod(k, CORES_PER_BATCH)
        out[b, r * TOK_PER_CORE:(r + 1) * TOK_PER_CORE, :] = xTs[k].T
    return out


def conv_diag(conv_dw):
    W, C = conv_dw.shape
    out = np.zeros((C // 128, 128, W, 128), np.float32)
    idx = np.arange(128)
    wt = np.asarray(conv_dw).T.reshape(C // 128, 128, W)
    out[:, idx, :, idx] = wt.transpose(1, 0, 2)
    return out.reshape(C // 128, 128, W * 128)
```
